# Optimizing a Trainium2 kernel written in Bass

```python
import math
import jax
import jax.numpy as jnp
from jax import lax
import numpy as np


D_MODEL = 1024
BATCH = 8
SEQ = 4096
DEPTH = 2

HEAD_DIM = 64
ROPE_THETA = 10000.0
BLOCK = 128
LN_EPS = 1e-5

POOL_WINDOWS = (2, 4, 8, 16)
POOL_GROUP = 64
POOL_WIDTH = len(POOL_WINDOWS) * POOL_GROUP
SWA_WINDOW = 128
SWA_Q_HEADS = 12
SWA_KV_HEADS = 4
SWA_Q_DIM = SWA_Q_HEADS * HEAD_DIM
SWA_KV_DIM = SWA_KV_HEADS * HEAD_DIM
DIL_PAIRS = ((128, 1), (512, 4), (2048, 16))
DIL_Q_HEADS = 8
DIL_KV_HEADS = 2
DIL_Q_DIM = DIL_Q_HEADS * HEAD_DIM
DIL_KV_DIM = DIL_KV_HEADS * HEAD_DIM
DIL_GROUP_IN = DIL_Q_DIM + 2 * DIL_KV_DIM
CONV_WIDTH = 512
CONV_K = 3
D_FF = 2816
N_EXPERTS = 8
TOP_K = 2
D_FF_EXPERT = 3584
MOE_BLOCK = 256

ALPHA = (2 * DEPTH) ** 0.25
BETA = (8 * DEPTH) ** -0.25
N_EVEN = (DEPTH + 1) // 2
N_ODD = DEPTH // 2
EVEN_IN = POOL_WIDTH + SWA_Q_DIM + 2 * SWA_KV_DIM
EVEN_MIX = POOL_WIDTH + SWA_Q_DIM
C_IN = len(DIL_PAIRS) * DIL_GROUP_IN
ODD_IN = C_IN + 3 * CONV_WIDTH
ODD_MIX = DIL_Q_DIM + CONV_WIDTH

kernel_name = 'hybrid_pool_swa_dilated_conv_moe'


def layer_norm(x, w, b):
    xf = x.astype(jnp.float32)
    mu = xf.mean(-1, keepdims=True)
    var = jnp.square(xf - mu).mean(-1, keepdims=True)
    return ((xf - mu) * lax.rsqrt(var + LN_EPS) * w + b).astype(x.dtype)


def rope(x, positions):
    half = x.shape[-1] // 2
    inv = ROPE_THETA ** (-jnp.arange(half, dtype=jnp.float32) / half)
    ang = positions.astype(jnp.float32)[..., None] * inv
    cos = jnp.cos(ang)[:, :, None, :]
    sin = jnp.sin(ang)[:, :, None, :]
    xf = x.astype(jnp.float32)
    x1, x2 = xf[..., :half], xf[..., half:]
    return jnp.concatenate([x1 * cos - x2 * sin, x2 * cos + x1 * sin], -1).astype(x.dtype)


def banded_attention(q, k, v, max_dist, sinks=None):
    n, L, H, dh = q.shape
    kvh = k.shape[2]
    g = H // kvh
    nblk = -(-L // BLOCK)
    Lp = nblk * BLOCK
    pad = ((0, 0), (0, Lp - L), (0, 0), (0, 0))
    q, k, v = jnp.pad(q, pad), jnp.pad(k, pad), jnp.pad(v, pad)
    qb = q.reshape(n, nblk, BLOCK, kvh, g, dh).transpose(1, 0, 2, 3, 4, 5)

    def windows(t):
        tb = t.reshape(n, nblk, BLOCK, kvh, dh)
        prev = jnp.concatenate([jnp.zeros_like(tb[:, :1]), tb[:, :-1]], axis=1)
        return jnp.concatenate([prev, tb], axis=2).transpose(1, 0, 2, 3, 4)

    kw, vw = windows(k), windows(v)
    qi = jnp.arange(BLOCK)[:, None]
    sj = jnp.arange(2 * BLOCK)[None, :]
    dist = BLOCK + qi - sj
    band = (dist >= 0) & (dist <= max_dist)
    scale = 1.0 / math.sqrt(dh)

    def block(args):
        qblk, kblk, vblk, bi = args
        s = jnp.einsum('nqkgd,nskd->nkgqs', qblk, kblk, preferred_element_type=jnp.float32) * scale
        valid = band & ((bi * BLOCK + sj - BLOCK) >= 0)
        s = jnp.where(valid, s, -jnp.inf)
        m = s.max(-1, keepdims=True)
        if sinks is not None:
            sk = sinks.astype(jnp.float32).reshape(kvh, g, 1, 1)
            m = jnp.maximum(m, sk)
        p = jnp.exp(s - m)
        den = p.sum(-1, keepdims=True)
        if sinks is not None:
            den = den + jnp.exp(sk - m)
        o = jnp.einsum('nkgqs,nskd->nqkgd', (p / den).astype(vblk.dtype), vblk)
        return o, (m + jnp.log(den))[..., 0]

    o, lse = lax.map(block, (qb, kw, vw, jnp.arange(nblk)))
    o = o.transpose(1, 0, 2, 3, 4, 5).reshape(n, Lp, H, dh)[:, :L]
    lse = lse.transpose(1, 0, 4, 2, 3).reshape(n, Lp, H)[:, :L]
    return o, lse


def multiscale_pool(u, w_group, scale):
    b, s, _ = u.shape
    uf = u.astype(jnp.float32)
    csum = jnp.cumsum(uf, axis=1)
    count = jnp.arange(1, s + 1, dtype=jnp.float32)[None, :, None]
    diffs = []
    for gi, w in enumerate(POOL_WINDOWS):
        sl = slice(gi * POOL_GROUP, (gi + 1) * POOL_GROUP)
        cg = csum[..., sl]
        lagged = jnp.pad(cg, ((0, 0), (w, 0), (0, 0)))[:, :s]
        diffs.append((cg - lagged) / jnp.minimum(count, w) - uf[..., sl])
    d = jnp.stack(diffs, axis=2).astype(u.dtype)
    y = jnp.einsum('bsgc,gcd->bsgd', d, w_group).reshape(b, s, POOL_WIDTH)
    return y * scale


def even_mixer(x, positions, w_in, pool_w, pool_scale, sinks, w_out):
    b, s, _ = x.shape
    h = x @ w_in
    u, q, k, v = jnp.split(h, [POOL_WIDTH, POOL_WIDTH + SWA_Q_DIM, POOL_WIDTH + SWA_Q_DIM + SWA_KV_DIM], axis=-1)
    a_out = multiscale_pool(u, pool_w, pool_scale)
    q = rope(q.reshape(b, s, SWA_Q_HEADS, HEAD_DIM), positions)
    k = rope(k.reshape(b, s, SWA_KV_HEADS, HEAD_DIM), positions)
    v = v.reshape(b, s, SWA_KV_HEADS, HEAD_DIM)
    b_out, _ = banded_attention(q, k, v, SWA_WINDOW - 1, sinks)
    return jnp.concatenate([a_out, b_out.reshape(b, s, SWA_Q_DIM)], -1) @ w_out


def dilated_attention(q, k, v, window, dil):
    b, s = q.shape[:2]

    def to_sub(t):
        return t.reshape(b, s // dil, dil, *t.shape[2:]).swapaxes(1, 2).reshape(b * dil, s // dil, *t.shape[2:])

    def from_sub(t):
        return t.reshape(b, dil, s // dil, *t.shape[2:]).swapaxes(1, 2).reshape(b, s, *t.shape[2:])

    o, lse = banded_attention(to_sub(q), to_sub(k), to_sub(v), window // dil)
    return from_sub(o), from_sub(lse)


def short_conv(h, gate_b, gate_c, conv_w):
    z = gate_c * h
    y = lax.conv_general_dilated(z, conv_w[:, None, :], window_strides=(1,), padding=[(CONV_K - 1, 0)],
                                 dimension_numbers=('NWC', 'WIO', 'NWC'), feature_group_count=CONV_WIDTH)
    return gate_b * y


def odd_mixer(x, positions, w_in, conv_w, w_out):
    b, s, _ = x.shape
    h = x @ w_in
    outs, lses = [], []
    for gi, (window, dil) in enumerate(DIL_PAIRS):
        hg = h[..., gi * DIL_GROUP_IN:(gi + 1) * DIL_GROUP_IN]
        q, k, v = jnp.split(hg, [DIL_Q_DIM, DIL_Q_DIM + DIL_KV_DIM], axis=-1)
        q = rope(q.reshape(b, s, DIL_Q_HEADS, HEAD_DIM), positions)
        k = rope(k.reshape(b, s, DIL_KV_HEADS, HEAD_DIM), positions)
        v = v.reshape(b, s, DIL_KV_HEADS, HEAD_DIM)
        o, lse = dilated_attention(q, k, v, window, dil)
        outs.append(o)
        lses.append(lse)
    wts = jax.nn.softmax(jnp.stack(lses), axis=0)
    c_out = jnp.sum(wts[..., None] * jnp.stack(outs).astype(jnp.float32), axis=0).astype(x.dtype)
    hd, gate_b, gate_c = jnp.split(h[..., C_IN:], 3, axis=-1)
    d_out = short_conv(hd, gate_b, gate_c, conv_w)
    return jnp.concatenate([c_out.reshape(b, s, DIL_Q_DIM), d_out], -1) @ w_out


def swiglu(x, w_gate, w_up, w_down):
    return (jax.nn.silu(x @ w_gate) * (x @ w_up)) @ w_down


def moe_swiglu(x, w_router, w_gate, w_up, w_down):
    b, s, d = x.shape
    xt = x.reshape(-1, d)
    T = xt.shape[0]
    logits = (xt @ w_router).astype(jnp.float32)
    top_logit, top_e = lax.top_k(logits, TOP_K)
    gates = jax.nn.softmax(top_logit, axis=-1)
    e_flat = top_e.reshape(-1)
    g_flat = gates.reshape(-1)
    tok_flat = jnp.arange(T * TOP_K, dtype=jnp.int32) // TOP_K
    order = jnp.argsort(e_flat)
    e_sorted = e_flat[order]
    counts = jnp.bincount(e_flat, length=N_EXPERTS)
    padded = (counts + MOE_BLOCK - 1) // MOE_BLOCK * MOE_BLOCK
    start = jnp.cumsum(counts) - counts
    pend = jnp.cumsum(padded)
    pstart = pend - padded
    dest = pstart[e_sorted] + jnp.arange(T * TOP_K) - start[e_sorted]
    n_rows = -(-(T * TOP_K + N_EXPERTS * (MOE_BLOCK - 1)) // MOE_BLOCK) * MOE_BLOCK
    n_blocks = n_rows // MOE_BLOCK
    row_tok = jnp.full((n_rows,), T, jnp.int32).at[dest].set(tok_flat[order])
    row_gate = jnp.zeros((n_rows,), jnp.float32).at[dest].set(g_flat[order])
    block_e = jnp.minimum(jnp.searchsorted(pend, jnp.arange(n_blocks) * MOE_BLOCK, side='right'), N_EXPERTS - 1)
    xrows = jnp.concatenate([xt, jnp.zeros((1, d), xt.dtype)], 0)[row_tok].reshape(n_blocks, MOE_BLOCK, d)

    def expert_block(args):
        xb, e = args
        return (jax.nn.silu(xb @ w_gate[e]) * (xb @ w_up[e])) @ w_down[e]

    y = lax.map(expert_block, (xrows, block_e)).reshape(n_rows, d)
    out = jnp.zeros((T + 1, d), jnp.float32).at[row_tok].add(y.astype(jnp.float32) * row_gate[:, None])
    return out[:T].astype(x.dtype).reshape(b, s, d)


def setup_inputs(seed: int = 0) -> dict:
    key = jax.random.key(seed)
    ks = jax.random.split(key, 20)
    f32 = jnp.float32

    def nrm(k, shape, fan_in, gain=1.0):
        return jax.random.normal(k, shape, f32) * (gain * fan_in ** -0.5)

    x = jax.random.normal(ks[0], (BATCH, SEQ, D_MODEL), f32)
    offsets = jax.random.randint(ks[1], (BATCH, 1), 0, SEQ, jnp.int32)
    positions = offsets + jnp.arange(SEQ, dtype=jnp.int32)[None, :]
    ln_w = 1.0 + 0.02 * jax.random.normal(ks[2], (DEPTH, 2, D_MODEL), f32)
    ln_b = 0.02 * jax.random.normal(ks[3], (DEPTH, 2, D_MODEL), f32)
    even_w_in = nrm(ks[4], (N_EVEN, D_MODEL, EVEN_IN), D_MODEL)
    pool_w = nrm(ks[5], (N_EVEN, len(POOL_WINDOWS), POOL_GROUP, POOL_GROUP), POOL_GROUP)
    pool_scale = 1.0 + 0.02 * jax.random.normal(ks[6], (N_EVEN, POOL_WIDTH), f32)
    swa_sinks = 0.5 * jax.random.normal(ks[7], (N_EVEN, SWA_Q_HEADS), f32)
    even_w_out = nrm(ks[8], (N_EVEN, EVEN_MIX, D_MODEL), EVEN_MIX, BETA)
    ffn_w_gate = nrm(ks[9], (N_EVEN, D_MODEL, D_FF), D_MODEL)
    ffn_w_up = nrm(ks[10], (N_EVEN, D_MODEL, D_FF), D_MODEL)
    ffn_w_down = nrm(ks[11], (N_EVEN, D_FF, D_MODEL), D_FF, BETA)
    odd_w_in = nrm(ks[12], (N_ODD, D_MODEL, ODD_IN), D_MODEL)
    conv_w = nrm(ks[13], (N_ODD, CONV_K, CONV_WIDTH), CONV_K)
    odd_w_out = nrm(ks[14], (N_ODD, ODD_MIX, D_MODEL), ODD_MIX, BETA)
    router_w = nrm(ks[15], (N_ODD, D_MODEL, N_EXPERTS), D_MODEL)
    moe_w_gate = nrm(ks[16], (N_ODD, N_EXPERTS, D_MODEL, D_FF_EXPERT), D_MODEL)
    moe_w_up = nrm(ks[17], (N_ODD, N_EXPERTS, D_MODEL, D_FF_EXPERT), D_MODEL)
    moe_w_down = nrm(ks[18], (N_ODD, N_EXPERTS, D_FF_EXPERT, D_MODEL), D_FF_EXPERT, BETA)
    return {'x': x, 'positions': positions, 'ln_w': ln_w, 'ln_b': ln_b,
            'even_w_in': even_w_in, 'pool_w': pool_w, 'pool_scale': pool_scale, 'swa_sinks': swa_sinks,
            'even_w_out': even_w_out, 'ffn_w_gate': ffn_w_gate, 'ffn_w_up': ffn_w_up, 'ffn_w_down': ffn_w_down,
            'odd_w_in': odd_w_in, 'conv_w': conv_w, 'odd_w_out': odd_w_out, 'router_w': router_w,
            'moe_w_gate': moe_w_gate, 'moe_w_up': moe_w_up, 'moe_w_down': moe_w_down}


def reference(x, positions, ln_w, ln_b, even_w_in, pool_w, pool_scale, swa_sinks, even_w_out,
              ffn_w_gate, ffn_w_up, ffn_w_down, odd_w_in, conv_w, odd_w_out, router_w,
              moe_w_gate, moe_w_up, moe_w_down):
    for layer in range(DEPTH):
        i = layer // 2
        if layer % 2 == 0:
            mix = even_mixer(x, positions, even_w_in[i], pool_w[i], pool_scale[i], swa_sinks[i], even_w_out[i])
        else:
            mix = odd_mixer(x, positions, odd_w_in[i], conv_w[i], odd_w_out[i])
        x = layer_norm(ALPHA * x + mix, ln_w[layer, 0], ln_b[layer, 0])
        if layer % 2 == 0:
            ffn = swiglu(x, ffn_w_gate[i], ffn_w_up[i], ffn_w_down[i])
        else:
            ffn = moe_swiglu(x, router_w[i], moe_w_gate[i], moe_w_up[i], moe_w_down[i])
        x = layer_norm(ALPHA * x + ffn, ln_w[layer, 1], ln_b[layer, 1])
    return x
```

```python
import numpy as np
import concourse.bass as bass
import concourse.mybir as mybir
from concourse.bass_utils import run_bass_kernel_spmd

F32, BF16, I32 = mybir.dt.float32, mybir.dt.bfloat16, mybir.dt.int32
AF = mybir.ActivationFunctionType
ALU = mybir.AluOpType
AX = mybir.AxisListType

D = 1024
S = 4096
NB = 8
ALPHA = 4.0 ** 0.25
LN_EPS = 1e-5
D_FF = 2816
N_EXP = 8
D_FFE = 3584
KC = D // 128
SAME_SYNC = True
NEG = -30000.0


class Eng:
    def __init__(self, nc, e, name):
        self.e = e
        self.sem = nc.alloc_semaphore(name)
        self.n = 0
        self.seen = {}
        self.name = name


class Buf:
    __slots__ = ("w", "r", "name")

    def __init__(self, name=""):
        self.w = None
        self.r = {}
        self.name = name


class Ctx:
    def __init__(self, nc):
        self.nc = nc
        self.PE = Eng(nc, nc.tensor, "s_pe")
        self.ACT = Eng(nc, nc.scalar, "s_act")
        self.DVE = Eng(nc, nc.vector, "s_dve")
        self.POOL = Eng(nc, nc.gpsimd, "s_pool")
        self.SP = Eng(nc, nc.sync, "s_sp")
        self.engs = [self.PE, self.ACT, self.DVE, self.POOL, self.SP]
        self.dsems = []
        self.stack = []
        self.psum = []

    def dsem(self, name):
        self.uid = getattr(self, "uid", 0) + 1
        d = Eng(self.nc, None, f"{name}_{self.uid}")
        self.dsems.append(d)
        return d

    def _wait(self, E, deps):
        best = {}
        for (E2, v) in deps:
            if E2 is E and not SAME_SYNC:
                continue
            if best.get(E2, 0) < v:
                best[E2] = v
        for E2, v in best.items():
            if E.seen.get(E2, 0) < v:
                E.e.wait_ge(E2.sem, v)
                E.seen[E2] = v

    @staticmethod
    def _deps(reads, writes):
        deps = []
        for b in reads:
            if b.w is not None:
                deps.append(b.w)
        for b in writes:
            if b.w is not None:
                deps.append(b.w)
            deps.extend(b.r.items())
        return deps

    def op(self, E, fn, reads=(), writes=()):
        self._wait(E, self._deps(reads, writes))
        ins = fn()
        E.n += 1
        ins.then_inc(E.sem, 1)
        ev = (E, E.n)
        for b in reads:
            b.r[E] = E.n
        for b in writes:
            b.w = ev
            b.r = {}
        return ev

    def dma(self, Q, Dm, fn, reads=(), writes=()):
        self._wait(Q, self._deps(reads, writes))
        ins = fn()
        Dm.n += 16
        ins.then_inc(Dm.sem, 16)
        ev = (Dm, Dm.n)
        for b in reads:
            b.r[Dm] = Dm.n
        for b in writes:
            b.w = ev
            b.r = {}
        return ev

    def barrier(self, include=None):
        allc = self.engs + [d for d in self.dsems if (include is None or d in include)]
        for E in self.engs:
            for X in allc:
                if X is E:
                    continue
                if X.n > 0 and E.seen.get(X, 0) < X.n:
                    E.e.wait_ge(X.sem, X.n)
                    E.seen[X] = X.n

    def sb(self, name, shape, dt):
        self.uid = getattr(self, "uid", 0) + 1
        g = self.nc.sbuf_tensor(f"{name}_{self.uid}", list(shape), dt)
        t = g.__enter__()
        self.stack.append(g)
        return t

    def mark(self):
        return len(self.stack)

    def release(self, mark):
        while len(self.stack) > mark:
            g = self.stack.pop()
            g.__exit__(None, None, None)


def _mm(c, out, lhsT, rhs, start, stop):
    return c.nc.tensor.matmul(out, lhsT, rhs, start=start, stop=stop)


def build_consts(c):
    nc = c.nc
    ident = c.sb("k_ident", [128, 128], BF16)
    idx = c.sb("k_idx", [128, 128], F32)
    iot = c.sb("k_iot", [128, 512], F32)
    gp = c.sb("k_gp", [128, 7], F32)
    b = Buf()
    c.op(c.POOL, lambda: nc.gpsimd.iota(gp[:], [[128, 7]], base=0, channel_multiplier=1,
                                        allow_small_or_imprecise_dtypes=True), writes=[b])
    c.op(c.POOL, lambda: nc.gpsimd.iota(idx[:], [[1, 128]], base=0, channel_multiplier=-1,
                                        allow_small_or_imprecise_dtypes=True), writes=[b])
    c.op(c.POOL, lambda: nc.gpsimd.iota(iot[:], [[1, 512]], base=0, channel_multiplier=0,
                                        allow_small_or_imprecise_dtypes=True), writes=[b])
    bi = Buf()
    c.op(c.DVE, lambda: nc.vector.tensor_scalar(ident[:], idx[:], 0.0, None, ALU.is_equal), reads=[b], writes=[bi])
    c.barrier()
    c.gp = gp
    return ident, idx, iot


def layer_norm_tile(c, src, dst, lnw, lnb, tmp, st, mv, bsrc, bdst, btmp, bsmall, pool_add=True):
    nc = c.nc
    c.op(c.DVE, lambda: nc.vector.bn_stats(st[:, 0:6], src[:, 0:512]), reads=[bsrc], writes=[bsmall])
    c.op(c.DVE, lambda: nc.vector.bn_stats(st[:, 6:12], src[:, 512:1024]), reads=[bsrc, bsmall], writes=[bsmall])
    c.op(c.DVE, lambda: nc.vector.bn_aggr(mv[:, 0:2], st[:, 0:12]), reads=[bsmall], writes=[bsmall])
    c.op(c.DVE, lambda: nc.vector.tensor_scalar(mv[:, 2:3], mv[:, 1:2], LN_EPS, None, ALU.add), reads=[bsmall], writes=[bsmall])
    c.op(c.ACT, lambda: nc.scalar.activation(out=mv[:, 3:4], in_=mv[:, 2:3], func=AF.Sqrt), reads=[bsmall], writes=[bsmall])
    c.op(c.DVE, lambda: nc.vector.reciprocal(mv[:, 4:5], mv[:, 3:4]), reads=[bsmall], writes=[bsmall])
    c.op(c.DVE, lambda: nc.vector.tensor_scalar(mv[:, 5:6], mv[:, 0:1], -1.0, mv[:, 4:5], ALU.mult, ALU.mult),
         reads=[bsmall], writes=[bsmall])
    c.op(c.ACT, lambda: nc.scalar.activation(out=dst, in_=src, func=AF.Identity, scale=mv[:, 4:5], bias=mv[:, 5:6]),
         reads=[bsrc, bsmall], writes=[bdst])
    c.op(c.DVE, lambda: nc.vector.tensor_tensor(dst, dst, lnw, ALU.mult), reads=[bdst], writes=[bdst])
    if pool_add:
        c.op(c.POOL, lambda: nc.gpsimd.tensor_tensor(dst, dst, lnb, ALU.add), reads=[bdst], writes=[bdst])
    else:
        c.op(c.DVE, lambda: nc.vector.tensor_tensor(dst, dst, lnb, ALU.add), reads=[bdst], writes=[bdst])


class LNPipe:
    def __init__(self, c, lnw, lnb, name, nbuf=4, pool_add=True, store="sp"):
        self.c, self.lnw, self.lnb, self.pool_add, self.store = c, lnw, lnb, pool_add, store
        self.st = [c.sb(f"{name}_st{i}", [128, 12], F32) for i in range(nbuf)]
        self.mv = [c.sb(f"{name}_mv{i}", [128, 8], F32) for i in range(nbuf)]
        self.bs = [Buf() for _ in range(nbuf)]
        self.n = 0
        self.q = []

    def push(self, src, dst, bsrc, bdst, fin=None):
        k = self.n % len(self.st)
        self.n += 1
        self.q.append(dict(stage=0, src=src, dst=dst, bsrc=bsrc, bdst=bdst, fin=fin, k=k))

    def step(self):
        c, nc = self.c, self.c.nc
        for it in list(self.q):
            st, mv, bsm = self.st[it["k"]], self.mv[it["k"]], self.bs[it["k"]]
            src, dst, bsrc, bdst = it["src"], it["dst"], it["bsrc"], it["bdst"]
            if it["stage"] == 0:
                c.op(c.DVE, lambda: nc.vector.bn_stats(st[:, 0:6], src[:, 0:512]), reads=[bsrc], writes=[bsm])
                c.op(c.DVE, lambda: nc.vector.bn_stats(st[:, 6:12], src[:, 512:1024]), reads=[bsrc, bsm], writes=[bsm])
                c.op(c.DVE, lambda: nc.vector.bn_aggr(mv[:, 0:2], st[:, 0:12]), reads=[bsm], writes=[bsm])
                c.op(c.DVE, lambda: nc.vector.tensor_scalar(mv[:, 2:3], mv[:, 1:2], LN_EPS, None, ALU.add), reads=[bsm], writes=[bsm])
                c.op(c.ACT, lambda: nc.scalar.activation(out=mv[:, 3:4], in_=mv[:, 2:3], func=AF.Sqrt), reads=[bsm], writes=[bsm])
            elif it["stage"] == 1:
                c.op(c.DVE, lambda: nc.vector.reciprocal(mv[:, 4:5], mv[:, 3:4]), reads=[bsm], writes=[bsm])
                c.op(c.DVE, lambda: nc.vector.tensor_scalar(mv[:, 5:6], mv[:, 0:1], -1.0, mv[:, 4:5], ALU.mult, ALU.mult),
                     reads=[bsm], writes=[bsm])
                c.op(c.ACT, lambda: nc.scalar.activation(out=dst, in_=src, func=AF.Identity, scale=mv[:, 4:5], bias=mv[:, 5:6]),
                     reads=[bsrc, bsm], writes=[bdst])
            elif it["stage"] == 2:
                c.op(c.DVE, lambda: nc.vector.tensor_tensor(dst, dst, self.lnw, ALU.mult), reads=[bdst], writes=[bdst])
                if self.pool_add:
                    c.op(c.POOL, lambda: nc.gpsimd.tensor_tensor(dst, dst, self.lnb, ALU.add), reads=[bdst], writes=[bdst])
                else:
                    c.op(c.DVE, lambda: nc.vector.tensor_tensor(dst, dst, self.lnb, ALU.add), reads=[bdst], writes=[bdst])
                if self.store == "pool":
                    if it["fin"] is not None:
                        it["fin"](c.POOL, nc.gpsimd)
                    self.q.remove(it)
                elif self.store == "sp":
                    if it["fin"] is not None:
                        it["fin"](c.SP, nc.sync)
                    self.q.remove(it)
            if it["stage"] == 3:
                if it["fin"] is not None:
                    it["fin"](c.SP, nc.sync)
                self.q.remove(it)
                continue
            it["stage"] += 1

    def flush(self):
        while self.q:
            self.step()


def ffn_phase(c, x_in, x_out, wg, wu, wd, E, FF, CG, lnw_d, lnb_d, ident, router_d=None, TP=1024, seq=S,
              wready=None, pre_hook=None, pass_hooks=None):
    nc = c.nc
    m0 = c.mark()
    if pre_hook is not None:
        pre_hook()
    NS = TP // 128
    NT = TP // 512
    NP = seq // TP
    NG = FF // (CG * 128)
    assert FF % (CG * 128) == 0 and CG % 2 == 0
    moe = router_d is not None

    xT2 = [c.sb(f"f_xT{i}", [128, KC, TP], BF16) for i in range(2)]
    yacc2 = [c.sb(f"f_yacc{i}", [128, NS, D], F32) for i in range(2)]
    xin = [c.sb(f"f_xin{i}", [128, D], F32) for i in range(2)]
    xb = [c.sb(f"f_xb{i}", [128, D], BF16) for i in range(2)]
    wgb = [c.sb(f"f_wg{i}", [128, KC, CG * 128], BF16) for i in range(2)]
    wub = [c.sb(f"f_wu{i}", [128, KC, CG * 128], BF16) for i in range(2)]
    wdb = [c.sb(f"f_wd{i}", [128, CG, D], BF16) for i in range(2)]
    sg = [c.sb(f"f_sg{i}", [128, 512], BF16) for i in range(2)]
    hT = [c.sb(f"f_hT{i}", [128, CG, 512], BF16) for i in range(2)]
    otile = [c.sb(f"f_ot{i}", [128, D], F32) for i in range(3)]
    tmp = c.sb("f_tmp", [128, D], F32)
    lnw = c.sb("f_lnw", [128, D], F32)
    lnb = c.sb("f_lnb", [128, D], F32)
    st = c.sb("f_st", [128, 12], F32)
    mv = c.sb("f_mv", [128, 8], F32)
    if moe:
        wr = c.sb("f_wr", [128, N_EXP, D], F32)
        G = c.sb("f_G", [128, NS, N_EXP], F32)
        lg = c.sb("f_lg", [128, 8 * N_EXP], F32)
    b_xT2 = [[Buf() for _ in range(NS)] for _ in range(2)]
    b_yacc2 = [[Buf() for _ in range(NS)] for _ in range(2)]
    lnp = LNPipe(c, lnw[:], lnb[:], "f_ln", pool_add=False)
    b_xin = [Buf() for _ in range(2)]
    b_xb = [Buf() for _ in range(2)]
    b_wg = [Buf() for _ in range(2)]
    b_wu = [Buf() for _ in range(2)]
    b_wd = [Buf() for _ in range(2)]
    b_sg = [Buf() for _ in range(2)]
    b_hT = [[Buf() for _ in range(CG)] for _ in range(2)]
    b_ot = [Buf() for _ in range(3)]
    b_tmp, b_small, b_const, b_G, b_lg = Buf(), Buf(), Buf(), [Buf() for _ in range(NS)], Buf()
    ps = c.psum
    b_ps = c.b_psum
    psG, psU, psY = ps[0:2], ps[2:4], ps[4:8]
    b_psG, b_psU, b_psY = b_ps[0:2], b_ps[2:4], b_ps[4:8]
    d_xin = [c.dsem(f"fd_xin{i}") for i in range(2)]
    d_w = [[c.dsem(f"fd_w{t}{i}") for i in range(2)] for t in range(3)]
    d_ot = [c.dsem(f"fd_ot{i}") for i in range(3)]
    d_c = c.dsem("fd_c")

    c.dma(c.SP, d_c, lambda: nc.sync.dma_start(out=lnw[:], in_=lnw_d.partition_broadcast(128)), writes=[b_const])
    c.dma(c.SP, d_c, lambda: nc.sync.dma_start(out=lnb[:], in_=lnb_d.partition_broadcast(128)), reads=[], writes=[b_const])
    if moe:
        for e in range(N_EXP):
            c.dma(c.SP, d_c, lambda e=e: nc.sync.dma_start(
                out=wr[:, e, :], in_=router_d[e].partition_broadcast(128)), writes=[b_const])
    c.barrier()

    def prologue(p):
        tok0 = p * TP
        xT, yacc, b_xT, b_yacc = xT2[p % 2], yacc2[p % 2], b_xT2[p % 2], b_yacc2[p % 2]
        for s in range(NS):
            i = s % 2
            t0 = tok0 + s * 128
            c.dma(c.SP, d_xin[i], lambda: nc.sync.dma_start(out=xin[i][:], in_=x_in[t0:t0 + 128, :]), writes=[b_xin[i]])
            c.op(c.ACT, lambda: nc.scalar.activation(out=yacc[:, s, :], in_=xin[i][:], func=AF.Copy, scale=ALPHA),
                 reads=[b_xin[i]], writes=[b_yacc[s]])
            c.op(c.DVE, lambda: nc.vector.tensor_copy(xb[i][:], xin[i][:]), reads=[b_xin[i]], writes=[b_xb[i]])
            if moe:
                for e in range(N_EXP):
                    c.op(c.DVE, lambda: nc.vector.tensor_tensor(tmp[:], xin[i][:], wr[:, e, :], ALU.mult),
                         reads=[b_xin[i], b_const], writes=[b_tmp])
                    c.op(c.DVE, lambda: nc.vector.reduce_sum(lg[:, e:e + 1], tmp[:], axis=AX.X), reads=[b_tmp], writes=[b_lg])
                L = lg[:, 0:8]
                m1, eq, l2, m2, sel, nm1, ex, es, ss, rs = (lg[:, 8:9], lg[:, 16:24], lg[:, 24:32], lg[:, 9:10],
                                                            lg[:, 32:40], lg[:, 10:11], lg[:, 40:48], lg[:, 48:56],
                                                            lg[:, 11:12], lg[:, 12:13])
                R = dict(reads=[b_lg], writes=[b_lg])
                c.op(c.DVE, lambda: nc.vector.reduce_max(m1, L, axis=AX.X), **R)
                c.op(c.DVE, lambda: nc.vector.tensor_scalar(eq, L, m1, None, ALU.is_equal), **R)
                c.op(c.DVE, lambda: nc.vector.scalar_tensor_tensor(l2, eq, -1e30, L, ALU.mult, ALU.add), **R)
                c.op(c.DVE, lambda: nc.vector.reduce_max(m2, l2, axis=AX.X), **R)
                c.op(c.DVE, lambda: nc.vector.tensor_scalar(sel, L, m2, None, ALU.is_ge), **R)
                c.op(c.DVE, lambda: nc.vector.tensor_scalar(nm1, m1, -1.0, None, ALU.mult), **R)
                c.op(c.ACT, lambda: nc.scalar.activation(out=ex, in_=L, func=AF.Exp, bias=nm1, scale=1.0), **R)
                c.op(c.DVE, lambda: nc.vector.tensor_tensor(es, ex, sel, ALU.mult), **R)
                c.op(c.DVE, lambda: nc.vector.reduce_sum(ss, es, axis=AX.X), **R)
                c.op(c.DVE, lambda: nc.vector.reciprocal(rs, ss), **R)
                c.op(c.DVE, lambda: nc.vector.tensor_scalar(G[:, s, :], es, rs, None, ALU.mult),
                     reads=[b_lg], writes=[b_G[s]])
            pT = psY[s % 4][:].bitcast(BF16)
            bpT = b_psY[s % 4]

            def tr():
                ins = None
                for kc in range(KC):
                    ins = nc.tensor.transpose(pT[:, kc * 128:(kc + 1) * 128], xb[i][:, kc * 128:(kc + 1) * 128], ident[:])
                return ins
            c.op(c.PE, tr, reads=[b_xb[i]], writes=[bpT])
            c.op(c.ACT, lambda: nc.scalar.copy(out=xT[:, :, s * 128:(s + 1) * 128],
                                               in_=pT.rearrange("p (k t) -> p k t", k=KC)),
                 reads=[bpT], writes=[b_xT[s]])


    def main(p, deferred):
        tok0 = p * TP
        if pass_hooks and p < len(pass_hooks):
            pass_hooks[p]()
        xT, yacc, b_xT, b_yacc = xT2[p % 2], yacc2[p % 2], b_xT2[p % 2], b_yacc2[p % 2]
        items = [(e, g, tt) for e in range(E) for g in range(NG) for tt in range(NT)]
        groups = [(e, g) for e in range(E) for g in range(NG)]
        loaded = [-1]

        def load_group(j):
            if j >= len(groups) or j <= loaded[0]:
                return
            assert j == loaded[0] + 1
            loaded[0] = j
            e, g = groups[j]
            sl = j % 2
            c0 = g * CG * 128
            if wready:
                c._wait(c.SP, wready[e])
            c.dma(c.SP, d_w[0][sl], lambda: nc.sync.dma_start(
                out=wgb[sl][:], in_=wg[e].rearrange("(k p) n -> p k n", p=128)[:, :, c0:c0 + CG * 128]), writes=[b_wg[sl]])
            c.dma(c.SP, d_w[1][sl], lambda: nc.sync.dma_start(
                out=wub[sl][:], in_=wu[e].rearrange("(k p) n -> p k n", p=128)[:, :, c0:c0 + CG * 128]), writes=[b_wu[sl]])
            c.dma(c.SP, d_w[2][sl], lambda: nc.sync.dma_start(
                out=wdb[sl][:], in_=wd[e][c0:c0 + CG * 128, :].rearrange("(ci p) d -> p ci d", p=128)), writes=[b_wd[sl]])

        kcount = [0]

        def GU(idx, cis):
            e, g, tt = items[idx]
            j = e * NG + g
            sl = j % 2
            hb = idx % 2
            for ci in cis:
                k = kcount[0] % 2
                kcount[0] += 1

                def mmg(w, out):
                    ins = None
                    for kc in range(KC):
                        ins = _mm(c, out[:], w[sl][:, kc, ci * 128:(ci + 1) * 128], xT[:, kc, tt * 512:(tt + 1) * 512],
                                  kc == 0, kc == KC - 1)
                    return ins
                rx = [b_xT[tt * 4 + q] for q in range(4)]
                c.op(c.PE, lambda: mmg(wgb, psG[k]), reads=[b_wg[sl]] + rx, writes=[b_psG[k]])
                c.op(c.PE, lambda: mmg(wub, psU[k]), reads=[b_wu[sl]] + rx, writes=[b_psU[k]])
                c.op(c.ACT, lambda: nc.scalar.activation(out=sg[k][:], in_=psG[k][:], func=AF.Silu),
                     reads=[b_psG[k]], writes=[b_sg[k]])
                c.op(c.DVE, lambda: nc.vector.tensor_tensor(hT[hb][:, ci, :], sg[k][:], psU[k][:], ALU.mult),
                     reads=[b_sg[k], b_psU[k]], writes=[b_hT[hb][ci]])

        def DN(idx, sp):
            e, g, tt = items[idx]
            j = e * NG + g
            sl = j % 2
            hb = idx % 2
            for sub in (2 * sp, 2 * sp + 1):
                s = tt * 4 + sub
                for dh in range(2):
                    bk = (sub % 2) * 2 + dh

                    def mmd():
                        ins = None
                        for ci in range(CG):
                            ins = _mm(c, psY[bk][:], hT[hb][:, ci, sub * 128:(sub + 1) * 128],
                                      wdb[sl][:, ci, dh * 512:(dh + 1) * 512], ci == 0, ci == CG - 1)
                        return ins
                    c.op(c.PE, mmd, reads=[b_wd[sl]] + b_hT[hb], writes=[b_psY[bk]])
                    ysl = yacc[:, s, dh * 512:(dh + 1) * 512]
                    if moe:
                        c.op(c.DVE, lambda: nc.vector.scalar_tensor_tensor(ysl, psY[bk][:], G[:, s, e:e + 1], ysl,
                                                                           ALU.mult, ALU.add),
                             reads=[b_psY[bk], b_G[s], b_yacc[s]], writes=[b_yacc[s]])
                    else:
                        c.op(c.DVE, lambda: nc.vector.tensor_tensor(ysl, psY[bk][:], ysl, ALU.add),
                             reads=[b_psY[bk], b_yacc[s]], writes=[b_yacc[s]])

        half = CG // 2
        load_group(0)
        load_group(1)
        GU(0, range(CG))
        for idx in range(len(items)):
            e, g, tt = items[idx]
            j = e * NG + g
            nxt = idx + 1 if idx + 1 < len(items) else None
            DN(idx, 0)
            if nxt is not None:
                GU(nxt, range(0, half))
            DN(idx, 1)
            if nxt is not None:
                GU(nxt, range(half, CG))
            if tt == NT - 1:
                load_group(j + 2)
            if deferred:
                deferred.pop(0)()

    def epilogue(p):
        tok0 = p * TP
        yacc, b_yacc = yacc2[p % 2], b_yacc2[p % 2]
        work = []
        for s in range(NS):
            def w(s=s):
                i = s % 3
                t0 = tok0 + s * 128

                def fin(Q, q):
                    c.dma(Q, d_ot[i], lambda: q.dma_start(out=x_out[t0:t0 + 128, :], in_=otile[i][:]), reads=[b_ot[i]])
                lnp.push(yacc[:, s, :], otile[i][:], b_yacc[s], b_ot[i], fin)
                lnp.step()
            work.append(w)
        work.append(lnp.step)
        work.append(lnp.step)
        work.append(lnp.step)
        return work

    prologue(0)
    deferred = []
    for p in range(NP):
        main(p, deferred)
        while deferred:
            deferred.pop(0)()
        if p + 1 < NP:
            prologue(p + 1)
        deferred = epilogue(p)
    while deferred:
        deferred.pop(0)()
    lnp.flush()
    c.barrier()
    c.release(m0)


TWO_PI = float(np.float32(2.0 * np.pi))
PI_F = float(np.float32(np.pi))


def build_rope(c, pos_d, invf, sgn, cosT, sinT, b_tab, seq=S):
    nc = c.nc
    m = c.mark()
    CH = 1024
    posi = c.sb("rp_posi", [128, CH], I32)
    ang = c.sb("rp_ang", [128, CH], F32)
    wk = c.sb("rp_wk", [128, CH], F32)
    wi = c.sb("rp_wi", [128, CH], I32)
    r = c.sb("rp_r", [128, CH], F32)
    bp, ba, bw, bi, br = Buf(), Buf(), Buf(), Buf(), Buf()
    d = c.dsem("rp_d")
    for ch in range(seq // CH):
        sl = slice(ch * CH, (ch + 1) * CH)
        c.dma(c.SP, d, lambda: nc.sync.dma_start(out=posi[:], in_=pos_d[sl].partition_broadcast(128)), writes=[bp])
        c.op(c.DVE, lambda: nc.vector.tensor_copy(ang[:], posi[:]), reads=[bp], writes=[ba])
        c.op(c.DVE, lambda: nc.vector.tensor_scalar(ang[:], ang[:], invf[:, 0:1], None, ALU.mult), reads=[ba, b_tab], writes=[ba])
        for which in range(2):
            if which == 1:
                c.op(c.DVE, lambda: nc.vector.tensor_scalar(ang[:], ang[:], PI_F / 2, None, ALU.add), reads=[ba], writes=[ba])
            c.op(c.DVE, lambda: nc.vector.tensor_scalar(wk[:], ang[:], 1.0 / TWO_PI, None, ALU.mult), reads=[ba], writes=[bw])
            c.op(c.DVE, lambda: nc.vector.tensor_copy(wi[:], wk[:]), reads=[bw], writes=[bi])
            c.op(c.DVE, lambda: nc.vector.tensor_copy(wk[:], wi[:]), reads=[bi], writes=[bw])
            c.op(c.DVE, lambda: nc.vector.scalar_tensor_tensor(r[:], wk[:], -TWO_PI, ang[:], ALU.mult, ALU.add),
                 reads=[bw, ba], writes=[br])
            c.op(c.DVE, lambda: nc.vector.tensor_scalar(wk[:], r[:], PI_F, -TWO_PI, ALU.is_gt, ALU.mult), reads=[br], writes=[bw])
            c.op(c.DVE, lambda: nc.vector.tensor_tensor(r[:], r[:], wk[:], ALU.add), reads=[bw, br], writes=[br])
            c.op(c.DVE, lambda: nc.vector.tensor_scalar(wk[:], r[:], -PI_F, TWO_PI, ALU.is_lt, ALU.mult), reads=[br], writes=[bw])
            c.op(c.DVE, lambda: nc.vector.tensor_tensor(r[:], r[:], wk[:], ALU.add), reads=[bw, br], writes=[br])
            c.op(c.DVE, lambda: nc.vector.tensor_scalar(r[:], r[:], 3.1415925, -3.1415925, ALU.min, ALU.max), reads=[br], writes=[br])
            if which == 0:
                c.op(c.ACT, lambda: nc.scalar.activation(out=sinT[:, sl], in_=r[:], func=AF.Sin, scale=sgn[:, 0:1]),
                     reads=[br, b_tab], writes=[b_tab])
            else:
                c.op(c.ACT, lambda: nc.scalar.activation(out=cosT[:, sl], in_=r[:], func=AF.Sin), reads=[br], writes=[b_tab])
    c.barrier()
    c.release(m)


def build_masks(c, m_own, m_prev, G, prev_strict, b_m):
    nc = c.nc
    m = c.mark()
    idx = c.sb("mk_idx", [128, 128], F32)
    b = Buf()
    c.op(c.POOL, lambda: nc.gpsimd.iota(idx[:], [[1, 128]], base=0, channel_multiplier=-1,
                                        allow_small_or_imprecise_dtypes=True), writes=[b])
    for g in range(G):
        c.op(c.DVE, lambda: nc.vector.tensor_scalar(m_own[:, g * 128:(g + 1) * 128], idx[:], 0.0, NEG, ALU.is_lt, ALU.mult),
             reads=[b], writes=[b_m])
        c.op(c.DVE, lambda: nc.vector.tensor_scalar(m_prev[:, g * 128:(g + 1) * 128], idx[:], 0.0, NEG,
                                                    ALU.is_ge if prev_strict else ALU.is_gt, ALU.mult),
             reads=[b], writes=[b_m])
    c.barrier()
    c.release(m)


def rope_evac(c, psq, psqs, bq, bqs, cos_sl, sin_sl, b_tab, t1, t2, bt1, bt2, out_ap, b_out):
    nc = c.nc
    c.op(c.DVE, lambda: nc.vector.tensor_tensor(t1, psq, cos_sl, ALU.mult), reads=[bq, b_tab], writes=[bt1])
    c.op(c.DVE, lambda: nc.vector.tensor_tensor(t2, psqs, sin_sl, ALU.mult), reads=[bqs, b_tab], writes=[bt2])
    c.op(c.DVE, lambda: nc.vector.tensor_tensor(out_ap, t1, t2, ALU.add), reads=[bt1, bt2], writes=[b_out])


def mixer0_phase(c, x_in, x_out, pos_d, w_in_d, w_out_d, poolw_d, pscale_d, sinks_d, lnw_d, lnb_d,
                 invf_d, sgn_d, wcol_d, ident, idx, iot, seq=S, after_weights=None, rope=None):
    nc = c.nc
    m0 = c.mark()
    NTL = seq // 512
    NBK = seq // 128
    WIN = 2560
    OU, OQ, OQS, OK_, OKS, OV = 0, 256, 1024, 1792, 2048, 2304
    ps, bps = c.psum, c.b_psum
    gcount = [0]

    def gb():
        k = gcount[0] % 4
        gcount[0] += 1
        return k

    w_in = c.sb("a_win", [128, KC, WIN], BF16)
    w_out = c.sb("a_wout", [128, KC, D], BF16)
    cosT, sinT, b_tab = rope
    KT = c.sb("a_KT", [128, 2, seq], BF16)
    Vs = c.sb("a_V", [128, NBK, 4, 65], BF16)
    invf = c.sb("a_invf", [128, 1], F32)
    sgn = c.sb("a_sgn", [128, 1], F32)
    wcol = c.sb("a_wcol", [128, 2], F32)
    rcw = c.sb("a_rcw", [128, 2], F32)
    Wf = c.sb("a_wf", [128, 2, 128], F32)
    SC = c.sb("a_sc", [128, 256], F32)
    Wbd = c.sb("a_wbd", [128, 2, 128], BF16)
    esink = c.sb("a_esink", [128, 12], F32)
    lnw = c.sb("a_lnw", [128, D], F32)
    lnb = c.sb("a_lnb", [128, D], F32)
    xres = [c.sb(f"a_xres{i}", [128, D], F32) for i in range(3)]
    b_xres = [Buf(), Buf(), Buf()]
    RC0 = xres[1][:].rearrange("p (j n) -> p j n", j=2)
    m_own = c.sb("a_mown", [128, 384], BF16)
    m_prev = c.sb("a_mprev", [128, 384], BF16)
    b_w, b_const, b_m = Buf(), Buf(), Buf()
    d_c = c.dsem("a_dc")
    d_w = [c.dsem(f"a_dw{i}") for i in range(3)]
    wv = w_in_d.rearrange("(k p) n -> p k n", p=128)
    c.dma(c.POOL, d_w[0], lambda: nc.gpsimd.dma_start(out=w_in[:, :, 0:1280], in_=wv[:, :, 0:1280]), writes=[b_w])
    c.dma(c.POOL, d_w[1], lambda: nc.gpsimd.dma_start(out=w_in[:, :, 1280:2560], in_=wv[:, :, 1280:2560]), writes=[b_w])
    c.dma(c.POOL, d_w[2], lambda: nc.gpsimd.dma_start(out=w_out[:], in_=w_out_d.rearrange("(k p) n -> p k n", p=128)), writes=[b_w])
    b_w.w = None
    if after_weights is not None:
        after_weights()
    w_events = [(d_w[0], 16), (d_w[1], 16), (d_w[2], 16)]
    for (dst, src) in ((invf, invf_d), (sgn, sgn_d)):
        c.dma(c.SP, d_c, lambda: nc.sync.dma_start(out=dst[:], in_=src), writes=[b_tab])
    c.dma(c.SP, d_c, lambda: nc.sync.dma_start(out=wcol[:], in_=wcol_d), writes=[b_const])
    c.dma(c.SP, d_c, lambda: nc.sync.dma_start(out=lnw[:], in_=lnw_d.partition_broadcast(128)), writes=[b_const])
    c.dma(c.SP, d_c, lambda: nc.sync.dma_start(out=lnb[:], in_=lnb_d.partition_broadcast(128)), writes=[b_const])
    c.dma(c.SP, d_c, lambda: nc.sync.dma_start(out=SC[:], in_=pscale_d.partition_broadcast(128)), writes=[b_const])
    c.dma(c.SP, d_c, lambda: nc.sync.dma_start(out=esink[:], in_=sinks_d.partition_broadcast(128)), writes=[b_const])
    c.op(c.DVE, lambda: nc.vector.memset(Wf[:], 0.0), writes=[b_const])
    for g in range(4):
        hf, j = g % 2, g // 2
        c.dma(c.SP, d_c, lambda: nc.sync.dma_start(out=Wf[hf * 64:(hf + 1) * 64, j, hf * 64:(hf + 1) * 64], in_=poolw_d[g]),
              writes=[b_const])
    c.op(c.DVE, lambda: nc.vector.tensor_tensor(Wbd[:], Wf[:], SC[:].rearrange("p (j n) -> p j n", j=2), ALU.mult),
         reads=[b_const], writes=[b_const])
    c.op(c.ACT, lambda: nc.scalar.activation(out=esink[:], in_=esink[:], func=AF.Exp), reads=[b_const], writes=[b_const])
    c.op(c.DVE, lambda: nc.vector.reciprocal(rcw[:], wcol[:]), reads=[b_const], writes=[b_const])
    for j in range(2):
        c.op(c.DVE, lambda: nc.vector.tensor_scalar(RC0[:, j, :], iot[:], 1.0, wcol[:, j:j + 1], ALU.add, ALU.min),
             reads=[b_const], writes=[b_const, b_xres[1]])
    c.op(c.DVE, lambda: nc.vector.reciprocal(RC0, RC0), reads=[b_const], writes=[b_const, b_xres[1]])
    c.barrier()
    build_rope(c, pos_d, invf, sgn, cosT, sinT, b_tab, seq=seq)
    for g in range(3):
        c.op(c.DVE, lambda: nc.vector.tensor_scalar(m_own[:, g * 128:(g + 1) * 128], idx[:], 0.0, NEG, ALU.is_lt, ALU.mult),
             writes=[b_m])
        c.op(c.DVE, lambda: nc.vector.tensor_scalar(m_prev[:, g * 128:(g + 1) * 128], idx[:], 0.0, NEG, ALU.is_ge, ALU.mult),
             writes=[b_m])
    c.barrier()

    xin = [c.sb(f"a_xin{i}", [128, D], F32) for i in range(2)]
    xb = [c.sb(f"a_xb{i}", [128, D], BF16) for i in range(4)]
    xT = c.sb("a_xT", [128, KC, 512], BF16)
    QT = c.sb("a_QT", [128, 6, 512], BF16)
    U = c.sb("a_U", [128, 2, 528], F32)
    T1 = c.sb("a_T1", [128, 528], F32)
    T2 = c.sb("a_T2", [128, 528], F32)
    dT = c.sb("a_dT", [128, 2, 512], BF16)
    t1 = [c.sb("a_t1", [128, 512], F32)] * 2
    t2 = [c.sb("a_t2", [128, 512], F32)] * 2
    PT = [[c.sb(f"a_PT{i}_{k}", [128, 384], BF16) for k in range(8)] for i in range(1)]
    PT = [PT[0], PT[0]]
    mix = [c.sb(f"a_mix{i}", [128, D], BF16) for i in range(2)]
    mixT = [c.sb(f"a_mixT{i}", [128, KC, 128], BF16) for i in range(2)]
    den = c.sb("a_den", [128, 24], F32)
    T3 = t1[0][:]
    tmp = None
    b_xin, b_xb = [Buf(), Buf()], [Buf() for _ in range(4)]
    b_xT = [Buf() for _ in range(4)]
    b_QT = [Buf() for _ in range(6)]
    b_KT = [[Buf() for _ in range(NTL)] for _ in range(2)]
    b_V = [Buf() for _ in range(NBK)]
    b_U, b_T1, b_T2 = [Buf(), Buf()], Buf(), Buf()
    b_dT = [Buf(), Buf()]
    b_t1, b_t2 = [Buf()] * 2, [Buf()] * 2
    b_PT = [[Buf() for _ in range(8)]]
    b_PT = [b_PT[0], b_PT[0]]
    b_mix, b_mixT = [Buf(), Buf()], [Buf(), Buf()]
    b_den, b_tmp, b_small = Buf(), Buf(), Buf()
    b_T3 = b_t1[0]
    d_xin = [c.dsem(f"a_dxin{i}") for i in range(2)]
    d_xres = [c.dsem(f"a_dxres{i}") for i in range(3)]
    d_ot = [c.dsem(f"a_dot{i}") for i in range(3)]
    lnp = LNPipe(c, lnw[:], lnb[:], "a_ln")

    c.op(c.DVE, lambda: nc.vector.memset(U[:], 0.0), writes=b_U)
    c.op(c.DVE, lambda: nc.vector.memset(Vs[:], 1.0), writes=b_V)
    for E_ in (c.PE,):
        c._wait(E_, w_events)
    rcount = [0]

    def fm_group(col0, tsl, out_bank):
        def f():
            ins = None
            for kc in range(KC):
                ins = _mm(c, ps[out_bank][:], w_in[:, kc, col0:col0 + 128], xT[:, kc, :], kc == 0, kc == KC - 1)
            return ins
        c.op(c.PE, f, reads=b_xT, writes=[bps[out_bank]])

    def prefetch_x(t):
        for sub in range(4):
            i = sub % 2
            r0 = t * 512 + sub * 128
            c.dma(c.SP, d_xin[i], lambda: nc.sync.dma_start(out=xin[i][:], in_=x_in[r0:r0 + 128, :]), writes=[b_xin[i]])
            c.op(c.POOL, lambda: nc.gpsimd.tensor_copy(xb[sub][:], xin[i][:]), reads=[b_xin[i]], writes=[b_xb[sub]])

    def transpose_x(t):
        for sub in range(4):
            bk = gb()
            pT = ps[bk][:].bitcast(BF16)

            def tr():
                ins = None
                for kc in range(KC):
                    ins = nc.tensor.transpose(pT[:, kc * 128:(kc + 1) * 128], xb[sub][:, kc * 128:(kc + 1) * 128], ident[:])
                return ins
            c.op(c.PE, tr, reads=[b_xb[sub]], writes=[bps[bk]])
            c.op(c.ACT, lambda: nc.scalar.copy(out=xT[:, :, sub * 128:(sub + 1) * 128],
                                               in_=pT.rearrange("p (k t) -> p k t", k=KC)),
                 reads=[bps[bk]], writes=[b_xT[sub]])

    pending = [None]
    prefetch_x(0)
    transpose_x(0)
    for t in range(NTL):
        tsl = slice(t * 512, (t + 1) * 512)
        if t + 1 < NTL:
            prefetch_x(t + 1)
        if t > 0:
            c.op(c.DVE, lambda: nc.vector.tensor_copy(U[:, :, 0:16], U[:, :, 512:528]), reads=b_U, writes=b_U)
        for j in range(2):
            bk = gb()
            fm_group(OU + j * 128, tsl, bk)
            c.op(c.ACT, lambda: nc.scalar.copy(out=U[:, j, 16:528], in_=ps[bk][:]), reads=[bps[bk]], writes=[b_U[j]])
        for j in range(2):
            A = U[:, j, :]
            c.op(c.DVE, lambda: nc.vector.tensor_tensor(T1[:, 1:528], A[:, 1:528], A[:, 0:527], ALU.add),
                 reads=[b_U[j]], writes=[b_T1])
            c.op(c.DVE, lambda: nc.vector.tensor_tensor(T2[:, 3:528], T1[:, 3:528], T1[:, 1:526], ALU.add),
                 reads=[b_T1], writes=[b_T2])
            if j == 0:
                srcs = [(T1, b_T1, 0, 64), (T2, b_T2, 64, 128)]
            else:
                c.op(c.DVE, lambda: nc.vector.tensor_tensor(T1[:, 7:528], T2[:, 7:528], T2[:, 3:524], ALU.add),
                     reads=[b_T2], writes=[b_T1])
                c.op(c.DVE, lambda: nc.vector.tensor_tensor(T2[:, 15:528], T1[:, 15:528], T1[:, 7:520], ALU.add),
                     reads=[b_T1], writes=[b_T2])
                srcs = [(T1, b_T1, 0, 64), (T2, b_T2, 64, 128)]
            for (Sw, bS, p0, p1) in srcs:
                if t == 0:
                    c.op(c.DVE, lambda: nc.vector.tensor_tensor(T3[p0:p1, :], Sw[p0:p1, 16:528], RC0[p0:p1, j, :], ALU.mult),
                         reads=[bS, b_const, b_xres[1]], writes=[b_T3])
                    c.op(c.DVE, lambda: nc.vector.tensor_tensor(dT[p0:p1, j, :], T3[p0:p1, :], A[p0:p1, 16:528], ALU.subtract),
                         reads=[b_T3, b_U[j]], writes=[b_dT[j]])
                else:
                    c.op(c.DVE, lambda: nc.vector.scalar_tensor_tensor(dT[p0:p1, j, :], Sw[p0:p1, 16:528], rcw[p0:p1, j:j + 1],
                                                                       A[p0:p1, 16:528], ALU.mult, ALU.subtract),
                         reads=[bS, b_U[j], b_const], writes=[b_dT[j]])
        for j in range(8):
            ba, bb = gb(), gb()
            if j < 6:
                fm_group(OQ + j * 128, tsl, ba)
                fm_group(OQS + j * 128, tsl, bb)
                out_ap, bo = QT[:, j, :], b_QT[j]
            else:
                fm_group(OK_ + (j - 6) * 128, tsl, ba)
                fm_group(OKS + (j - 6) * 128, tsl, bb)
                out_ap, bo = KT[:, j - 6, tsl], b_KT[j - 6][t]
            k = rcount[0] % 2
            rcount[0] += 1
            rope_evac(c, ps[ba][:], ps[bb][:], bps[ba], bps[bb], cosT[:, tsl], sinT[:, tsl], b_tab,
                      t1[k][:], t2[k][:], b_t1[k], b_t2[k], out_ap, bo)
        for sub in range(4):
            blk = t * 4 + sub
            bk = gb()

            def fv():
                ins = None
                for kc in range(KC):
                    ins = _mm(c, ps[bk][:, 0:256], xT[:, kc, sub * 128:(sub + 1) * 128], w_in[:, kc, OV:OV + 256],
                              kc == 0, kc == KC - 1)
                return ins
            c.op(c.PE, fv, reads=[b_xT[sub]], writes=[bps[bk]])
            c.op(c.ACT, lambda: nc.scalar.copy(out=Vs[:, blk, :, 0:64],
                                               in_=ps[bk][:, 0:256].rearrange("p (h d) -> p h d", h=4)),
                 reads=[bps[bk]], writes=[b_V[blk]])
        if t + 1 < NTL:
            transpose_x(t + 1)
        for sub in range(4):
            bi_ = t * 4 + sub
            mi = bi_ % 2
            bk = gb()

            def fp():
                ins = None
                for j in range(2):
                    ins = _mm(c, ps[bk][:, j * 128:(j + 1) * 128], dT[:, j, sub * 128:(sub + 1) * 128], Wbd[:, j, :], True, True)
                return ins
            c.op(c.PE, fp, reads=b_dT + [b_const], writes=[bps[bk]])
            c.op(c.ACT, lambda: nc.scalar.copy(out=mix[mi][:, 0:256], in_=ps[bk][:, 0:256]), reads=[bps[bk]], writes=[b_mix[mi]])
            kbs = [bi_ - 1, bi_] if bi_ > 0 else [bi_]
            for kh in range(4):
                kt, ph = kh // 2, kh % 2
                psl = slice(ph * 64, (ph + 1) * 64)
                for kbi, kb in enumerate(kbs):
                    own = (kb == bi_)
                    sb_ = 4 + (kh * 2 + kbi) % 2
                    pidx = kh * 2 + kbi

                    def fs():
                        _mm(c, ps[sb_][:, 0:384], KT[psl, kt, kb * 128:(kb + 1) * 128],
                            QT[psl, 3 * kt:3 * kt + 3, sub * 128:(sub + 1) * 128], True, False)
                        return _mm(c, ps[sb_][:, 0:384], ident[:], (m_own if own else m_prev)[:], False, True)
                    c.op(c.PE, fs, reads=[b_KT[kt][kb // 4], b_m] + b_QT[3 * kt:3 * kt + 3], writes=[bps[sb_]])
                    c.op(c.ACT, lambda: nc.scalar.activation(out=PT[mi][pidx][:], in_=ps[sb_][:, 0:384], func=AF.Exp, scale=0.125),
                         reads=[bps[sb_]], writes=[b_PT[mi][pidx]])
            for hb in range(2):
                ob = 6 + hb

                def fo():
                    ins = None
                    for hh in range(6):
                        h = hb * 6 + hh
                        kh, i3 = h // 3, h % 3
                        for kbi, kb in enumerate(kbs):
                            ins = _mm(c, ps[ob][:, hh * 65:(hh + 1) * 65], PT[mi][kh * 2 + kbi][:, i3 * 128:(i3 + 1) * 128],
                                      Vs[:, kb, kh, :], kbi == 0, kbi == len(kbs) - 1)
                    return ins
                c.op(c.PE, fo, reads=b_PT[mi] + [b_V[kb] for kb in kbs], writes=[bps[ob]])
                O3 = ps[ob][:, 0:390].rearrange("p (h c) -> p h c", c=65)
                c.op(c.DVE, lambda: nc.vector.tensor_tensor(den[:, hb * 6:(hb + 1) * 6].rearrange("p (h o) -> p h o", o=1),
                                                            O3[:, :, 64:65],
                                                            esink[:, hb * 6:(hb + 1) * 6].rearrange("p (h o) -> p h o", o=1), ALU.add),
                     reads=[bps[ob], b_const], writes=[b_den])
                c.op(c.DVE, lambda: nc.vector.reciprocal(den[:, 12 + hb * 6:12 + (hb + 1) * 6], den[:, hb * 6:(hb + 1) * 6]),
                     reads=[b_den], writes=[b_den])
                rb = den[:, 12 + hb * 6:12 + (hb + 1) * 6].unsqueeze(2).to_broadcast([128, 6, 64])
                c.op(c.DVE, lambda: nc.vector.tensor_tensor(
                    mix[mi][:, 256 + hb * 384:256 + (hb + 1) * 384].rearrange("p (h d) -> p h d", h=6),
                    O3[:, :, 0:64], rb, ALU.mult), reads=[bps[ob], b_den], writes=[b_mix[mi]])
            bk = gb()
            pT = ps[bk][:].bitcast(BF16)

            def trm():
                ins = None
                for kc in range(KC):
                    ins = nc.tensor.transpose(pT[:, kc * 128:(kc + 1) * 128], mix[mi][:, kc * 128:(kc + 1) * 128], ident[:])
                return ins
            c.op(c.PE, trm, reads=[b_mix[mi]], writes=[bps[bk]])
            c.op(c.ACT, lambda: nc.scalar.copy(out=mixT[mi][:], in_=pT.rearrange("p (k t) -> p k t", k=KC)),
                 reads=[bps[bk]], writes=[b_mixT[mi]])
            r0 = bi_ * 128
            x3i = bi_ % 3
            lnp.step()
            c.dma(c.SP, d_xres[x3i], lambda: nc.sync.dma_start(out=xres[x3i][:], in_=x_in[r0:r0 + 128, :]), writes=[b_xres[x3i]])
            bos = []
            for dh in range(2):
                bo = gb()
                bos.append(bo)

                def fproj():
                    ins = None
                    for kc in range(KC):
                        ins = _mm(c, ps[bo][:], mixT[mi][:, kc, :], w_out[:, kc, dh * 512:(dh + 1) * 512], kc == 0, kc == KC - 1)
                    return ins
                c.op(c.PE, fproj, reads=[b_mixT[mi]], writes=[bps[bo]])
            for dh in range(2):
                bo = bos[dh]
                c.op(c.DVE, lambda: nc.vector.scalar_tensor_tensor(xres[x3i][:, dh * 512:(dh + 1) * 512], xres[x3i][:, dh * 512:(dh + 1) * 512],
                                                                   ALPHA, ps[bo][:], ALU.mult, ALU.add),
                     reads=[bps[bo], b_xres[x3i]], writes=[b_xres[x3i]])

            def fin(Q, q, x3i=x3i, r0=r0):
                c.dma(Q, d_ot[x3i], lambda: q.dma_start(out=x_out[r0:r0 + 128, :], in_=xres[x3i][:]), reads=[b_xres[x3i]])
            lnp.push(xres[x3i][:], xres[x3i][:], b_xres[x3i], b_xres[x3i], fin)
    lnp.flush()
    c.barrier()
    c.release(m0)


def make_ctx(nc):
    c = Ctx(nc)
    c.psum = [nc.alloc_psum_tensor(f"ps{i}", [128, 512], F32) for i in range(8)]
    c.b_psum = [Buf() for _ in range(8)]
    return c


def swap_half(cols):
    cols = np.asarray(cols).reshape(-1, 2, 32)
    return cols[:, ::-1, :].reshape(-1)


def even_perm():
    u = np.arange(0, 256)
    qb, kb, vb = 256, 1024, 1280
    qt = []
    for j in range(6):
        for hf in range(2):
            h = 3 * (2 * (j // 3) + hf) + (j % 3)
            qt.append(qb + h * 64 + np.arange(64))
    qt = np.concatenate(qt)
    kt = kb + np.arange(256)
    v = vb + np.arange(256)
    return np.concatenate([u, qt, swap_half(qt), kt, swap_half(kt), v])


def rope_consts():
    half = 32
    inv = (np.float32(10000.0) ** (-np.arange(half, dtype=np.float32) / np.float32(half))).astype(np.float32)
    p = np.arange(128)
    invf = inv[p % 32].reshape(128, 1).astype(np.float32)
    sgn = np.where((p % 64) < 32, -1.0, 1.0).astype(np.float32).reshape(128, 1)
    wcol = np.where(p[:, None] < 64, np.array([[2.0, 8.0]]), np.array([[4.0, 16.0]])).astype(np.float32)
    return invf, sgn, wcol


ODD_G = 1408
ODD_CONV0 = 3 * ODD_G
ODD_W = ODD_CONV0 + 1536
DILS = (1, 4, 16)


def odd_perm():
    cols = []
    for g in range(3):
        base = g * 768
        qt = []
        for j in range(4):
            for hf in range(2):
                h = j + 4 * hf
                qt.append(base + h * 64 + np.arange(64))
        qt = np.concatenate(qt)
        kt = base + 512 + np.arange(128)
        v = base + 640 + np.arange(128)
        cols += [qt, swap_half(qt), kt, swap_half(kt), v]
    cols.append(2304 + np.arange(1536))
    return np.concatenate(cols)


def mixer1_phase(c, x_in, x_out, pos_d, w_in_bf, w_out_bf, convw_d, lnw_d, lnb_d, invf_d, sgn_d,
                 obuf, dT_d, ident, idx, wready, seq=S, rope=None, hooks=None, zero_fill=None):
    nc = c.nc
    m0 = c.mark()
    NTL = seq // 512
    NBK = seq // 128
    ps, bps = c.psum, c.b_psum
    gcount = [0]
    nrot = [4]

    def gb():
        k = gcount[0] % nrot[0]
        gcount[0] += 1
        return k

    build_tab = rope is None
    if build_tab:
        cosT = c.sb("c_cos", [128, seq], F32)
        sinT = c.sb("c_sin", [128, seq], F32)
        b_tab = Buf()
    else:
        cosT, sinT, b_tab = rope
    invf = c.sb("c_invf", [128, 1], F32)
    sgn = c.sb("c_sgn", [128, 1], F32)
    cw = c.sb("c_cw", [128, 4, 3], F32)
    lnw = c.sb("c_lnw", [128, D], F32)
    lnb = c.sb("c_lnb", [128, D], F32)
    if zero_fill is not None:
        zt = c.sb("c_zero", [128, 1, D], BF16)
        b_zt, d_zt = Buf(), c.dsem("c_dzero")
        c.op(c.DVE, lambda: nc.vector.memset(zt[:], 0.0), writes=[b_zt])
    m_own = c.sb("c_mown", [128, 512], BF16)
    m_prev = c.sb("c_mprev", [128, 512], BF16)
    xin = [c.sb(f"c_xin{i}", [128, D], F32) for i in range(2)]
    xb = [c.sb(f"c_xb{i}", [128, D], BF16) for i in range(4)]
    xT = c.sb("c_xT", [128, KC, 512], BF16)
    t1 = [c.sb(f"c_t1{i}", [128, 512], F32) for i in range(2)]
    t2 = [c.sb(f"c_t2{i}", [128, 512], F32) for i in range(2)]
    b_const, b_m = Buf(), Buf()
    b_xin, b_xb = [Buf(), Buf()], [Buf() for _ in range(4)]
    b_xT = [Buf() for _ in range(4)]
    b_t1, b_t2 = [Buf(), Buf()], [Buf(), Buf()]
    d_c = c.dsem("c_dc")
    d_xin = [c.dsem(f"c_dxin{i}") for i in range(2)]
    d_w = c.dsem("c_dw")
    for (dst, src) in ((invf, invf_d), (sgn, sgn_d)):
        c.dma(c.SP, d_c, lambda: nc.sync.dma_start(out=dst[:], in_=src), writes=[b_tab])
    c.dma(c.SP, d_c, lambda: nc.sync.dma_start(out=cw[:], in_=convw_d), writes=[b_const])
    c.dma(c.SP, d_c, lambda: nc.sync.dma_start(out=lnw[:], in_=lnw_d.partition_broadcast(128)), writes=[b_const])
    c.dma(c.SP, d_c, lambda: nc.sync.dma_start(out=lnb[:], in_=lnb_d.partition_broadcast(128)), writes=[b_const])
    for g in range(4):
        c.op(c.DVE, lambda: nc.vector.tensor_scalar(m_own[:, g * 128:(g + 1) * 128], idx[:], 0.0, NEG, ALU.is_lt, ALU.mult),
             writes=[b_m])
        c.op(c.DVE, lambda: nc.vector.tensor_scalar(m_prev[:, g * 128:(g + 1) * 128], idx[:], 0.0, NEG, ALU.is_gt, ALU.mult),
             writes=[b_m])
    c.barrier()
    if build_tab:
        build_rope(c, pos_d, invf, sgn, cosT, sinT, b_tab, seq=seq)
    if wready:
        c._wait(c.SP, wready)
    wv = w_in_bf.rearrange("(k p) n -> p k n", p=128)

    def prefetch_x(t):
        for sub in range(4):
            i = sub % 2
            r0 = t * 512 + sub * 128
            c.dma(c.SP, d_xin[i], lambda: nc.sync.dma_start(out=xin[i][:], in_=x_in[r0:r0 + 128, :]), writes=[b_xin[i]])
            c.op(c.ACT, lambda: nc.scalar.copy(out=xb[sub][:], in_=xin[i][:]), reads=[b_xin[i]], writes=[b_xb[sub]])

    def transpose_x(t):
        for sub in range(4):
            bk = gb()
            pT = ps[bk][:].bitcast(BF16)

            def tr():
                ins = None
                for kc in range(KC):
                    ins = nc.tensor.transpose(pT[:, kc * 128:(kc + 1) * 128], xb[sub][:, kc * 128:(kc + 1) * 128], ident[:])
                return ins
            c.op(c.PE, tr, reads=[b_xb[sub]], writes=[bps[bk]])
            c.op(c.ACT, lambda: nc.scalar.copy(out=xT[:, :, sub * 128:(sub + 1) * 128],
                                               in_=pT.rearrange("p (k t) -> p k t", k=KC)),
                 reads=[bps[bk]], writes=[b_xT[sub]])

    def fm_group(w, b_wt, col0, out_bank):
        def f():
            ins = None
            for kc in range(KC):
                ins = _mm(c, ps[out_bank][:], w[:, kc, col0:col0 + 128], xT[:, kc, :], kc == 0, kc == KC - 1)
            return ins
        c.op(c.PE, f, reads=b_xT + [b_wt], writes=[bps[out_bank]])

    m1 = c.mark()
    nrot[0] = 8
    if hooks:
        hooks[0]()
    wc = c.sb("c_wc", [128, KC, 1536], BF16)
    Z = c.sb("c_Z", [128, 4, 514], F32)
    hd = [c.sb(f"c_hd{i}", [128, 512], F32) for i in range(2)]
    acc = [c.sb(f"c_acc{i}", [128, 512], F32) for i in range(2)]
    dts = [c.sb(f"c_dts{i}", [128, 4, 512], BF16) for i in range(2)]
    b_wc, b_Z = Buf(), [Buf() for _ in range(4)]
    b_hd, b_acc, b_dts = [Buf(), Buf()], [Buf(), Buf()], [Buf(), Buf()]
    d_dts = [c.dsem(f"c_ddts{i}") for i in range(2)]
    c.dma(c.SP, d_w, lambda: nc.sync.dma_start(out=wc[:], in_=wv[:, :, ODD_CONV0:ODD_CONV0 + 1536]), writes=[b_wc])
    c.op(c.DVE, lambda: nc.vector.memset(Z[:], 0.0), writes=b_Z)
    kk = 0
    prefetch_x(0)
    transpose_x(0)
    for t in range(NTL):
        prefetch_x((t + 1) % NTL)
        di = t % 2
        for ch in range(4):
            k = kk % 2
            kk += 1
            ba, bb, bc = gb(), gb(), gb()
            fm_group(wc, b_wc, 0 + ch * 128, ba)
            fm_group(wc, b_wc, 1024 + ch * 128, bb)
            fm_group(wc, b_wc, 512 + ch * 128, bc)
            if t > 0:
                c.op(c.DVE, lambda: nc.vector.tensor_copy(Z[:, ch, 0:2], Z[:, ch, 512:514]), reads=[b_Z[ch]], writes=[b_Z[ch]])
            c.op(c.ACT, lambda: nc.scalar.copy(out=hd[k][:], in_=ps[ba][:]), reads=[bps[ba]], writes=[b_hd[k]])
            c.op(c.DVE, lambda: nc.vector.tensor_tensor(Z[:, ch, 2:514], hd[k][:], ps[bb][:], ALU.mult),
                 reads=[b_hd[k], bps[bb]], writes=[b_Z[ch]])
            c.op(c.DVE, lambda: nc.vector.tensor_scalar(acc[k][:], Z[:, ch, 0:512], cw[:, ch, 0:1], None, ALU.mult),
                 reads=[b_Z[ch], b_const], writes=[b_acc[k]])
            c.op(c.DVE, lambda: nc.vector.scalar_tensor_tensor(acc[k][:], Z[:, ch, 1:513], cw[:, ch, 1:2], acc[k][:], ALU.mult, ALU.add),
                 reads=[b_Z[ch], b_acc[k]], writes=[b_acc[k]])
            c.op(c.DVE, lambda: nc.vector.scalar_tensor_tensor(acc[k][:], Z[:, ch, 2:514], cw[:, ch, 2:3], acc[k][:], ALU.mult, ALU.add),
                 reads=[b_Z[ch], b_acc[k]], writes=[b_acc[k]])
            c.op(c.DVE, lambda: nc.vector.tensor_tensor(dts[di][:, ch, :], acc[k][:], ps[bc][:], ALU.mult),
                 reads=[b_acc[k], bps[bc]], writes=[b_dts[di]])
        c.dma(c.SP, d_dts[di], lambda: nc.sync.dma_start(out=dT_d[:, :, t * 512:(t + 1) * 512].rearrange("c p t -> p c t"),
                                                          in_=dts[di][:]), reads=[b_dts[di]])
        transpose_x((t + 1) % NTL)
    c.barrier()
    c.release(m1)

    m2 = c.mark()
    nrot[0] = 4
    wg_ = c.sb("c_wg", [128, KC, ODD_G], BF16)
    QT = c.sb("c_QT", [128, 4, seq], BF16)
    KT = c.sb("c_KT", [128, seq], BF16)
    VT = c.sb("c_VT", [128, seq], BF16)
    Vs = c.sb("c_V", [128, NBK, 2, 65], BF16)
    PT = [[c.sb(f"c_PT{i}_{k}", [128, 512], BF16) for k in range(4)] for i in range(2)]
    Osb = [c.sb(f"c_O{i}", [128, 520], F32) for i in range(2)]
    b_wg = Buf()
    b_Q = [[Buf() for _ in range(NTL)] for _ in range(4)]
    b_K = [Buf() for _ in range(NTL)]
    b_VT = [Buf() for _ in range(NTL)]
    b_V = [Buf() for _ in range(NBK)]
    b_PT = [[Buf() for _ in range(4)] for _ in range(2)]
    b_O = [Buf(), Buf()]
    d_O = [c.dsem(f"c_dO{i}") for i in range(2)]
    c.op(c.DVE, lambda: nc.vector.memset(Vs[:], 1.0), writes=b_V)
    rcount = 0
    for g in range(3):
        if hooks:
            hooks[1 + g]()
        if zero_fill is not None and g == 0:
            zv = zero_fill.rearrange("(n p) d -> p n d", p=128)
            NZ = zv.shape[1]
            for z0 in range(0, NZ, 23):
                z1 = min(NZ, z0 + 23)
                c.dma(c.POOL, d_zt, lambda: nc.gpsimd.dma_start(out=zv[:, z0:z1, :], in_=zt[:].to_broadcast([128, z1 - z0, D])), reads=[b_zt])
        dil = DILS[g]
        SD = seq // dil
        NPT = 512 // dil
        c.dma(c.SP, d_w, lambda: nc.sync.dma_start(out=wg_[:], in_=wv[:, :, g * ODD_G:(g + 1) * ODD_G]), writes=[b_wg])

        def perm_out(buf_ap, t):
            return buf_ap.rearrange("p (r n) -> p r n", r=dil)[:, :, t * NPT:(t + 1) * NPT]

        def perm_in(ap):
            return ap.rearrange("p (n r) -> p r n", r=dil)

        for t in range(NTL):
            tsl = slice(t * 512, (t + 1) * 512)
            if not (g == 2 and t == NTL - 1):
                prefetch_x((t + 1) % NTL)
            for j in range(6):
                k = rcount % 2
                rcount += 1
                if j < 5:
                    ba, bb = gb(), gb()
                    c0 = j * 128 if j < 4 else 1024
                    fm_group(wg_, b_wg, c0, ba)
                    fm_group(wg_, b_wg, c0 + (512 if j < 4 else 128), bb)
                    dst = perm_out(QT[:, j, :], t) if j < 4 else perm_out(KT[:], t)
                    bo = b_Q[j][t] if j < 4 else b_K[t]
                    c.op(c.DVE, lambda: nc.vector.tensor_tensor(t1[k][:], ps[ba][:], cosT[:, tsl], ALU.mult),
                         reads=[bps[ba], b_tab], writes=[b_t1[k]])
                    c.op(c.DVE, lambda: nc.vector.tensor_tensor(t2[k][:], ps[bb][:], sinT[:, tsl], ALU.mult),
                         reads=[bps[bb], b_tab], writes=[b_t2[k]])
                    c.op(c.DVE, lambda: nc.vector.tensor_tensor(dst, perm_in(t1[k][:]), perm_in(t2[k][:]), ALU.add),
                         reads=[b_t1[k], b_t2[k]], writes=[bo])
                else:
                    ba = gb()
                    fm_group(wg_, b_wg, 1280, ba)
                    c.op(c.ACT, lambda: nc.scalar.copy(out=perm_out(VT[:], t), in_=perm_in(ps[ba][:])),
                         reads=[bps[ba]], writes=[b_VT[t]])
            if not (g == 2 and t == NTL - 1):
                transpose_x((t + 1) % NTL)
        for blk in range(NBK):
            bk = gb()
            pT = ps[bk][:].bitcast(BF16)
            c.op(c.PE, lambda: nc.tensor.transpose(pT[:, 0:128], VT[:, blk * 128:(blk + 1) * 128], ident[:]),
                 reads=b_VT, writes=[bps[bk]])
            c.op(c.ACT, lambda: nc.scalar.copy(out=Vs[:, blk, :, 0:64], in_=pT[:, 0:128].rearrange("p (h d) -> p h d", h=2)),
                 reads=[bps[bk]], writes=[b_V[blk]])
        NJ = SD // 128
        for r in range(dil):
            for jb in range(NJ):
                blk = r * NJ + jb
                mi = blk % 2
                kbs = [jb - 1, jb] if jb > 0 else [jb]
                for kh in range(2):
                    psl = slice(kh * 64, (kh + 1) * 64)
                    for kbi, kb in enumerate(kbs):
                        own = (kb == jb)
                        sb_ = 4 + (kh * 2 + kbi) % 2
                        pidx = kh * 2 + kbi
                        kc0 = (r * NJ + kb) * 128

                        def fs():
                            _mm(c, ps[sb_][:], KT[psl, kc0:kc0 + 128], QT[psl, :, blk * 128:(blk + 1) * 128], True, False)
                            return _mm(c, ps[sb_][:], ident[:], (m_own if own else m_prev)[:], False, True)
                        c.op(c.PE, fs, reads=b_K + [b_m] + [b for bl in b_Q for b in bl], writes=[bps[sb_]])
                        c.op(c.ACT, lambda: nc.scalar.activation(out=PT[mi][pidx][:], in_=ps[sb_][:], func=AF.Exp, scale=0.125),
                             reads=[bps[sb_]], writes=[b_PT[mi][pidx]])
                for kh in range(2):
                    ob = 6 + kh

                    def fo():
                        ins = None
                        for i4 in range(4):
                            for kbi, kb in enumerate(kbs):
                                ins = _mm(c, ps[ob][:, i4 * 65:(i4 + 1) * 65], PT[mi][kh * 2 + kbi][:, i4 * 128:(i4 + 1) * 128],
                                          Vs[:, r * NJ + kb, kh, :], kbi == 0, kbi == len(kbs) - 1)
                        return ins
                    c.op(c.PE, fo, reads=b_PT[mi] + [b_V[r * NJ + kb] for kb in kbs], writes=[bps[ob]])
                    c.op(c.ACT, lambda: nc.scalar.copy(out=Osb[mi][:, kh * 260:(kh + 1) * 260], in_=ps[ob][:, 0:260]),
                         reads=[bps[ob]], writes=[b_O[mi]])
                tok0 = jb * 128 * dil + r
                c.dma(c.SP, d_O[mi], lambda: nc.sync.dma_start(out=obuf.rearrange("(n r) g c -> r n g c", r=dil)[r, jb * 128:(jb + 1) * 128, g, :], in_=Osb[mi][:]),
                      reads=[b_O[mi]])
    c.barrier()
    c.release(m2)

    w_out = c.sb("c_wout", [128, KC, D], BF16)
    Ob = [c.sb(f"c_Ob{i}", [128, 3, 520], F32) for i in range(2)]
    dl = [c.sb(f"c_dl{i}", [128, 4, 512], BF16) for i in range(2)]
    cmix = [c.sb(f"c_cmix{i}", [128, 512], BF16) for i in range(2)]
    cT = [c.sb(f"c_cT{i}", [128, 4, 128], BF16) for i in range(2)]
    xres = [c.sb(f"c_xres{i}", [128, D], F32) for i in range(3)]
    ot = [c.sb(f"c_ot{i}", [128, D], F32) for i in range(3)]
    lnp = LNPipe(c, lnw[:], lnb[:], "c_ln", pool_add=True, store="sp_delayed")
    rd = c.sb("c_rd", [128, 8], F32)
    z = c.sb("c_z", [128, D], F32)
    tmp = c.sb("c_tmp", [128, D], F32)
    st = c.sb("c_st", [128, 12], F32)
    mv = c.sb("c_mv", [128, 8], F32)
    b_wo, b_Ob, b_dl, b_cmix, b_cT = Buf(), [Buf(), Buf()], [Buf(), Buf()], [Buf(), Buf()], [Buf(), Buf()]
    b_xres, b_ot, b_rd, b_z, b_tmp, b_small = [Buf(), Buf(), Buf()], [Buf(), Buf(), Buf()], Buf(), Buf(), Buf(), Buf()
    d_Ob = [c.dsem(f"c_dOb{i}") for i in range(2)]
    d_dl = [c.dsem(f"c_ddl{i}") for i in range(2)]
    d_xres = [c.dsem(f"c_dxres{i}") for i in range(3)]
    d_ot = [c.dsem(f"c_dot{i}") for i in range(3)]
    c.dma(c.SP, d_w, lambda: nc.sync.dma_start(out=w_out[:], in_=w_out_bf.rearrange("(k p) n -> p k n", p=128)), writes=[b_wo])
    nrot[0] = 8
    rd2 = [rd, c.sb("c_rd2", [128, 8], F32)]
    b_rd2 = [b_rd, Buf()]
    proj_banks = {}

    def stage1(bi_):
        mi = bi_ % 2
        r0 = bi_ * 128
        t, sub = bi_ // 4, bi_ % 4
        li = t % 2
        rdm, b_rdm = rd2[mi], b_rd2[mi]
        if sub == 0:
            c.dma(c.SP, d_dl[li], lambda: nc.sync.dma_start(out=dl[li][:], in_=dT_d[:, :, t * 512:(t + 1) * 512].rearrange("c p t -> p c t")),
                  writes=[b_dl[li]])
        c.dma(c.SP, d_Ob[mi], lambda: nc.sync.dma_start(out=Ob[mi][:], in_=obuf[r0:r0 + 128, :, :]), writes=[b_Ob[mi]])
        x3i = bi_ % 3
        c.dma(c.SP, d_xres[x3i], lambda: nc.sync.dma_start(out=xres[x3i][:], in_=x_in[r0:r0 + 128, :]), writes=[b_xres[x3i]])
        c.op(c.DVE, lambda: nc.vector.tensor_tensor(Ob[mi][:, 0, :], Ob[mi][:, 0, :], Ob[mi][:, 1, :], ALU.add),
             reads=[b_Ob[mi]], writes=[b_Ob[mi]])
        c.op(c.DVE, lambda: nc.vector.tensor_tensor(Ob[mi][:, 0, :], Ob[mi][:, 0, :], Ob[mi][:, 2, :], ALU.add),
             reads=[b_Ob[mi]], writes=[b_Ob[mi]])
        A3 = Ob[mi][:, 0, :].rearrange("p (h c) -> p h c", c=65)
        c.op(c.DVE, lambda: nc.vector.reciprocal(rdm[:].rearrange("p (h o) -> p h o", o=1), A3[:, :, 64:65]),
             reads=[b_Ob[mi]], writes=[b_rdm])
        c.op(c.DVE, lambda: nc.vector.tensor_tensor(cmix[mi][:].rearrange("p (h d) -> p h d", h=8), A3[:, :, 0:64],
                                                    rdm[:, 0:8].unsqueeze(2).to_broadcast([128, 8, 64]), ALU.mult),
             reads=[b_Ob[mi], b_rdm], writes=[b_cmix[mi]])
        bk = gb()
        pT = ps[bk][:].bitcast(BF16)

        def trm():
            ins = None
            for kc in range(4):
                ins = nc.tensor.transpose(pT[:, kc * 128:(kc + 1) * 128], cmix[mi][:, kc * 128:(kc + 1) * 128], ident[:])
            return ins
        c.op(c.PE, trm, reads=[b_cmix[mi]], writes=[bps[bk]])
        c.op(c.ACT, lambda: nc.scalar.copy(out=cT[mi][:], in_=pT[:, 0:512].rearrange("p (k t) -> p k t", k=4)),
             reads=[bps[bk]], writes=[b_cT[mi]])
        bos = []
        for dh in range(2):
            bo = gb()
            bos.append(bo)

            def fproj():
                ins = None
                for kc in range(KC):
                    lhs = cT[mi][:, kc, :] if kc < 4 else dl[li][:, kc - 4, sub * 128:(sub + 1) * 128]
                    ins = _mm(c, ps[bo][:], lhs, w_out[:, kc, dh * 512:(dh + 1) * 512], kc == 0, kc == KC - 1)
                return ins
            c.op(c.PE, fproj, reads=[b_cT[mi], b_dl[li], b_wo], writes=[bps[bo]])
        proj_banks[bi_] = bos

    def stage2(bi_):
        mi = bi_ % 2
        x3i = bi_ % 3
        r0 = bi_ * 128
        for dh in range(2):
            bo = proj_banks[bi_][dh]
            c.op(c.DVE, lambda: nc.vector.scalar_tensor_tensor(xres[x3i][:, dh * 512:(dh + 1) * 512], xres[x3i][:, dh * 512:(dh + 1) * 512],
                                                               ALPHA, ps[bo][:], ALU.mult, ALU.add),
                 reads=[bps[bo], b_xres[x3i]], writes=[b_xres[x3i]])

        def fin(Q, q):
            c.dma(Q, d_ot[x3i], lambda: q.dma_start(out=x_out[r0:r0 + 128, :], in_=ot[x3i][:]), reads=[b_ot[x3i]])
        lnp.push(xres[x3i][:], ot[x3i][:], b_xres[x3i], b_ot[x3i], fin)

    stage1(0)
    for bi_ in range(NBK):
        if bi_ + 1 < NBK:
            stage1(bi_ + 1)
        stage2(bi_)
        lnp.step()
    lnp.flush()
    c.barrier()
    c.release(m0)


def flat_view(ap, b):
    nd = len(ap.shape)
    names = " ".join(f"d{i}" for i in range(nd))
    f = ap.rearrange(f"{names} -> ({names})")
    return f.rearrange("(p a b) -> p a b", p=128, b=b)


def build_program(seq=S):
    nc = bass.Bass("TRN2", target_bir_lowering=False)
    c = make_ctx(nc)

    def inp(name, shape, dt=F32):
        return nc.dram_tensor(name, list(shape), dt, kind="ExternalInput").ap()

    def scr(name, shape, dt):
        return nc.dram_tensor(name, list(shape), dt).ap()

    x = inp("x", [seq, D])
    pos = inp("pos", [seq], I32)
    ln_w = inp("ln_w", [4, D])
    ln_b = inp("ln_b", [4, D])
    even_w_in = inp("even_w_in", [D, 2560])
    pool_w = inp("pool_w", [4, 64, 64])
    pool_scale = inp("pool_scale", [256])
    sinks = inp("sinks", [12])
    even_w_out = inp("even_w_out", [D, D])
    ffn_g = inp("ffn_w_gate", [1, D, D_FF])
    ffn_u = inp("ffn_w_up", [1, D, D_FF])
    ffn_d = inp("ffn_w_down", [1, D_FF, D])
    odd_w_in = inp("odd_w_in", [D, ODD_W])
    conv_w = inp("conv_w", [128, 4, 3])
    odd_w_out = inp("odd_w_out", [D, D])
    router = inp("router_wT", [N_EXP, D])
    router_kd = inp("router_w", [D, N_EXP])
    moe_g = inp("moe_w_gate", [N_EXP, D, D_FFE])
    moe_u = inp("moe_w_up", [N_EXP, D, D_FFE])
    moe_d = inp("moe_w_down", [N_EXP, D_FFE, D])
    invf = inp("invf", [128, 1])
    sgn = inp("sgn", [128, 1])
    wcol = inp("wcol", [128, 2])
    out = nc.dram_tensor("out", [seq, D], F32, kind="ExternalOutput").ap()

    x1 = scr("x1", [seq, D], F32)
    x2 = scr("x2", [seq, D], F32)
    x3 = scr("x3", [seq, D], F32)
    ffn_g_bf = scr("ffn_g_bf", [1, D, D_FF], BF16)
    ffn_u_bf = scr("ffn_u_bf", [1, D, D_FF], BF16)
    ffn_d_bf = scr("ffn_d_bf", [1, D_FF, D], BF16)
    odd_in_bf = scr("odd_in_bf", [D, ODD_W], BF16)
    odd_out_bf = scr("odd_out_bf", [D, D], BF16)
    NROW = N_EXP * MOE_NG * 128
    wg_s = scr("moe_wg_s", [NROW, 4096], BF16)
    wu_s = scr("moe_wu_s", [NROW, 4096], BF16)
    wd_s = scr("moe_wd_s", [NROW, 4096], BF16)
    NBLK = (2 * seq) // MOE_BLK + N_EXP - 1
    xs_d = scr("moe_xs", [NBLK * MOE_BLK, D], BF16)
    yrow_d = scr("moe_yrow", [NBLK * MOE_BLK, D], F32)
    obuf = scr("obuf", [seq, 3, 520], F32)
    dT_d = scr("dT_d", [4, 128, seq], BF16)

    ident, idx, iot = build_consts(c)
    rope = (c.sb("k_cos", [128, seq], F32), c.sb("k_sin", [128, seq], F32), Buf())
    pc_ffn = Eng(nc, None, "pc_ffn")
    pc_odd = Eng(nc, None, "pc_odd")
    pc_moe = [Eng(nc, None, f"pc_moe{e}") for e in range(N_EXP)]

    def cast(Dm, dst, src, b):
        c.dma(c.POOL, Dm, lambda: nc.gpsimd.dma_start(out=flat_view(dst, b), in_=flat_view(src, b)))

    def precasts():
        cast(pc_ffn, ffn_g_bf, ffn_g, 2048)
        cast(pc_ffn, ffn_u_bf, ffn_u, 2048)
        cast(pc_ffn, ffn_d_bf, ffn_d, 2048)
        cast(pc_odd, odd_in_bf, odd_w_in, 1536)
        cast(pc_odd, odd_out_bf, odd_w_out, 2048)

    def precast_expert(e):
        wg5 = wg_s.rearrange("(e g p) (k n) -> e g p k n", e=N_EXP, g=MOE_NG, k=KC)
        wu5 = wu_s.rearrange("(e g p) (k n) -> e g p k n", e=N_EXP, g=MOE_NG, k=KC)
        wd5 = wd_s.rearrange("(e g p) (c d) -> e g p c d", e=N_EXP, g=MOE_NG, c=4)
        if c.PE.n > 0:
            c._wait(c.POOL, [(c.PE, c.PE.n)])
        if True:
            sg_ = moe_g[e].rearrange("(k p) (g n) -> g p k n", p=128, n=512)
            su_ = moe_u[e].rearrange("(k p) (g n) -> g p k n", p=128, n=512)
            sd_ = moe_d[e].rearrange("(g c p) d -> g p c d", c=4, p=128)
            for g in range(MOE_NG):
                c.dma(c.POOL, pc_moe[e], lambda: nc.gpsimd.dma_start(out=wg5[e, g], in_=sg_[g]))
                c.dma(c.POOL, pc_moe[e], lambda: nc.gpsimd.dma_start(out=wu5[e, g], in_=su_[g]))
                c.dma(c.POOL, pc_moe[e], lambda: nc.gpsimd.dma_start(out=wd5[e, g], in_=sd_[g]))

    mixer0_phase(c, x, x1, pos, even_w_in, even_w_out, pool_w, pool_scale, sinks, ln_w[0], ln_b[0],
                 invf, sgn, wcol, ident, idx, iot, seq=seq, after_weights=precasts, rope=rope)
    ffn_phase(c, x1, x2, ffn_g_bf, ffn_u_bf, ffn_d_bf, 1, D_FF, 2, ln_w[1], ln_b[1], ident, TP=1024, seq=seq,
              wready=[[(pc_ffn, 48)]], pass_hooks=[(lambda e=e: precast_expert(e)) for e in range(4)])
    mixer1_phase(c, x2, x3, pos, odd_in_bf, odd_out_bf, conv_w, ln_w[2], ln_b[2], invf, sgn, obuf, dT_d, ident, idx,
                 wready=[(pc_odd, 32)], seq=seq, rope=rope, hooks=[(lambda e=e: precast_expert(e)) for e in range(4, 8)], zero_fill=xs_d)
    moe_sparse_phase(c, x3, out, wg_s, wu_s, wd_s, router, ln_w[3], ln_b[3], ident, idx, iot, c.gp, xs_d, yrow_d,
                     wready=[(pc_moe[e], 48 * MOE_NG) for e in range(N_EXP)], seq=seq, router_kd=router_kd)
    return nc


_CACHE = {}


def prep_shared(inputs):
    f = lambda a: np.ascontiguousarray(np.asarray(a))
    invf, sgn, wcol = rope_consts()
    conv = np.asarray(inputs["conv_w"])[0]
    conv_l = np.ascontiguousarray(conv.reshape(3, 4, 128).transpose(2, 1, 0))
    return {
        "ln_w": f(np.asarray(inputs["ln_w"]).reshape(4, D)),
        "ln_b": f(np.asarray(inputs["ln_b"]).reshape(4, D)),
        "even_w_in": f(np.asarray(inputs["even_w_in"])[0][:, even_perm()]),
        "pool_w": f(np.asarray(inputs["pool_w"])[0]),
        "pool_scale": f(np.asarray(inputs["pool_scale"])[0]),
        "sinks": f(np.asarray(inputs["swa_sinks"])[0]),
        "even_w_out": f(np.asarray(inputs["even_w_out"])[0]),
        "ffn_w_gate": f(inputs["ffn_w_gate"]),
        "ffn_w_up": f(inputs["ffn_w_up"]),
        "ffn_w_down": f(inputs["ffn_w_down"]),
        "odd_w_in": f(np.asarray(inputs["odd_w_in"])[0][:, odd_perm()]),
        "conv_w": conv_l,
        "odd_w_out": f(np.asarray(inputs["odd_w_out"])[0]),
        "router_wT": f(np.asarray(inputs["router_w"])[0].T),
        "router_w": f(np.asarray(inputs["router_w"])[0]),
        "moe_w_gate": f(np.asarray(inputs["moe_w_gate"])[0]),
        "moe_w_up": f(np.asarray(inputs["moe_w_up"])[0]),
        "moe_w_down": f(np.asarray(inputs["moe_w_down"])[0]),
        "invf": invf, "sgn": sgn, "wcol": wcol,
    }


def kernel(**inputs):
    x = np.asarray(inputs["x"], dtype=np.float32)
    pos = np.asarray(inputs["positions"]).astype(np.int32)
    shared = prep_shared(inputs)
    if "nc" not in _CACHE:
        _CACHE["nc"] = build_program()
    nc = _CACHE["nc"]
    in_maps = []
    for b in range(NB):
        m = dict(shared)
        m["x"] = np.ascontiguousarray(x[b])
        m["pos"] = np.ascontiguousarray(pos[b])
        in_maps.append(m)
    res = run_bass_kernel_spmd(nc, in_maps, core_ids=list(range(NB)))
    return np.stack([np.asarray(r["out"]) for r in res.results], axis=0).astype(np.float32)


U32 = mybir.dt.uint32
MOE_BLK = 512
MOE_NBLK = (2 * S) // MOE_BLK + N_EXP - 1
MOE_NG = D_FFE // 512
IND = bass.IndirectOffsetOnAxis


def moe_sparse_phase(c, x_in, x_out, wg_s, wu_s, wd_s, router_d, lnw_d, lnb_d, ident, idx, iot, gp,
                     xs_d, yrow_d, wready, seq=S, dbg=None, router_kd=None):
    nc = c.nc
    m0 = c.mark()
    NTK = seq // 128
    NBLK = (2 * seq) // MOE_BLK + N_EXP - 1
    ps, bps = c.psum, c.b_psum
    gcount = [0]

    def gb():
        k = gcount[0] % 4
        gcount[0] += 1
        return k

    GG = c.sb("m_GG", [128, NTK, 2], F32)
    DSTu = c.sb("m_DSTu", [128, NTK, 2], U32)
    IWu = c.sb("m_IWu", [128, NBLK * MOE_NG], U32)
    lnw = c.sb("m_lnw", [128, D], F32)
    lnb = c.sb("m_lnb", [128, D], F32)
    b_GG, b_DST, b_IW, b_const = Buf(), Buf(), Buf(), Buf()
    d_c = c.dsem("m_dc")
    c.dma(c.SP, d_c, lambda: nc.sync.dma_start(out=lnw[:], in_=lnw_d.partition_broadcast(128)), writes=[b_const])
    c.dma(c.SP, d_c, lambda: nc.sync.dma_start(out=lnb[:], in_=lnb_d.partition_broadcast(128)), writes=[b_const])

    m1 = c.mark()
    wr32 = c.sb("m_wr32", [128, KC, N_EXP], F32)
    id32 = c.sb("m_id32", [128, 128], F32)
    xT32 = [c.sb(f"m_xT32{i}", [128, KC, 128], F32) for i in range(2)]
    b_xT32 = [Buf(), Buf()]
    XB = c.sb("m_XB", [128, NTK, D], BF16)
    xin = [c.sb(f"m_xin{i}", [128, D], F32) for i in range(2)]
    tmp = c.sb("m_tmp", [128, D], F32)
    LG = c.sb("m_LG", [128, NTK, N_EXP], F32)
    L2 = c.sb("m_L2", [128, NTK, N_EXP], F32)
    CN = c.sb("m_CN", [128, NTK, N_EXP], F32)
    BASE = c.sb("m_BASE", [128, NTK, N_EXP], F32)
    sv = c.sb("m_sv", [128, 5, NTK], F32)
    RK = c.sb("m_RK", [128, NTK, N_EXP], F32)
    E1 = c.sb("m_E1", [128, NTK, N_EXP], F32)
    E2 = c.sb("m_E2", [128, NTK, N_EXP], F32)
    selb = c.sb("m_selb", [128, NTK * N_EXP], BF16)
    tri = c.sb("m_tri", [128, 128], BF16)
    ones = c.sb("m_ones", [128, 128], BF16)
    base = c.sb("m_base", [128, N_EXP], F32)
    sm = c.sb("m_sm", [128, 64], F32)
    smi = c.sb("m_smi", [128, 8], I32)
    DST = c.sb("m_DST", [128, NTK, 2], F32)
    IW = c.sb("m_IW", [128, NBLK, MOE_NG], F32)
    EBf = c.sb("m_EBf", [128, NBLK], F32)
    b_xin, b_XB = [Buf(), Buf()], [Buf() for _ in range(NTK)]
    b_tmp, b_lg, b_sel, b_base, b_sm = Buf(), Buf(), [Buf(), Buf()], Buf(), Buf()
    b_RK, b_E = Buf(), Buf()
    d_xin = [c.dsem(f"m_dxin{i}") for i in range(2)]
    d_sc = c.dsem("m_dsc")
    c.dma(c.SP, d_c, lambda: nc.sync.dma_start(out=wr32[:], in_=router_kd.rearrange("(k p) e -> p k e", p=128)), writes=[b_const])
    c.op(c.DVE, lambda: nc.vector.tensor_scalar(id32[:], idx[:], 0.0, None, ALU.is_equal), writes=[b_const])
    c.op(c.DVE, lambda: nc.vector.tensor_scalar(tri[:], idx[:], 0.0, None, ALU.is_gt), writes=[b_const])
    c.op(c.DVE, lambda: nc.vector.memset(ones[:], 1.0), writes=[b_const])
    c.op(c.DVE, lambda: nc.vector.memset(base[:], 0.0), writes=[b_base])
    c.barrier()
    for s in range(NTK):
        i = s % 2
        t0 = s * 128
        c.dma(c.SP, d_xin[i], lambda: nc.sync.dma_start(out=xin[i][:], in_=x_in[t0:t0 + 128, :]), writes=[b_xin[i]])
        c.op(c.ACT, lambda: nc.scalar.copy(out=XB[:, s, :], in_=xin[i][:]), reads=[b_xin[i]], writes=[b_XB[s]])
        bt0, bt1, bl = 4 + 2 * i, 5 + 2 * i, gb()
        for hfx, bt in enumerate((bt0, bt1)):
            def trx():
                ins = None
                for q in range(4):
                    kc = hfx * 4 + q
                    ins = nc.tensor.transpose(ps[bt][:, q * 128:(q + 1) * 128], xin[i][:, kc * 128:(kc + 1) * 128], id32[:])
                return ins
            c.op(c.PE, trx, reads=[b_xin[i]], writes=[bps[bt]])
            c.op(c.ACT, lambda: nc.scalar.copy(out=xT32[i][:, hfx * 4:(hfx + 1) * 4, :],
                                               in_=ps[bt][:].rearrange("p (k t) -> p k t", k=4)),
                 reads=[bps[bt]], writes=[b_xT32[i]])

        def mlog():
            ins = None
            for kc in range(KC):
                ins = _mm(c, ps[bl][:, 0:8], xT32[i][:, kc, :], wr32[:, kc, :], kc == 0, kc == KC - 1)
            return ins
        c.op(c.PE, mlog, reads=[b_xT32[i]], writes=[bps[bl]])
        c.op(c.DVE, lambda: nc.vector.tensor_copy(LG[:, s, :], ps[bl][:, 0:8]), reads=[bps[bl]], writes=[b_lg])
    T_ = NTK
    R = dict(reads=[b_lg, b_E], writes=[b_lg, b_E])

    def bc(ap2):
        return ap2.unsqueeze(2).to_broadcast([128, T_, N_EXP])
    M1, M2, DD, ED, DEN = sv[:, 0, :], sv[:, 1, :], sv[:, 2, :], sv[:, 3, :], sv[:, 4, :]
    c.op(c.DVE, lambda: nc.vector.reduce_max(M1, LG[:], axis=AX.X), **R)
    c.op(c.DVE, lambda: nc.vector.tensor_tensor(E1[:], LG[:], bc(M1), ALU.is_equal), **R)
    c.op(c.DVE, lambda: nc.vector.scalar_tensor_tensor(L2[:], E1[:], -1e30, LG[:], ALU.mult, ALU.add), **R)
    c.op(c.DVE, lambda: nc.vector.reduce_max(M2, L2[:], axis=AX.X), **R)
    c.op(c.DVE, lambda: nc.vector.tensor_tensor(E2[:], L2[:], bc(M2), ALU.is_equal), **R)
    c.op(c.DVE, lambda: nc.vector.tensor_tensor(L2[:], E1[:], E2[:], ALU.add), **R)
    c.op(c.DVE, lambda: nc.vector.tensor_copy(selb[:], L2[:].rearrange("p t e -> p (t e)")), reads=[b_lg], writes=[b_sel[0]])
    c.op(c.DVE, lambda: nc.vector.tensor_tensor(DD, M2, M1, ALU.subtract), **R)
    c.op(c.ACT, lambda: nc.scalar.activation(out=ED, in_=DD, func=AF.Exp), **R)
    c.op(c.DVE, lambda: nc.vector.tensor_scalar(DEN, ED, 1.0, None, ALU.add), **R)
    c.op(c.DVE, lambda: nc.vector.reciprocal(GG[:, :, 0], DEN), reads=[b_lg], writes=[b_GG])
    c.op(c.DVE, lambda: nc.vector.tensor_tensor(GG[:, :, 1], ED, GG[:, :, 0], ALU.mult), reads=[b_lg, b_GG], writes=[b_GG])
    b1, b2 = gb(), gb()
    NC_ = T_ * N_EXP
    c.op(c.PE, lambda: _mm(c, ps[b1][:, 0:NC_], tri[:], selb[:], True, True), reads=[b_sel[0]], writes=[bps[b1]])
    c.op(c.PE, lambda: _mm(c, ps[b2][:, 0:NC_], ones[:], selb[:], True, True), reads=[b_sel[0]], writes=[bps[b2]])
    c.op(c.DVE, lambda: nc.vector.tensor_copy(CN[:].rearrange("p t e -> p (t e)"), ps[b2][:, 0:NC_]), reads=[bps[b2]], writes=[b_base])
    c.op(c.DVE, lambda: nc.vector.memset(BASE[:, 0, :], 0.0), reads=[b_base], writes=[b_base])
    for s in range(1, T_):
        c.op(c.DVE, lambda: nc.vector.tensor_tensor(BASE[:, s, :], BASE[:, s - 1, :], CN[:, s - 1, :], ALU.add),
             reads=[b_base], writes=[b_base])
    c.op(c.DVE, lambda: nc.vector.tensor_tensor(base[:], BASE[:, T_ - 1, :], CN[:, T_ - 1, :], ALU.add), reads=[b_base], writes=[b_base])
    c.op(c.DVE, lambda: nc.vector.tensor_tensor(RK[:].rearrange("p t e -> p (t e)"), ps[b1][:, 0:NC_],
                                                BASE[:].rearrange("p t e -> p (t e)"), ALU.add),
         reads=[bps[b1], b_base], writes=[b_RK])
    cnt, padf, pst, pend = base[:], sm[:, 0:8], sm[:, 8:16], sm[:, 16:24]
    S_ = dict(reads=[b_base, b_sm], writes=[b_sm])
    qv, qf, qc = sm[:, 32:40], sm[:, 40:48], sm[:, 48:56]
    c.op(c.DVE, lambda: nc.vector.tensor_scalar(qv, cnt, float(MOE_BLK - 1), 1.0 / MOE_BLK, ALU.add, ALU.mult), **S_)
    c.op(c.DVE, lambda: nc.vector.tensor_copy(smi[:], qv), **S_)
    c.op(c.DVE, lambda: nc.vector.tensor_copy(qf, smi[:]), **S_)
    c.op(c.DVE, lambda: nc.vector.tensor_tensor(qc, qf, qv, ALU.is_gt), **S_)
    c.op(c.DVE, lambda: nc.vector.tensor_tensor(qf, qf, qc, ALU.subtract), **S_)
    c.op(c.DVE, lambda: nc.vector.tensor_scalar(padf, qf, float(MOE_BLK), None, ALU.mult), **S_)
    c.op(c.DVE, lambda: nc.vector.memset(pst[:, 0:1], 0.0), **S_)
    for e in range(1, N_EXP):
        c.op(c.DVE, lambda: nc.vector.tensor_tensor(pst[:, e:e + 1], pst[:, e - 1:e], padf[:, e - 1:e], ALU.add), **S_)
    c.op(c.DVE, lambda: nc.vector.tensor_tensor(pend, pst, padf, ALU.add), **S_)
    BVt = c.sb("m_bv", [128, NBLK], F32)
    BV = BVt[:]
    tbuf = c.sb("m_tb", [128, NBLK], F32)
    c.op(c.DVE, lambda: nc.vector.tensor_scalar(BV, iot[:, 0:NBLK], float(MOE_BLK), None, ALU.mult), **S_)
    c.op(c.DVE, lambda: nc.vector.memset(EBf[:], 0.0), **S_)
    for e in range(N_EXP):
        c.op(c.DVE, lambda: nc.vector.tensor_scalar(tbuf[:], BV, pend[:, e:e + 1], None, ALU.is_ge), **S_)
        c.op(c.DVE, lambda: nc.vector.tensor_tensor(EBf[:], EBf[:], tbuf[:], ALU.add), **S_)
    c.op(c.DVE, lambda: nc.vector.tensor_scalar(EBf[:], EBf[:], float(N_EXP - 1), float(MOE_NG * 128), ALU.min, ALU.mult), **S_)
    for b in range(NBLK):
        c.op(c.DVE, lambda: nc.vector.tensor_scalar(IW[:, b, :], gp[:], EBf[:, b:b + 1], None, ALU.add), **S_)
    c.op(c.DVE, lambda: nc.vector.tensor_copy(IWu[:], IW[:].rearrange("p b g -> p (b g)")), reads=[b_sm], writes=[b_IW])
    c.op(c.DVE, lambda: nc.vector.tensor_tensor(RK[:], RK[:], pst.unsqueeze(1).to_broadcast([128, T_, N_EXP]), ALU.add),
         reads=[b_RK, b_sm], writes=[b_RK])
    for k, EE in enumerate((E1, E2)):
        c.op(c.DVE, lambda: nc.vector.tensor_tensor(L2[:], RK[:], EE[:], ALU.mult), reads=[b_RK, b_E, b_lg], writes=[b_lg])
        c.op(c.DVE, lambda: nc.vector.reduce_sum(DST[:, :, k], L2[:], axis=AX.X), reads=[b_lg], writes=[b_DST])
    c.op(c.DVE, lambda: nc.vector.tensor_copy(DSTu[:], DST[:]), reads=[b_DST], writes=[b_DST])
    if dbg is not None:
        dd = c.dsem("m_dbg")
        c.dma(c.SP, dd, lambda: nc.sync.dma_start(out=dbg["dst"], in_=DSTu[:]), reads=[b_DST])
        c.dma(c.SP, dd, lambda: nc.sync.dma_start(out=dbg["iw"], in_=IWu[:]), reads=[b_IW])
        c.dma(c.SP, dd, lambda: nc.sync.dma_start(out=dbg["gg"], in_=GG[:]), reads=[b_GG])
        c.dma(c.SP, dd, lambda: nc.sync.dma_start(out=dbg["sm"], in_=sm[:]), reads=[b_sm])
        c.barrier()
        if dbg.get("stage", "route") == "route":
            return
    for s in range(NTK):
        for k in range(2):
            c.dma(c.POOL, d_sc, lambda: nc.gpsimd.indirect_dma_start(out=xs_d[:, :], out_offset=IND(ap=DSTu[:, s, k:k + 1], axis=0),
                                                                     in_=XB[:, s, :], in_offset=None),
                  reads=[b_DST, b_XB[s]])
    c.barrier()
    c.release(m1)
    if dbg is not None and dbg.get("stage") == "scatter":
        return

    m2 = c.mark()
    xsb = [c.sb(f"m_xsb{i}", [128, 4, D], BF16) for i in range(2)]
    xT2 = [c.sb(f"m_xT{i}", [128, KC, 512], BF16) for i in range(2)]
    hT = c.sb("m_hT", [128, 4 * MOE_NG, 512], BF16)
    wgb = [c.sb(f"m_wg{i}", [128, KC, 512], BF16) for i in range(2)]
    wub = [c.sb(f"m_wu{i}", [128, KC, 512], BF16) for i in range(2)]
    wdf = c.sb("m_wd", [128, MOE_NG, 4, D], BF16)
    sg = [c.sb(f"m_sg{i}", [128, 512], BF16) for i in range(2)]
    yt = [c.sb(f"m_yt{i}", [128, D], F32) for i in range(2)]
    b_xsb, b_xT2 = [Buf(), Buf()], [[Buf() for _ in range(4)] for _ in range(2)]
    b_hT = [Buf() for _ in range(4 * MOE_NG)]
    b_wg, b_wu, b_wd = [Buf(), Buf()], [Buf(), Buf()], [Buf() for _ in range(MOE_NG)]
    b_sg, b_yt = [Buf(), Buf()], [Buf(), Buf()]
    d_xsb = [c.dsem(f"m_dxsb{i}") for i in range(2)]
    d_wg = [c.dsem(f"m_dwg{i}") for i in range(2)]
    d_wu = [c.dsem(f"m_dwu{i}") for i in range(2)]
    d_wd = [c.dsem(f"m_dwd{i}") for i in range(MOE_NG)]
    d_yt = [c.dsem(f"m_dyt{i}") for i in range(2)]
    psG, psU, psY = ps[0:2], ps[2:4], ps[4:8]
    b_psG, b_psU, b_psY = bps[0:2], bps[2:4], bps[4:8]
    if wready:
        c._wait(c.POOL, wready)
    wg3 = wg_s.rearrange("r (k n) -> r k n", k=KC)
    wu3 = wu_s.rearrange("r (k n) -> r k n", k=KC)
    wd3 = wd_s.rearrange("r (c d) -> r c d", c=4)
    NGT = NBLK * MOE_NG
    loaded = [-1]

    def load_gu(j):
        if j >= NGT or j <= loaded[0]:
            return
        loaded[0] = j
        sl = j % 2
        c.dma(c.POOL, d_wg[sl], lambda: nc.gpsimd.indirect_dma_start(out=wgb[sl][:].rearrange("p k n -> p (k n)"), out_offset=None, in_=wg_s[:, :],
                                                                     in_offset=IND(ap=IWu[:, j:j + 1], axis=0)),
              reads=[b_IW], writes=[b_wg[sl]])
        c.dma(c.POOL, d_wu[sl], lambda: nc.gpsimd.indirect_dma_start(out=wub[sl][:].rearrange("p k n -> p (k n)"), out_offset=None, in_=wu_s[:, :],
                                                                     in_offset=IND(ap=IWu[:, j:j + 1], axis=0)),
              reads=[b_IW], writes=[b_wu[sl]])

    def load_wd(b):
        if b >= NBLK:
            return
        for g in range(MOE_NG):
            j = b * MOE_NG + g
            c.dma(c.POOL, d_wd[g], lambda: nc.gpsimd.indirect_dma_start(out=wdf[:, g, :, :].rearrange("p c d -> p (c d)"), out_offset=None, in_=wd_s[:, :],
                                                                        in_offset=IND(ap=IWu[:, j:j + 1], axis=0)),
                  reads=[b_IW], writes=[b_wd[g]])

    def load_x(b):
        if b >= NBLK:
            return
        i = b % 2
        c.dma(c.SP, d_xsb[i], lambda: nc.sync.dma_start(
            out=xsb[i][:], in_=xs_d[b * MOE_BLK:(b + 1) * MOE_BLK, :].rearrange("(s p) d -> p s d", p=128)), writes=[b_xsb[i]])

    load_x(0)
    load_gu(0)
    load_gu(1)
    load_wd(0)
    kk = 0
    ycount = 0
    def transposes(b):
        if b >= NBLK:
            return
        i = b % 2
        xT, b_xT = xT2[i], b_xT2[i]
        for sub in range(4):
            bk = 4 + sub
            pT = ps[bk][:].bitcast(BF16)

            def tr():
                ins = None
                for kc in range(KC):
                    ins = nc.tensor.transpose(pT[:, kc * 128:(kc + 1) * 128], xsb[i][:, sub, kc * 128:(kc + 1) * 128], ident[:])
                return ins
            c.op(c.PE, tr, reads=[b_xsb[i]], writes=[bps[bk]])
            c.op(c.ACT, lambda: nc.scalar.copy(out=xT[:, :, sub * 128:(sub + 1) * 128],
                                               in_=pT.rearrange("p (k t) -> p k t", k=KC)),
                 reads=[bps[bk]], writes=[b_xT[sub]])

    load_x(1)
    transposes(0)
    for b in range(NBLK):
        load_x(b + 2)
        i = b % 2
        xT, b_xT = xT2[i], b_xT2[i]
        for g in range(MOE_NG):
            j = b * MOE_NG + g
            sl = j % 2
            for ci in range(4):
                k = kk % 2
                kk += 1
                ch = g * 4 + ci

                def mmg(w, out):
                    ins = None
                    for kc in range(KC):
                        ins = _mm(c, out[:], w[sl][:, kc, ci * 128:(ci + 1) * 128], xT[:, kc, :], kc == 0, kc == KC - 1)
                    return ins
                c.op(c.PE, lambda: mmg(wgb, psG[k]), reads=[b_wg[sl]] + b_xT, writes=[b_psG[k]])
                c.op(c.PE, lambda: mmg(wub, psU[k]), reads=[b_wu[sl]] + b_xT, writes=[b_psU[k]])
                c.op(c.ACT, lambda: nc.scalar.activation(out=sg[k][:], in_=psG[k][:], func=AF.Silu),
                     reads=[b_psG[k]], writes=[b_sg[k]])
                c.op(c.DVE, lambda: nc.vector.tensor_tensor(hT[:, ch, :], sg[k][:], psU[k][:], ALU.mult),
                     reads=[b_sg[k], b_psU[k]], writes=[b_hT[ch]])
            load_gu(j + 2)
        transposes(b + 1)
        for hf in range(2):
            for sb2 in range(2):
                sub = hf * 2 + sb2
                for dh in range(2):
                    bk = sb2 * 2 + dh

                    def mmd():
                        ins = None
                        for ch in range(4 * MOE_NG):
                            ins = _mm(c, psY[bk][:], hT[:, ch, sub * 128:(sub + 1) * 128],
                                      wdf[:, ch // 4, ch % 4, dh * 512:(dh + 1) * 512], ch == 0, ch == 4 * MOE_NG - 1)
                        return ins
                    c.op(c.PE, mmd, reads=b_wd + b_hT, writes=[b_psY[bk]])
                yi = ycount % 2
                ycount += 1
                for dh in range(2):
                    bk = sb2 * 2 + dh
                    eng = c.ACT if dh == 0 else c.DVE
                    if dh == 0:
                        c.op(c.ACT, lambda: nc.scalar.copy(out=yt[yi][:, 0:512], in_=psY[bk][:]), reads=[b_psY[bk]], writes=[b_yt[yi]])
                    else:
                        c.op(c.DVE, lambda: nc.vector.tensor_copy(yt[yi][:, 512:1024], psY[bk][:]), reads=[b_psY[bk]], writes=[b_yt[yi]])
                r0 = b * MOE_BLK + sub * 128
                c.dma(c.SP, d_yt[yi], lambda: nc.sync.dma_start(out=yrow_d[r0:r0 + 128, :], in_=yt[yi][:]), reads=[b_yt[yi]])
        load_wd(b + 1)
    c.barrier()
    c.release(m2)
    if dbg is not None and dbg.get("stage") == "blocks":
        return

    y1 = [c.sb(f"m_y1{i}", [128, D], F32) for i in range(3)]
    y2 = [c.sb(f"m_y2{i}", [128, D], F32) for i in range(3)]
    xr = [c.sb(f"m_xr{i}", [128, D], F32) for i in range(5)]
    lnp = LNPipe(c, lnw[:], lnb[:], "m_ln", pool_add=True, store="sp_delayed")
    tmp2 = c.sb("m_tmp2", [128, D], F32)
    st = c.sb("m_st", [128, 12], F32)
    mv = c.sb("m_mv", [128, 8], F32)
    b_y1, b_y2, b_xr = [Buf(), Buf(), Buf()], [Buf(), Buf(), Buf()], [Buf() for _ in range(5)]
    b_tmp2, b_small = Buf(), Buf()
    d_y1 = [c.dsem(f"m_dy1{i}") for i in range(3)]
    d_y2 = [c.dsem(f"m_dy2{i}") for i in range(3)]
    d_xr = [c.dsem(f"m_dxr{i}") for i in range(5)]
    d_o = [c.dsem(f"m_do{i}") for i in range(5)]
    def gath(s):
        if s >= NTK:
            return
        i = s % 3
        c.dma(c.POOL, d_y1[i], lambda: nc.gpsimd.indirect_dma_start(out=y1[i][:], out_offset=None, in_=yrow_d[:, :],
                                                                    in_offset=IND(ap=DSTu[:, s, 0:1], axis=0)),
              reads=[b_DST], writes=[b_y1[i]])
        c.dma(c.POOL, d_y2[i], lambda: nc.gpsimd.indirect_dma_start(out=y2[i][:], out_offset=None, in_=yrow_d[:, :],
                                                                    in_offset=IND(ap=DSTu[:, s, 1:2], axis=0)),
              reads=[b_DST], writes=[b_y2[i]])

    def ldx(s):
        if s >= NTK:
            return
        j = s % 5
        t0 = s * 128
        c.dma(c.SP, d_xr[j], lambda: nc.sync.dma_start(out=xr[j][:], in_=x_in[t0:t0 + 128, :]), writes=[b_xr[j]])
        c.op(c.ACT, lambda: nc.scalar.activation(out=xr[j][:], in_=xr[j][:], func=AF.Copy, scale=ALPHA),
             reads=[b_xr[j]], writes=[b_xr[j]])

    gath(0)
    gath(1)
    ldx(0)
    for s in range(NTK):
        i = s % 3
        j = s % 5
        t0 = s * 128
        lnp.step()
        ldx(s + 1)
        c.op(c.DVE, lambda: nc.vector.scalar_tensor_tensor(xr[j][:], y1[i][:], GG[:, s, 0:1], xr[j][:], ALU.mult, ALU.add),
             reads=[b_y1[i], b_xr[j], b_GG], writes=[b_xr[j]])
        c.op(c.DVE, lambda: nc.vector.scalar_tensor_tensor(xr[j][:], y2[i][:], GG[:, s, 1:2], xr[j][:], ALU.mult, ALU.add),
             reads=[b_y2[i], b_xr[j], b_GG], writes=[b_xr[j]])
        gath(s + 2)

        def fin(Q, q, j=j, t0=t0):
            c.dma(Q, d_o[j], lambda: q.dma_start(out=x_out[t0:t0 + 128, :], in_=xr[j][:]), reads=[b_xr[j]])
        lnp.push(xr[j][:], xr[j][:], b_xr[j], b_xr[j], fin)
    lnp.flush()
    c.barrier()
    c.release(m0)
```

```python
import numpy as np
import concourse.bass as bass
import concourse.mybir as mybir
from concourse.bass_utils import run_bass_kernel_spmd

F32, BF16, I32 = mybir.dt.float32, mybir.dt.bfloat16, mybir.dt.int32
AF = mybir.ActivationFunctionType
ALU = mybir.AluOpType
AX = mybir.AxisListType

D = 1024
S = 4096
NB = 8
ALPHA = 4.0 ** 0.25
LN_EPS = 1e-5
D_FF = 2816
N_EXP = 8
D_FFE = 3584
KC = D // 128
SAME_SYNC = True
NEG = -30000.0


class Eng:
    def __init__(self, nc, e, name):
        self.e = e
        self.sem = nc.alloc_semaphore(name)
        self.n = 0
        self.seen = {}
        self.name = name


class Buf:
    __slots__ = ("w", "r", "name")

    def __init__(self, name=""):
        self.w = None
        self.r = {}
        self.name = name


class Ctx:
    def __init__(self, nc):
        self.nc = nc
        self.PE = Eng(nc, nc.tensor, "s_pe")
        self.ACT = Eng(nc, nc.scalar, "s_act")
        self.DVE = Eng(nc, nc.vector, "s_dve")
        self.POOL = Eng(nc, nc.gpsimd, "s_pool")
        self.SP = Eng(nc, nc.sync, "s_sp")
        self.engs = [self.PE, self.ACT, self.DVE, self.POOL, self.SP]
        self.dsems = []
        self.stack = []
        self.psum = []

    def dsem(self, name):
        self.uid = getattr(self, "uid", 0) + 1
        d = Eng(self.nc, None, f"{name}_{self.uid}")
        self.dsems.append(d)
        return d

    def _wait(self, E, deps):
        best = {}
        for (E2, v) in deps:
            if E2 is E and not SAME_SYNC:
                continue
            if best.get(E2, 0) < v:
                best[E2] = v
        for E2, v in best.items():
            if E.seen.get(E2, 0) < v:
                E.e.wait_ge(E2.sem, v)
                E.seen[E2] = v

    @staticmethod
    def _deps(reads, writes):
        deps = []
        for b in reads:
            if b.w is not None:
                deps.append(b.w)
        for b in writes:
            if b.w is not None:
                deps.append(b.w)
            deps.extend(b.r.items())
        return deps

    def op(self, E, fn, reads=(), writes=()):
        self._wait(E, self._deps(reads, writes))
        ins = fn()
        E.n += 1
        ins.then_inc(E.sem, 1)
        ev = (E, E.n)
        for b in reads:
            b.r[E] = E.n
        for b in writes:
            b.w = ev
            b.r = {}
        return ev

    def dma(self, Q, Dm, fn, reads=(), writes=()):
        self._wait(Q, self._deps(reads, writes))
        ins = fn()
        Dm.n += 16
        ins.then_inc(Dm.sem, 16)
        ev = (Dm, Dm.n)
        for b in reads:
            b.r[Dm] = Dm.n
        for b in writes:
            b.w = ev
            b.r = {}
        return ev

    def barrier(self, include=None):
        allc = self.engs + [d for d in self.dsems if (include is None or d in include)]
        for E in self.engs:
            for X in allc:
                if X is E:
                    continue
                if X.n > 0 and E.seen.get(X, 0) < X.n:
                    E.e.wait_ge(X.sem, X.n)
                    E.seen[X] = X.n

    def sb(self, name, shape, dt):
        self.uid = getattr(self, "uid", 0) + 1
        g = self.nc.sbuf_tensor(f"{name}_{self.uid}", list(shape), dt)
        t = g.__enter__()
        self.stack.append(g)
        return t

    def mark(self):
        return len(self.stack)

    def release(self, mark):
        while len(self.stack) > mark:
            g = self.stack.pop()
            g.__exit__(None, None, None)


def _mm(c, out, lhsT, rhs, start, stop):
    return c.nc.tensor.matmul(out, lhsT, rhs, start=start, stop=stop)


def build_consts(c):
    nc = c.nc
    ident = c.sb("k_ident", [128, 128], BF16)
    idx = c.sb("k_idx", [128, 128], F32)
    iot = c.sb("k_iot", [128, 512], F32)
    gp = c.sb("k_gp", [128, 7], F32)
    b = Buf()
    c.op(c.POOL, lambda: nc.gpsimd.iota(gp[:], [[128, 7]], base=0, channel_multiplier=1,
                                        allow_small_or_imprecise_dtypes=True), writes=[b])
    c.op(c.POOL, lambda: nc.gpsimd.iota(idx[:], [[1, 128]], base=0, channel_multiplier=-1,
                                        allow_small_or_imprecise_dtypes=True), writes=[b])
    c.op(c.POOL, lambda: nc.gpsimd.iota(iot[:], [[1, 512]], base=0, channel_multiplier=0,
                                        allow_small_or_imprecise_dtypes=True), writes=[b])
    bi = Buf()
    c.op(c.DVE, lambda: nc.vector.tensor_scalar(ident[:], idx[:], 0.0, None, ALU.is_equal), reads=[b], writes=[bi])
    c.barrier()
    c.gp = gp
    return ident, idx, iot


def layer_norm_tile(c, src, dst, lnw, lnb, tmp, st, mv, bsrc, bdst, btmp, bsmall, pool_add=True):
    nc = c.nc
    c.op(c.DVE, lambda: nc.vector.bn_stats(st[:, 0:6], src[:, 0:512]), reads=[bsrc], writes=[bsmall])
    c.op(c.DVE, lambda: nc.vector.bn_stats(st[:, 6:12], src[:, 512:1024]), reads=[bsrc, bsmall], writes=[bsmall])
    c.op(c.DVE, lambda: nc.vector.bn_aggr(mv[:, 0:2], st[:, 0:12]), reads=[bsmall], writes=[bsmall])
    c.op(c.DVE, lambda: nc.vector.tensor_scalar(mv[:, 2:3], mv[:, 1:2], LN_EPS, None, ALU.add), reads=[bsmall], writes=[bsmall])
    c.op(c.ACT, lambda: nc.scalar.activation(out=mv[:, 3:4], in_=mv[:, 2:3], func=AF.Sqrt), reads=[bsmall], writes=[bsmall])
    c.op(c.DVE, lambda: nc.vector.reciprocal(mv[:, 4:5], mv[:, 3:4]), reads=[bsmall], writes=[bsmall])
    c.op(c.DVE, lambda: nc.vector.tensor_scalar(mv[:, 5:6], mv[:, 0:1], -1.0, mv[:, 4:5], ALU.mult, ALU.mult),
         reads=[bsmall], writes=[bsmall])
    c.op(c.ACT, lambda: nc.scalar.activation(out=dst, in_=src, func=AF.Identity, scale=mv[:, 4:5], bias=mv[:, 5:6]),
         reads=[bsrc, bsmall], writes=[bdst])
    c.op(c.DVE, lambda: nc.vector.tensor_tensor(dst, dst, lnw, ALU.mult), reads=[bdst], writes=[bdst])
    if pool_add:
        c.op(c.POOL, lambda: nc.gpsimd.tensor_tensor(dst, dst, lnb, ALU.add), reads=[bdst], writes=[bdst])
    else:
        c.op(c.DVE, lambda: nc.vector.tensor_tensor(dst, dst, lnb, ALU.add), reads=[bdst], writes=[bdst])


class LNPipe:
    def __init__(self, c, lnw, lnb, name, nbuf=4, pool_add=True, store="sp"):
        self.c, self.lnw, self.lnb, self.pool_add, self.store = c, lnw, lnb, pool_add, store
        self.st = [c.sb(f"{name}_st{i}", [128, 12], F32) for i in range(nbuf)]
        self.mv = [c.sb(f"{name}_mv{i}", [128, 8], F32) for i in range(nbuf)]
        self.bs = [Buf() for _ in range(nbuf)]
        self.n = 0
        self.q = []

    def push(self, src, dst, bsrc, bdst, fin=None):
        k = self.n % len(self.st)
        self.n += 1
        self.q.append(dict(stage=0, src=src, dst=dst, bsrc=bsrc, bdst=bdst, fin=fin, k=k))

    def step(self):
        c, nc = self.c, self.c.nc
        for it in list(self.q):
            st, mv, bsm = self.st[it["k"]], self.mv[it["k"]], self.bs[it["k"]]
            src, dst, bsrc, bdst = it["src"], it["dst"], it["bsrc"], it["bdst"]
            if it["stage"] == 0:
                c.op(c.DVE, lambda: nc.vector.bn_stats(st[:, 0:6], src[:, 0:512]), reads=[bsrc], writes=[bsm])
                c.op(c.DVE, lambda: nc.vector.bn_stats(st[:, 6:12], src[:, 512:1024]), reads=[bsrc, bsm], writes=[bsm])
                c.op(c.DVE, lambda: nc.vector.bn_aggr(mv[:, 0:2], st[:, 0:12]), reads=[bsm], writes=[bsm])
                c.op(c.DVE, lambda: nc.vector.tensor_scalar(mv[:, 2:3], mv[:, 1:2], LN_EPS, None, ALU.add), reads=[bsm], writes=[bsm])
                c.op(c.ACT, lambda: nc.scalar.activation(out=mv[:, 3:4], in_=mv[:, 2:3], func=AF.Sqrt), reads=[bsm], writes=[bsm])
            elif it["stage"] == 1:
                c.op(c.DVE, lambda: nc.vector.reciprocal(mv[:, 4:5], mv[:, 3:4]), reads=[bsm], writes=[bsm])
                c.op(c.DVE, lambda: nc.vector.tensor_scalar(mv[:, 5:6], mv[:, 0:1], -1.0, mv[:, 4:5], ALU.mult, ALU.mult),
                     reads=[bsm], writes=[bsm])
                c.op(c.ACT, lambda: nc.scalar.activation(out=dst, in_=src, func=AF.Identity, scale=mv[:, 4:5], bias=mv[:, 5:6]),
                     reads=[bsrc, bsm], writes=[bdst])
            elif it["stage"] == 2:
                c.op(c.DVE, lambda: nc.vector.tensor_tensor(dst, dst, self.lnw, ALU.mult), reads=[bdst], writes=[bdst])
                if self.pool_add:
                    c.op(c.POOL, lambda: nc.gpsimd.tensor_tensor(dst, dst, self.lnb, ALU.add), reads=[bdst], writes=[bdst])
                else:
                    c.op(c.DVE, lambda: nc.vector.tensor_tensor(dst, dst, self.lnb, ALU.add), reads=[bdst], writes=[bdst])
                if self.store == "pool":
                    if it["fin"] is not None:
                        it["fin"](c.POOL, nc.gpsimd)
                    self.q.remove(it)
                elif self.store == "sp":
                    if it["fin"] is not None:
                        it["fin"](c.SP, nc.sync)
                    self.q.remove(it)
            if it["stage"] == 3:
                if it["fin"] is not None:
                    it["fin"](c.SP, nc.sync)
                self.q.remove(it)
                continue
            it["stage"] += 1

    def flush(self):
        while self.q:
            self.step()


def ffn_phase(c, x_in, x_out, wg, wu, wd, E, FF, CG, lnw_d, lnb_d, ident, router_d=None, TP=1024, seq=S,
              wready=None, pre_hook=None, pass_hooks=None):
    nc = c.nc
    m0 = c.mark()
    if pre_hook is not None:
        pre_hook()
    NS = TP // 128
    NT = TP // 512
    NP = seq // TP
    NG = FF // (CG * 128)
    assert FF % (CG * 128) == 0 and CG % 2 == 0
    moe = router_d is not None

    xT2 = [c.sb(f"f_xT{i}", [128, KC, TP], BF16) for i in range(2)]
    yacc2 = [c.sb(f"f_yacc{i}", [128, NS, D], F32) for i in range(2)]
    xin = [c.sb(f"f_xin{i}", [128, D], F32) for i in range(2)]
    xb = [c.sb(f"f_xb{i}", [128, D], BF16) for i in range(2)]
    wgb = [c.sb(f"f_wg{i}", [128, KC, CG * 128], BF16) for i in range(2)]
    wub = [c.sb(f"f_wu{i}", [128, KC, CG * 128], BF16) for i in range(2)]
    wdb = [c.sb(f"f_wd{i}", [128, CG, D], BF16) for i in range(2)]
    sg = [c.sb(f"f_sg{i}", [128, 512], BF16) for i in range(2)]
    hT = [c.sb(f"f_hT{i}", [128, CG, 512], BF16) for i in range(2)]
    otile = [c.sb(f"f_ot{i}", [128, D], F32) for i in range(3)]
    tmp = c.sb("f_tmp", [128, D], F32)
    lnw = c.sb("f_lnw", [128, D], F32)
    lnb = c.sb("f_lnb", [128, D], F32)
    st = c.sb("f_st", [128, 12], F32)
    mv = c.sb("f_mv", [128, 8], F32)
    if moe:
        wr = c.sb("f_wr", [128, N_EXP, D], F32)
        G = c.sb("f_G", [128, NS, N_EXP], F32)
        lg = c.sb("f_lg", [128, 8 * N_EXP], F32)
    b_xT2 = [[Buf() for _ in range(NS)] for _ in range(2)]
    b_yacc2 = [[Buf() for _ in range(NS)] for _ in range(2)]
    lnp = LNPipe(c, lnw[:], lnb[:], "f_ln", pool_add=False)
    b_xin = [Buf() for _ in range(2)]
    b_xb = [Buf() for _ in range(2)]
    b_wg = [Buf() for _ in range(2)]
    b_wu = [Buf() for _ in range(2)]
    b_wd = [Buf() for _ in range(2)]
    b_sg = [Buf() for _ in range(2)]
    b_hT = [[Buf() for _ in range(CG)] for _ in range(2)]
    b_ot = [Buf() for _ in range(3)]
    b_tmp, b_small, b_const, b_G, b_lg = Buf(), Buf(), Buf(), [Buf() for _ in range(NS)], Buf()
    ps = c.psum
    b_ps = c.b_psum
    psG, psU, psY = ps[0:2], ps[2:4], ps[4:8]
    b_psG, b_psU, b_psY = b_ps[0:2], b_ps[2:4], b_ps[4:8]
    d_xin = [c.dsem(f"fd_xin{i}") for i in range(2)]
    d_w = [[c.dsem(f"fd_w{t}{i}") for i in range(2)] for t in range(3)]
    d_ot = [c.dsem(f"fd_ot{i}") for i in range(3)]
    d_c = c.dsem("fd_c")

    c.dma(c.SP, d_c, lambda: nc.sync.dma_start(out=lnw[:], in_=lnw_d.partition_broadcast(128)), writes=[b_const])
    c.dma(c.SP, d_c, lambda: nc.sync.dma_start(out=lnb[:], in_=lnb_d.partition_broadcast(128)), reads=[], writes=[b_const])
    if moe:
        for e in range(N_EXP):
            c.dma(c.SP, d_c, lambda e=e: nc.sync.dma_start(
                out=wr[:, e, :], in_=router_d[e].partition_broadcast(128)), writes=[b_const])
    c.barrier()

    def prologue(p):
        tok0 = p * TP
        xT, yacc, b_xT, b_yacc = xT2[p % 2], yacc2[p % 2], b_xT2[p % 2], b_yacc2[p % 2]
        for s in range(NS):
            i = s % 2
            t0 = tok0 + s * 128
            c.dma(c.SP, d_xin[i], lambda: nc.sync.dma_start(out=xin[i][:], in_=x_in[t0:t0 + 128, :]), writes=[b_xin[i]])
            c.op(c.ACT, lambda: nc.scalar.activation(out=yacc[:, s, :], in_=xin[i][:], func=AF.Copy, scale=ALPHA),
                 reads=[b_xin[i]], writes=[b_yacc[s]])
            c.op(c.DVE, lambda: nc.vector.tensor_copy(xb[i][:], xin[i][:]), reads=[b_xin[i]], writes=[b_xb[i]])
            if moe:
                for e in range(N_EXP):
                    c.op(c.DVE, lambda: nc.vector.tensor_tensor(tmp[:], xin[i][:], wr[:, e, :], ALU.mult),
                         reads=[b_xin[i], b_const], writes=[b_tmp])
                    c.op(c.DVE, lambda: nc.vector.reduce_sum(lg[:, e:e + 1], tmp[:], axis=AX.X), reads=[b_tmp], writes=[b_lg])
                L = lg[:, 0:8]
                m1, eq, l2, m2, sel, nm1, ex, es, ss, rs = (lg[:, 8:9], lg[:, 16:24], lg[:, 24:32], lg[:, 9:10],
                                                            lg[:, 32:40], lg[:, 10:11], lg[:, 40:48], lg[:, 48:56],
                                                            lg[:, 11:12], lg[:, 12:13])
                R = dict(reads=[b_lg], writes=[b_lg])
                c.op(c.DVE, lambda: nc.vector.reduce_max(m1, L, axis=AX.X), **R)
                c.op(c.DVE, lambda: nc.vector.tensor_scalar(eq, L, m1, None, ALU.is_equal), **R)
                c.op(c.DVE, lambda: nc.vector.scalar_tensor_tensor(l2, eq, -1e30, L, ALU.mult, ALU.add), **R)
                c.op(c.DVE, lambda: nc.vector.reduce_max(m2, l2, axis=AX.X), **R)
                c.op(c.DVE, lambda: nc.vector.tensor_scalar(sel, L, m2, None, ALU.is_ge), **R)
                c.op(c.DVE, lambda: nc.vector.tensor_scalar(nm1, m1, -1.0, None, ALU.mult), **R)
                c.op(c.ACT, lambda: nc.scalar.activation(out=ex, in_=L, func=AF.Exp, bias=nm1, scale=1.0), **R)
                c.op(c.DVE, lambda: nc.vector.tensor_tensor(es, ex, sel, ALU.mult), **R)
                c.op(c.DVE, lambda: nc.vector.reduce_sum(ss, es, axis=AX.X), **R)
                c.op(c.DVE, lambda: nc.vector.reciprocal(rs, ss), **R)
                c.op(c.DVE, lambda: nc.vector.tensor_scalar(G[:, s, :], es, rs, None, ALU.mult),
                     reads=[b_lg], writes=[b_G[s]])
            pT = psY[s % 4][:].bitcast(BF16)
            bpT = b_psY[s % 4]

            def tr():
                ins = None
                for kc in range(KC):
                    ins = nc.tensor.transpose(pT[:, kc * 128:(kc + 1) * 128], xb[i][:, kc * 128:(kc + 1) * 128], ident[:])
                return ins
            c.op(c.PE, tr, reads=[b_xb[i]], writes=[bpT])
            c.op(c.ACT, lambda: nc.scalar.copy(out=xT[:, :, s * 128:(s + 1) * 128],
                                               in_=pT.rearrange("p (k t) -> p k t", k=KC)),
                 reads=[bpT], writes=[b_xT[s]])


    def main(p, deferred):
        tok0 = p * TP
        if pass_hooks and p < len(pass_hooks):
            pass_hooks[p]()
        xT, yacc, b_xT, b_yacc = xT2[p % 2], yacc2[p % 2], b_xT2[p % 2], b_yacc2[p % 2]
        items = [(e, g, tt) for e in range(E) for g in range(NG) for tt in range(NT)]
        groups = [(e, g) for e in range(E) for g in range(NG)]
        loaded = [-1]

        def load_group(j):
            if j >= len(groups) or j <= loaded[0]:
                return
            assert j == loaded[0] + 1
            loaded[0] = j
            e, g = groups[j]
            sl = j % 2
            c0 = g * CG * 128
            if wready:
                c._wait(c.SP, wready[e])
            c.dma(c.SP, d_w[0][sl], lambda: nc.sync.dma_start(
                out=wgb[sl][:], in_=wg[e].rearrange("(k p) n -> p k n", p=128)[:, :, c0:c0 + CG * 128]), writes=[b_wg[sl]])
            c.dma(c.SP, d_w[1][sl], lambda: nc.sync.dma_start(
                out=wub[sl][:], in_=wu[e].rearrange("(k p) n -> p k n", p=128)[:, :, c0:c0 + CG * 128]), writes=[b_wu[sl]])
            c.dma(c.SP, d_w[2][sl], lambda: nc.sync.dma_start(
                out=wdb[sl][:], in_=wd[e][c0:c0 + CG * 128, :].rearrange("(ci p) d -> p ci d", p=128)), writes=[b_wd[sl]])

        kcount = [0]

        def GU(idx, cis):
            e, g, tt = items[idx]
            j = e * NG + g
            sl = j % 2
            hb = idx % 2
            for ci in cis:
                k = kcount[0] % 2
                kcount[0] += 1

                def mmg(w, out):
                    ins = None
                    for kc in range(KC):
                        ins = _mm(c, out[:], w[sl][:, kc, ci * 128:(ci + 1) * 128], xT[:, kc, tt * 512:(tt + 1) * 512],
                                  kc == 0, kc == KC - 1)
                    return ins
                rx = [b_xT[tt * 4 + q] for q in range(4)]
                c.op(c.PE, lambda: mmg(wgb, psG[k]), reads=[b_wg[sl]] + rx, writes=[b_psG[k]])
                c.op(c.PE, lambda: mmg(wub, psU[k]), reads=[b_wu[sl]] + rx, writes=[b_psU[k]])
                c.op(c.ACT, lambda: nc.scalar.activation(out=sg[k][:], in_=psG[k][:], func=AF.Silu),
                     reads=[b_psG[k]], writes=[b_sg[k]])
                c.op(c.DVE, lambda: nc.vector.tensor_tensor(hT[hb][:, ci, :], sg[k][:], psU[k][:], ALU.mult),
                     reads=[b_sg[k], b_psU[k]], writes=[b_hT[hb][ci]])

        def DN(idx, sp):
            e, g, tt = items[idx]
            j = e * NG + g
            sl = j % 2
            hb = idx % 2
            for sub in (2 * sp, 2 * sp + 1):
                s = tt * 4 + sub
                for dh in range(2):
                    bk = (sub % 2) * 2 + dh

                    def mmd():
                        ins = None
                        for ci in range(CG):
                            ins = _mm(c, psY[bk][:], hT[hb][:, ci, sub * 128:(sub + 1) * 128],
                                      wdb[sl][:, ci, dh * 512:(dh + 1) * 512], ci == 0, ci == CG - 1)
                        return ins
                    c.op(c.PE, mmd, reads=[b_wd[sl]] + b_hT[hb], writes=[b_psY[bk]])
                    ysl = yacc[:, s, dh * 512:(dh + 1) * 512]
                    if moe:
                        c.op(c.DVE, lambda: nc.vector.scalar_tensor_tensor(ysl, psY[bk][:], G[:, s, e:e + 1], ysl,
                                                                           ALU.mult, ALU.add),
                             reads=[b_psY[bk], b_G[s], b_yacc[s]], writes=[b_yacc[s]])
                    else:
                        c.op(c.DVE, lambda: nc.vector.tensor_tensor(ysl, psY[bk][:], ysl, ALU.add),
                             reads=[b_psY[bk], b_yacc[s]], writes=[b_yacc[s]])

        half = CG // 2
        load_group(0)
        load_group(1)
        GU(0, range(CG))
        for idx in range(len(items)):
            e, g, tt = items[idx]
            j = e * NG + g
            nxt = idx + 1 if idx + 1 < len(items) else None
            DN(idx, 0)
            if nxt is not None:
                GU(nxt, range(0, half))
            DN(idx, 1)
            if nxt is not None:
                GU(nxt, range(half, CG))
            if tt == NT - 1:
                load_group(j + 2)
            if deferred:
                deferred.pop(0)()

    def epilogue(p):
        tok0 = p * TP
        yacc, b_yacc = yacc2[p % 2], b_yacc2[p % 2]
        work = []
        for s in range(NS):
            def w(s=s):
                i = s % 3
                t0 = tok0 + s * 128

                def fin(Q, q):
                    c.dma(Q, d_ot[i], lambda: q.dma_start(out=x_out[t0:t0 + 128, :], in_=otile[i][:]), reads=[b_ot[i]])
                lnp.push(yacc[:, s, :], otile[i][:], b_yacc[s], b_ot[i], fin)
                lnp.step()
            work.append(w)
        work.append(lnp.step)
        work.append(lnp.step)
        work.append(lnp.step)
        return work

    prologue(0)
    deferred = []
    for p in range(NP):
        main(p, deferred)
        while deferred:
            deferred.pop(0)()
        if p + 1 < NP:
            prologue(p + 1)
        deferred = epilogue(p)
    while deferred:
        deferred.pop(0)()
    lnp.flush()
    c.barrier()
    c.release(m0)


TWO_PI = float(np.float32(2.0 * np.pi))
PI_F = float(np.float32(np.pi))


def build_rope(c, pos_d, invf, sgn, cosT, sinT, b_tab, seq=S):
    nc = c.nc
    m = c.mark()
    CH = 1024
    posi = c.sb("rp_posi", [128, CH], I32)
    ang = c.sb("rp_ang", [128, CH], F32)
    wk = c.sb("rp_wk", [128, CH], F32)
    wi = c.sb("rp_wi", [128, CH], I32)
    r = c.sb("rp_r", [128, CH], F32)
    bp, ba, bw, bi, br = Buf(), Buf(), Buf(), Buf(), Buf()
    d = c.dsem("rp_d")
    for ch in range(seq // CH):
        sl = slice(ch * CH, (ch + 1) * CH)
        c.dma(c.SP, d, lambda: nc.sync.dma_start(out=posi[:], in_=pos_d[sl].partition_broadcast(128)), writes=[bp])
        c.op(c.DVE, lambda: nc.vector.tensor_copy(ang[:], posi[:]), reads=[bp], writes=[ba])
        c.op(c.DVE, lambda: nc.vector.tensor_scalar(ang[:], ang[:], invf[:, 0:1], None, ALU.mult), reads=[ba, b_tab], writes=[ba])
        for which in range(2):
            if which == 1:
                c.op(c.DVE, lambda: nc.vector.tensor_scalar(ang[:], ang[:], PI_F / 2, None, ALU.add), reads=[ba], writes=[ba])
            c.op(c.DVE, lambda: nc.vector.tensor_scalar(wk[:], ang[:], 1.0 / TWO_PI, None, ALU.mult), reads=[ba], writes=[bw])
            c.op(c.DVE, lambda: nc.vector.tensor_copy(wi[:], wk[:]), reads=[bw], writes=[bi])
            c.op(c.DVE, lambda: nc.vector.tensor_copy(wk[:], wi[:]), reads=[bi], writes=[bw])
            c.op(c.DVE, lambda: nc.vector.scalar_tensor_tensor(r[:], wk[:], -TWO_PI, ang[:], ALU.mult, ALU.add),
                 reads=[bw, ba], writes=[br])
            c.op(c.DVE, lambda: nc.vector.tensor_scalar(wk[:], r[:], PI_F, -TWO_PI, ALU.is_gt, ALU.mult), reads=[br], writes=[bw])
            c.op(c.DVE, lambda: nc.vector.tensor_tensor(r[:], r[:], wk[:], ALU.add), reads=[bw, br], writes=[br])
            c.op(c.DVE, lambda: nc.vector.tensor_scalar(wk[:], r[:], -PI_F, TWO_PI, ALU.is_lt, ALU.mult), reads=[br], writes=[bw])
            c.op(c.DVE, lambda: nc.vector.tensor_tensor(r[:], r[:], wk[:], ALU.add), reads=[bw, br], writes=[br])
            c.op(c.DVE, lambda: nc.vector.tensor_scalar(r[:], r[:], 3.1415925, -3.1415925, ALU.min, ALU.max), reads=[br], writes=[br])
            if which == 0:
                c.op(c.ACT, lambda: nc.scalar.activation(out=sinT[:, sl], in_=r[:], func=AF.Sin, scale=sgn[:, 0:1]),
                     reads=[br, b_tab], writes=[b_tab])
            else:
                c.op(c.ACT, lambda: nc.scalar.activation(out=cosT[:, sl], in_=r[:], func=AF.Sin), reads=[br], writes=[b_tab])
    c.barrier()
    c.release(m)


def build_masks(c, m_own, m_prev, G, prev_strict, b_m):
    nc = c.nc
    m = c.mark()
    idx = c.sb("mk_idx", [128, 128], F32)
    b = Buf()
    c.op(c.POOL, lambda: nc.gpsimd.iota(idx[:], [[1, 128]], base=0, channel_multiplier=-1,
                                        allow_small_or_imprecise_dtypes=True), writes=[b])
    for g in range(G):
        c.op(c.DVE, lambda: nc.vector.tensor_scalar(m_own[:, g * 128:(g + 1) * 128], idx[:], 0.0, NEG, ALU.is_lt, ALU.mult),
             reads=[b], writes=[b_m])
        c.op(c.DVE, lambda: nc.vector.tensor_scalar(m_prev[:, g * 128:(g + 1) * 128], idx[:], 0.0, NEG,
                                                    ALU.is_ge if prev_strict else ALU.is_gt, ALU.mult),
             reads=[b], writes=[b_m])
    c.barrier()
    c.release(m)


def rope_evac(c, psq, psqs, bq, bqs, cos_sl, sin_sl, b_tab, t1, t2, bt1, bt2, out_ap, b_out):
    nc = c.nc
    c.op(c.DVE, lambda: nc.vector.tensor_tensor(t1, psq, cos_sl, ALU.mult), reads=[bq, b_tab], writes=[bt1])
    c.op(c.DVE, lambda: nc.vector.tensor_tensor(t2, psqs, sin_sl, ALU.mult), reads=[bqs, b_tab], writes=[bt2])
    c.op(c.DVE, lambda: nc.vector.tensor_tensor(out_ap, t1, t2, ALU.add), reads=[bt1, bt2], writes=[b_out])


def mixer0_phase(c, x_in, x_out, pos_d, w_in_d, w_out_d, poolw_d, pscale_d, sinks_d, lnw_d, lnb_d,
                 invf_d, sgn_d, wcol_d, ident, idx, iot, seq=S, after_weights=None, rope=None):
    nc = c.nc
    m0 = c.mark()
    NTL = seq // 512
    NBK = seq // 128
    WIN = 2560
    OU, OQ, OQS, OK_, OKS, OV = 0, 256, 1024, 1792, 2048, 2304
    ps, bps = c.psum, c.b_psum
    gcount = [0]

    def gb():
        k = gcount[0] % 4
        gcount[0] += 1
        return k

    w_in = c.sb("a_win", [128, KC, WIN], BF16)
    w_out = c.sb("a_wout", [128, KC, D], BF16)
    cosT, sinT, b_tab = rope
    KT = c.sb("a_KT", [128, 2, seq], BF16)
    Vs = c.sb("a_V", [128, NBK, 4, 65], BF16)
    invf = c.sb("a_invf", [128, 1], F32)
    sgn = c.sb("a_sgn", [128, 1], F32)
    wcol = c.sb("a_wcol", [128, 2], F32)
    rcw = c.sb("a_rcw", [128, 2], F32)
    Wf = c.sb("a_wf", [128, 2, 128], F32)
    SC = c.sb("a_sc", [128, 256], F32)
    Wbd = c.sb("a_wbd", [128, 2, 128], BF16)
    esink = c.sb("a_esink", [128, 12], F32)
    lnw = c.sb("a_lnw", [128, D], F32)
    lnb = c.sb("a_lnb", [128, D], F32)
    xres = [c.sb(f"a_xres{i}", [128, D], F32) for i in range(3)]
    b_xres = [Buf(), Buf(), Buf()]
    RC0 = xres[1][:].rearrange("p (j n) -> p j n", j=2)
    m_own = c.sb("a_mown", [128, 384], BF16)
    m_prev = c.sb("a_mprev", [128, 384], BF16)
    b_w, b_const, b_m = Buf(), Buf(), Buf()
    d_c = c.dsem("a_dc")
    d_w = [c.dsem(f"a_dw{i}") for i in range(3)]
    wv = w_in_d.rearrange("(k p) n -> p k n", p=128)
    c.dma(c.POOL, d_w[0], lambda: nc.gpsimd.dma_start(out=w_in[:, :, 0:1280], in_=wv[:, :, 0:1280]), writes=[b_w])
    c.dma(c.POOL, d_w[1], lambda: nc.gpsimd.dma_start(out=w_in[:, :, 1280:2560], in_=wv[:, :, 1280:2560]), writes=[b_w])
    c.dma(c.POOL, d_w[2], lambda: nc.gpsimd.dma_start(out=w_out[:], in_=w_out_d.rearrange("(k p) n -> p k n", p=128)), writes=[b_w])
    b_w.w = None
    if after_weights is not None:
        after_weights()
    w_events = [(d_w[0], 16), (d_w[1], 16), (d_w[2], 16)]
    for (dst, src) in ((invf, invf_d), (sgn, sgn_d)):
        c.dma(c.SP, d_c, lambda: nc.sync.dma_start(out=dst[:], in_=src), writes=[b_tab])
    c.dma(c.SP, d_c, lambda: nc.sync.dma_start(out=wcol[:], in_=wcol_d), writes=[b_const])
    c.dma(c.SP, d_c, lambda: nc.sync.dma_start(out=lnw[:], in_=lnw_d.partition_broadcast(128)), writes=[b_const])
    c.dma(c.SP, d_c, lambda: nc.sync.dma_start(out=lnb[:], in_=lnb_d.partition_broadcast(128)), writes=[b_const])
    c.dma(c.SP, d_c, lambda: nc.sync.dma_start(out=SC[:], in_=pscale_d.partition_broadcast(128)), writes=[b_const])
    c.dma(c.SP, d_c, lambda: nc.sync.dma_start(out=esink[:], in_=sinks_d.partition_broadcast(128)), writes=[b_const])
    c.op(c.DVE, lambda: nc.vector.memset(Wf[:], 0.0), writes=[b_const])
    for g in range(4):
        hf, j = g % 2, g // 2
        c.dma(c.SP, d_c, lambda: nc.sync.dma_start(out=Wf[hf * 64:(hf + 1) * 64, j, hf * 64:(hf + 1) * 64], in_=poolw_d[g]),
              writes=[b_const])
    c.op(c.DVE, lambda: nc.vector.tensor_tensor(Wbd[:], Wf[:], SC[:].rearrange("p (j n) -> p j n", j=2), ALU.mult),
         reads=[b_const], writes=[b_const])
    c.op(c.ACT, lambda: nc.scalar.activation(out=esink[:], in_=esink[:], func=AF.Exp), reads=[b_const], writes=[b_const])
    c.op(c.DVE, lambda: nc.vector.reciprocal(rcw[:], wcol[:]), reads=[b_const], writes=[b_const])
    for j in range(2):
        c.op(c.DVE, lambda: nc.vector.tensor_scalar(RC0[:, j, :], iot[:], 1.0, wcol[:, j:j + 1], ALU.add, ALU.min),
             reads=[b_const], writes=[b_const, b_xres[1]])
    c.op(c.DVE, lambda: nc.vector.reciprocal(RC0, RC0), reads=[b_const], writes=[b_const, b_xres[1]])
    c.barrier()
    build_rope(c, pos_d, invf, sgn, cosT, sinT, b_tab, seq=seq)
    for g in range(3):
        c.op(c.DVE, lambda: nc.vector.tensor_scalar(m_own[:, g * 128:(g + 1) * 128], idx[:], 0.0, NEG, ALU.is_lt, ALU.mult),
             writes=[b_m])
        c.op(c.DVE, lambda: nc.vector.tensor_scalar(m_prev[:, g * 128:(g + 1) * 128], idx[:], 0.0, NEG, ALU.is_ge, ALU.mult),
             writes=[b_m])
    c.barrier()

    xin = [c.sb(f"a_xin{i}", [128, D], F32) for i in range(2)]
    xb = [c.sb(f"a_xb{i}", [128, D], BF16) for i in range(4)]
    xT = c.sb("a_xT", [128, KC, 512], BF16)
    QT = c.sb("a_QT", [128, 6, 512], BF16)
    U = c.sb("a_U", [128, 2, 528], F32)
    T1 = c.sb("a_T1", [128, 528], F32)
    T2 = c.sb("a_T2", [128, 528], F32)
    dT = c.sb("a_dT", [128, 2, 512], BF16)
    t1 = [c.sb("a_t1", [128, 512], F32)] * 2
    t2 = [c.sb("a_t2", [128, 512], F32)] * 2
    PT = [[c.sb(f"a_PT{i}_{k}", [128, 384], BF16) for k in range(8)] for i in range(1)]
    PT = [PT[0], PT[0]]
    mix = [c.sb(f"a_mix{i}", [128, D], BF16) for i in range(2)]
    mixT = [c.sb(f"a_mixT{i}", [128, KC, 128], BF16) for i in range(2)]
    den = c.sb("a_den", [128, 24], F32)
    T3 = t1[0][:]
    tmp = None
    b_xin, b_xb = [Buf(), Buf()], [Buf() for _ in range(4)]
    b_xT = [Buf() for _ in range(4)]
    b_QT = [Buf() for _ in range(6)]
    b_KT = [[Buf() for _ in range(NTL)] for _ in range(2)]
    b_V = [Buf() for _ in range(NBK)]
    b_U, b_T1, b_T2 = [Buf(), Buf()], Buf(), Buf()
    b_dT = [Buf(), Buf()]
    b_t1, b_t2 = [Buf()] * 2, [Buf()] * 2
    b_PT = [[Buf() for _ in range(8)]]
    b_PT = [b_PT[0], b_PT[0]]
    b_mix, b_mixT = [Buf(), Buf()], [Buf(), Buf()]
    b_den, b_tmp, b_small = Buf(), Buf(), Buf()
    b_T3 = b_t1[0]
    d_xin = [c.dsem(f"a_dxin{i}") for i in range(2)]
    d_xres = [c.dsem(f"a_dxres{i}") for i in range(3)]
    d_ot = [c.dsem(f"a_dot{i}") for i in range(3)]
    lnp = LNPipe(c, lnw[:], lnb[:], "a_ln")

    c.op(c.DVE, lambda: nc.vector.memset(U[:], 0.0), writes=b_U)
    c.op(c.DVE, lambda: nc.vector.memset(Vs[:], 1.0), writes=b_V)
    for E_ in (c.PE,):
        c._wait(E_, w_events)
    rcount = [0]

    def fm_group(col0, tsl, out_bank):
        def f():
            ins = None
            for kc in range(KC):
                ins = _mm(c, ps[out_bank][:], w_in[:, kc, col0:col0 + 128], xT[:, kc, :], kc == 0, kc == KC - 1)
            return ins
        c.op(c.PE, f, reads=b_xT, writes=[bps[out_bank]])

    def prefetch_x(t):
        for sub in range(4):
            i = sub % 2
            r0 = t * 512 + sub * 128
            c.dma(c.SP, d_xin[i], lambda: nc.sync.dma_start(out=xin[i][:], in_=x_in[r0:r0 + 128, :]), writes=[b_xin[i]])
            c.op(c.POOL, lambda: nc.gpsimd.tensor_copy(xb[sub][:], xin[i][:]), reads=[b_xin[i]], writes=[b_xb[sub]])

    def transpose_x(t):
        for sub in range(4):
            bk = gb()
            pT = ps[bk][:].bitcast(BF16)

            def tr():
                ins = None
                for kc in range(KC):
                    ins = nc.tensor.transpose(pT[:, kc * 128:(kc + 1) * 128], xb[sub][:, kc * 128:(kc + 1) * 128], ident[:])
                return ins
            c.op(c.PE, tr, reads=[b_xb[sub]], writes=[bps[bk]])
            c.op(c.ACT, lambda: nc.scalar.copy(out=xT[:, :, sub * 128:(sub + 1) * 128],
                                               in_=pT.rearrange("p (k t) -> p k t", k=KC)),
                 reads=[bps[bk]], writes=[b_xT[sub]])

    pending = [None]
    prefetch_x(0)
    transpose_x(0)
    for t in range(NTL):
        tsl = slice(t * 512, (t + 1) * 512)
        if t + 1 < NTL:
            prefetch_x(t + 1)
        if t > 0:
            c.op(c.DVE, lambda: nc.vector.tensor_copy(U[:, :, 0:16], U[:, :, 512:528]), reads=b_U, writes=b_U)
        for j in range(2):
            bk = gb()
            fm_group(OU + j * 128, tsl, bk)
            c.op(c.ACT, lambda: nc.scalar.copy(out=U[:, j, 16:528], in_=ps[bk][:]), reads=[bps[bk]], writes=[b_U[j]])
        for j in range(2):
            A = U[:, j, :]
            c.op(c.DVE, lambda: nc.vector.tensor_tensor(T1[:, 1:528], A[:, 1:528], A[:, 0:527], ALU.add),
                 reads=[b_U[j]], writes=[b_T1])
            c.op(c.DVE, lambda: nc.vector.tensor_tensor(T2[:, 3:528], T1[:, 3:528], T1[:, 1:526], ALU.add),
                 reads=[b_T1], writes=[b_T2])
            if j == 0:
                srcs = [(T1, b_T1, 0, 64), (T2, b_T2, 64, 128)]
            else:
                c.op(c.DVE, lambda: nc.vector.tensor_tensor(T1[:, 7:528], T2[:, 7:528], T2[:, 3:524], ALU.add),
                     reads=[b_T2], writes=[b_T1])
                c.op(c.DVE, lambda: nc.vector.tensor_tensor(T2[:, 15:528], T1[:, 15:528], T1[:, 7:520], ALU.add),
                     reads=[b_T1], writes=[b_T2])
                srcs = [(T1, b_T1, 0, 64), (T2, b_T2, 64, 128)]
            for (Sw, bS, p0, p1) in srcs:
                if t == 0:
                    c.op(c.DVE, lambda: nc.vector.tensor_tensor(T3[p0:p1, :], Sw[p0:p1, 16:528], RC0[p0:p1, j, :], ALU.mult),
                         reads=[bS, b_const, b_xres[1]], writes=[b_T3])
                    c.op(c.DVE, lambda: nc.vector.tensor_tensor(dT[p0:p1, j, :], T3[p0:p1, :], A[p0:p1, 16:528], ALU.subtract),
                         reads=[b_T3, b_U[j]], writes=[b_dT[j]])
                else:
                    c.op(c.DVE, lambda: nc.vector.scalar_tensor_tensor(dT[p0:p1, j, :], Sw[p0:p1, 16:528], rcw[p0:p1, j:j + 1],
                                                                       A[p0:p1, 16:528], ALU.mult, ALU.subtract),
                         reads=[bS, b_U[j], b_const], writes=[b_dT[j]])
        for j in range(8):
            ba, bb = gb(), gb()
            if j < 6:
                fm_group(OQ + j * 128, tsl, ba)
                fm_group(OQS + j * 128, tsl, bb)
                out_ap, bo = QT[:, j, :], b_QT[j]
            else:
                fm_group(OK_ + (j - 6) * 128, tsl, ba)
                fm_group(OKS + (j - 6) * 128, tsl, bb)
                out_ap, bo = KT[:, j - 6, tsl], b_KT[j - 6][t]
            k = rcount[0] % 2
            rcount[0] += 1
            rope_evac(c, ps[ba][:], ps[bb][:], bps[ba], bps[bb], cosT[:, tsl], sinT[:, tsl], b_tab,
                      t1[k][:], t2[k][:], b_t1[k], b_t2[k], out_ap, bo)
        for sub in range(4):
            blk = t * 4 + sub
            bk = gb()

            def fv():
                ins = None
                for kc in range(KC):
                    ins = _mm(c, ps[bk][:, 0:256], xT[:, kc, sub * 128:(sub + 1) * 128], w_in[:, kc, OV:OV + 256],
                              kc == 0, kc == KC - 1)
                return ins
            c.op(c.PE, fv, reads=[b_xT[sub]], writes=[bps[bk]])
            c.op(c.ACT, lambda: nc.scalar.copy(out=Vs[:, blk, :, 0:64],
                                               in_=ps[bk][:, 0:256].rearrange("p (h d) -> p h d", h=4)),
                 reads=[bps[bk]], writes=[b_V[blk]])
        if t + 1 < NTL:
            transpose_x(t + 1)
        for sub in range(4):
            bi_ = t * 4 + sub
            mi = bi_ % 2
            bk = gb()

            def fp():
                ins = None
                for j in range(2):
                    ins = _mm(c, ps[bk][:, j * 128:(j + 1) * 128], dT[:, j, sub * 128:(sub + 1) * 128], Wbd[:, j, :], True, True)
                return ins
            c.op(c.PE, fp, reads=b_dT + [b_const], writes=[bps[bk]])
            c.op(c.ACT, lambda: nc.scalar.copy(out=mix[mi][:, 0:256], in_=ps[bk][:, 0:256]), reads=[bps[bk]], writes=[b_mix[mi]])
            kbs = [bi_ - 1, bi_] if bi_ > 0 else [bi_]
            for kh in range(4):
                kt, ph = kh // 2, kh % 2
                psl = slice(ph * 64, (ph + 1) * 64)
                for kbi, kb in enumerate(kbs):
                    own = (kb == bi_)
                    sb_ = 4 + (kh * 2 + kbi) % 2
                    pidx = kh * 2 + kbi

                    def fs():
                        _mm(c, ps[sb_][:, 0:384], KT[psl, kt, kb * 128:(kb + 1) * 128],
                            QT[psl, 3 * kt:3 * kt + 3, sub * 128:(sub + 1) * 128], True, False)
                        return _mm(c, ps[sb_][:, 0:384], ident[:], (m_own if own else m_prev)[:], False, True)
                    c.op(c.PE, fs, reads=[b_KT[kt][kb // 4], b_m] + b_QT[3 * kt:3 * kt + 3], writes=[bps[sb_]])
                    c.op(c.ACT, lambda: nc.scalar.activation(out=PT[mi][pidx][:], in_=ps[sb_][:, 0:384], func=AF.Exp, scale=0.125),
                         reads=[bps[sb_]], writes=[b_PT[mi][pidx]])
            for hb in range(2):
                ob = 6 + hb

                def fo():
                    ins = None
                    for hh in range(6):
                        h = hb * 6 + hh
                        kh, i3 = h // 3, h % 3
                        for kbi, kb in enumerate(kbs):
                            ins = _mm(c, ps[ob][:, hh * 65:(hh + 1) * 65], PT[mi][kh * 2 + kbi][:, i3 * 128:(i3 + 1) * 128],
                                      Vs[:, kb, kh, :], kbi == 0, kbi == len(kbs) - 1)
                    return ins
                c.op(c.PE, fo, reads=b_PT[mi] + [b_V[kb] for kb in kbs], writes=[bps[ob]])
                O3 = ps[ob][:, 0:390].rearrange("p (h c) -> p h c", c=65)
                c.op(c.DVE, lambda: nc.vector.tensor_tensor(den[:, hb * 6:(hb + 1) * 6].rearrange("p (h o) -> p h o", o=1),
                                                            O3[:, :, 64:65],
                                                            esink[:, hb * 6:(hb + 1) * 6].rearrange("p (h o) -> p h o", o=1), ALU.add),
                     reads=[bps[ob], b_const], writes=[b_den])
                c.op(c.DVE, lambda: nc.vector.reciprocal(den[:, 12 + hb * 6:12 + (hb + 1) * 6], den[:, hb * 6:(hb + 1) * 6]),
                     reads=[b_den], writes=[b_den])
                rb = den[:, 12 + hb * 6:12 + (hb + 1) * 6].unsqueeze(2).to_broadcast([128, 6, 64])
                c.op(c.DVE, lambda: nc.vector.tensor_tensor(
                    mix[mi][:, 256 + hb * 384:256 + (hb + 1) * 384].rearrange("p (h d) -> p h d", h=6),
                    O3[:, :, 0:64], rb, ALU.mult), reads=[bps[ob], b_den], writes=[b_mix[mi]])
            bk = gb()
            pT = ps[bk][:].bitcast(BF16)

            def trm():
                ins = None
                for kc in range(KC):
                    ins = nc.tensor.transpose(pT[:, kc * 128:(kc + 1) * 128], mix[mi][:, kc * 128:(kc + 1) * 128], ident[:])
                return ins
            c.op(c.PE, trm, reads=[b_mix[mi]], writes=[bps[bk]])
            c.op(c.ACT, lambda: nc.scalar.copy(out=mixT[mi][:], in_=pT.rearrange("p (k t) -> p k t", k=KC)),
                 reads=[bps[bk]], writes=[b_mixT[mi]])
            r0 = bi_ * 128
            x3i = bi_ % 3
            lnp.step()
            c.dma(c.SP, d_xres[x3i], lambda: nc.sync.dma_start(out=xres[x3i][:], in_=x_in[r0:r0 + 128, :]), writes=[b_xres[x3i]])
            bos = []
            for dh in range(2):
                bo = gb()
                bos.append(bo)

                def fproj():
                    ins = None
                    for kc in range(KC):
                        ins = _mm(c, ps[bo][:], mixT[mi][:, kc, :], w_out[:, kc, dh * 512:(dh + 1) * 512], kc == 0, kc == KC - 1)
                    return ins
                c.op(c.PE, fproj, reads=[b_mixT[mi]], writes=[bps[bo]])
            for dh in range(2):
                bo = bos[dh]
                c.op(c.DVE, lambda: nc.vector.scalar_tensor_tensor(xres[x3i][:, dh * 512:(dh + 1) * 512], xres[x3i][:, dh * 512:(dh + 1) * 512],
                                                                   ALPHA, ps[bo][:], ALU.mult, ALU.add),
                     reads=[bps[bo], b_xres[x3i]], writes=[b_xres[x3i]])

            def fin(Q, q, x3i=x3i, r0=r0):
                c.dma(Q, d_ot[x3i], lambda: q.dma_start(out=x_out[r0:r0 + 128, :], in_=xres[x3i][:]), reads=[b_xres[x3i]])
            lnp.push(xres[x3i][:], xres[x3i][:], b_xres[x3i], b_xres[x3i], fin)
    lnp.flush()
    c.barrier()
    c.release(m0)


def make_ctx(nc):
    c = Ctx(nc)
    c.psum = [nc.alloc_psum_tensor(f"ps{i}", [128, 512], F32) for i in range(8)]
    c.b_psum = [Buf() for _ in range(8)]
    return c


def swap_half(cols):
    cols = np.asarray(cols).reshape(-1, 2, 32)
    return cols[:, ::-1, :].reshape(-1)


def even_perm():
    u = np.arange(0, 256)
    qb, kb, vb = 256, 1024, 1280
    qt = []
    for j in range(6):
        for hf in range(2):
            h = 3 * (2 * (j // 3) + hf) + (j % 3)
            qt.append(qb + h * 64 + np.arange(64))
    qt = np.concatenate(qt)
    kt = kb + np.arange(256)
    v = vb + np.arange(256)
    return np.concatenate([u, qt, swap_half(qt), kt, swap_half(kt), v])


def rope_consts():
    half = 32
    inv = (np.float32(10000.0) ** (-np.arange(half, dtype=np.float32) / np.float32(half))).astype(np.float32)
    p = np.arange(128)
    invf = inv[p % 32].reshape(128, 1).astype(np.float32)
    sgn = np.where((p % 64) < 32, -1.0, 1.0).astype(np.float32).reshape(128, 1)
    wcol = np.where(p[:, None] < 64, np.array([[2.0, 8.0]]), np.array([[4.0, 16.0]])).astype(np.float32)
    return invf, sgn, wcol


ODD_G = 1408
ODD_CONV0 = 3 * ODD_G
ODD_W = ODD_CONV0 + 1536
DILS = (1, 4, 16)


def odd_perm():
    cols = []
    for g in range(3):
        base = g * 768
        qt = []
        for j in range(4):
            for hf in range(2):
                h = j + 4 * hf
                qt.append(base + h * 64 + np.arange(64))
        qt = np.concatenate(qt)
        kt = base + 512 + np.arange(128)
        v = base + 640 + np.arange(128)
        cols += [qt, swap_half(qt), kt, swap_half(kt), v]
    cols.append(2304 + np.arange(1536))
    return np.concatenate(cols)


def mixer1_phase(c, x_in, x_out, pos_d, w_in_bf, w_out_bf, convw_d, lnw_d, lnb_d, invf_d, sgn_d,
                 obuf, dT_d, ident, idx, wready, seq=S, rope=None, hooks=None, zero_fill=None):
    nc = c.nc
    m0 = c.mark()
    NTL = seq // 512
    NBK = seq // 128
    ps, bps = c.psum, c.b_psum
    gcount = [0]
    nrot = [4]

    def gb():
        k = gcount[0] % nrot[0]
        gcount[0] += 1
        return k

    build_tab = rope is None
    if build_tab:
        cosT = c.sb("c_cos", [128, seq], F32)
        sinT = c.sb("c_sin", [128, seq], F32)
        b_tab = Buf()
    else:
        cosT, sinT, b_tab = rope
    invf = c.sb("c_invf", [128, 1], F32)
    sgn = c.sb("c_sgn", [128, 1], F32)
    cw = c.sb("c_cw", [128, 4, 3], F32)
    lnw = c.sb("c_lnw", [128, D], F32)
    lnb = c.sb("c_lnb", [128, D], F32)
    if zero_fill is not None:
        zt = c.sb("c_zero", [128, 1, D], BF16)
        b_zt, d_zt = Buf(), c.dsem("c_dzero")
        c.op(c.DVE, lambda: nc.vector.memset(zt[:], 0.0), writes=[b_zt])
    m_own = c.sb("c_mown", [128, 512], BF16)
    m_prev = c.sb("c_mprev", [128, 512], BF16)
    xin = [c.sb(f"c_xin{i}", [128, D], F32) for i in range(2)]
    xb = [c.sb(f"c_xb{i}", [128, D], BF16) for i in range(4)]
    xT = c.sb("c_xT", [128, KC, 512], BF16)
    t1 = [c.sb(f"c_t1{i}", [128, 512], F32) for i in range(2)]
    t2 = [c.sb(f"c_t2{i}", [128, 512], F32) for i in range(2)]
    b_const, b_m = Buf(), Buf()
    b_xin, b_xb = [Buf(), Buf()], [Buf() for _ in range(4)]
    b_xT = [Buf() for _ in range(4)]
    b_t1, b_t2 = [Buf(), Buf()], [Buf(), Buf()]
    d_c = c.dsem("c_dc")
    d_xin = [c.dsem(f"c_dxin{i}") for i in range(2)]
    d_w = c.dsem("c_dw")
    for (dst, src) in ((invf, invf_d), (sgn, sgn_d)):
        c.dma(c.SP, d_c, lambda: nc.sync.dma_start(out=dst[:], in_=src), writes=[b_tab])
    c.dma(c.SP, d_c, lambda: nc.sync.dma_start(out=cw[:], in_=convw_d), writes=[b_const])
    c.dma(c.SP, d_c, lambda: nc.sync.dma_start(out=lnw[:], in_=lnw_d.partition_broadcast(128)), writes=[b_const])
    c.dma(c.SP, d_c, lambda: nc.sync.dma_start(out=lnb[:], in_=lnb_d.partition_broadcast(128)), writes=[b_const])
    for g in range(4):
        c.op(c.DVE, lambda: nc.vector.tensor_scalar(m_own[:, g * 128:(g + 1) * 128], idx[:], 0.0, NEG, ALU.is_lt, ALU.mult),
             writes=[b_m])
        c.op(c.DVE, lambda: nc.vector.tensor_scalar(m_prev[:, g * 128:(g + 1) * 128], idx[:], 0.0, NEG, ALU.is_gt, ALU.mult),
             writes=[b_m])
    c.barrier()
    if build_tab:
        build_rope(c, pos_d, invf, sgn, cosT, sinT, b_tab, seq=seq)
    if wready:
        c._wait(c.SP, wready)
    wv = w_in_bf.rearrange("(k p) n -> p k n", p=128)

    def prefetch_x(t):
        for sub in range(4):
            i = sub % 2
            r0 = t * 512 + sub * 128
            c.dma(c.SP, d_xin[i], lambda: nc.sync.dma_start(out=xin[i][:], in_=x_in[r0:r0 + 128, :]), writes=[b_xin[i]])
            c.op(c.ACT, lambda: nc.scalar.copy(out=xb[sub][:], in_=xin[i][:]), reads=[b_xin[i]], writes=[b_xb[sub]])

    def transpose_x(t):
        for sub in range(4):
            bk = gb()
            pT = ps[bk][:].bitcast(BF16)

            def tr():
                ins = None
                for kc in range(KC):
                    ins = nc.tensor.transpose(pT[:, kc * 128:(kc + 1) * 128], xb[sub][:, kc * 128:(kc + 1) * 128], ident[:])
                return ins
            c.op(c.PE, tr, reads=[b_xb[sub]], writes=[bps[bk]])
            c.op(c.ACT, lambda: nc.scalar.copy(out=xT[:, :, sub * 128:(sub + 1) * 128],
                                               in_=pT.rearrange("p (k t) -> p k t", k=KC)),
                 reads=[bps[bk]], writes=[b_xT[sub]])

    def fm_group(w, b_wt, col0, out_bank):
        def f():
            ins = None
            for kc in range(KC):
                ins = _mm(c, ps[out_bank][:], w[:, kc, col0:col0 + 128], xT[:, kc, :], kc == 0, kc == KC - 1)
            return ins
        c.op(c.PE, f, reads=b_xT + [b_wt], writes=[bps[out_bank]])

    m1 = c.mark()
    nrot[0] = 8
    if hooks:
        hooks[0]()
    wc = c.sb("c_wc", [128, KC, 1536], BF16)
    Z = c.sb("c_Z", [128, 4, 514], F32)
    hd = [c.sb(f"c_hd{i}", [128, 512], F32) for i in range(2)]
    acc = [c.sb(f"c_acc{i}", [128, 512], F32) for i in range(2)]
    dts = [c.sb(f"c_dts{i}", [128, 4, 512], BF16) for i in range(2)]
    b_wc, b_Z = Buf(), [Buf() for _ in range(4)]
    b_hd, b_acc, b_dts = [Buf(), Buf()], [Buf(), Buf()], [Buf(), Buf()]
    d_dts = [c.dsem(f"c_ddts{i}") for i in range(2)]
    c.dma(c.SP, d_w, lambda: nc.sync.dma_start(out=wc[:], in_=wv[:, :, ODD_CONV0:ODD_CONV0 + 1536]), writes=[b_wc])
    c.op(c.DVE, lambda: nc.vector.memset(Z[:], 0.0), writes=b_Z)
    kk = 0
    prefetch_x(0)
    transpose_x(0)
    for t in range(NTL):
        prefetch_x((t + 1) % NTL)
        di = t % 2
        for ch in range(4):
            k = kk % 2
            kk += 1
            ba, bb, bc = gb(), gb(), gb()
            fm_group(wc, b_wc, 0 + ch * 128, ba)
            fm_group(wc, b_wc, 1024 + ch * 128, bb)
            fm_group(wc, b_wc, 512 + ch * 128, bc)
            if t > 0:
                c.op(c.DVE, lambda: nc.vector.tensor_copy(Z[:, ch, 0:2], Z[:, ch, 512:514]), reads=[b_Z[ch]], writes=[b_Z[ch]])
            c.op(c.ACT, lambda: nc.scalar.copy(out=hd[k][:], in_=ps[ba][:]), reads=[bps[ba]], writes=[b_hd[k]])
            c.op(c.DVE, lambda: nc.vector.tensor_tensor(Z[:, ch, 2:514], hd[k][:], ps[bb][:], ALU.mult),
                 reads=[b_hd[k], bps[bb]], writes=[b_Z[ch]])
            c.op(c.DVE, lambda: nc.vector.tensor_scalar(acc[k][:], Z[:, ch, 0:512], cw[:, ch, 0:1], None, ALU.mult),
                 reads=[b_Z[ch], b_const], writes=[b_acc[k]])
            c.op(c.DVE, lambda: nc.vector.scalar_tensor_tensor(acc[k][:], Z[:, ch, 1:513], cw[:, ch, 1:2], acc[k][:], ALU.mult, ALU.add),
                 reads=[b_Z[ch], b_acc[k]], writes=[b_acc[k]])
            c.op(c.DVE, lambda: nc.vector.scalar_tensor_tensor(acc[k][:], Z[:, ch, 2:514], cw[:, ch, 2:3], acc[k][:], ALU.mult, ALU.add),
                 reads=[b_Z[ch], b_acc[k]], writes=[b_acc[k]])
            c.op(c.DVE, lambda: nc.vector.tensor_tensor(dts[di][:, ch, :], acc[k][:], ps[bc][:], ALU.mult),
                 reads=[b_acc[k], bps[bc]], writes=[b_dts[di]])
        c.dma(c.SP, d_dts[di], lambda: nc.sync.dma_start(out=dT_d[:, :, t * 512:(t + 1) * 512].rearrange("c p t -> p c t"),
                                                          in_=dts[di][:]), reads=[b_dts[di]])
        transpose_x((t + 1) % NTL)
    c.barrier()
    c.release(m1)

    m2 = c.mark()
    nrot[0] = 4
    wg_ = c.sb("c_wg", [128, KC, ODD_G], BF16)
    QT = c.sb("c_QT", [128, 4, seq], BF16)
    KT = c.sb("c_KT", [128, seq], BF16)
    VT = c.sb("c_VT", [128, seq], BF16)
    Vs = c.sb("c_V", [128, NBK, 2, 65], BF16)
    PT = [[c.sb(f"c_PT{i}_{k}", [128, 512], BF16) for k in range(4)] for i in range(2)]
    Osb = [c.sb(f"c_O{i}", [128, 520], F32) for i in range(2)]
    b_wg = Buf()
    b_Q = [[Buf() for _ in range(NTL)] for _ in range(4)]
    b_K = [Buf() for _ in range(NTL)]
    b_VT = [Buf() for _ in range(NTL)]
    b_V = [Buf() for _ in range(NBK)]
    b_PT = [[Buf() for _ in range(4)] for _ in range(2)]
    b_O = [Buf(), Buf()]
    d_O = [c.dsem(f"c_dO{i}") for i in range(2)]
    c.op(c.DVE, lambda: nc.vector.memset(Vs[:], 1.0), writes=b_V)
    rcount = 0
    for g in range(3):
        if hooks:
            hooks[1 + g]()
        if zero_fill is not None and g == 0:
            zv = zero_fill.rearrange("(n p) d -> p n d", p=128)
            NZ = zv.shape[1]
            for z0 in range(0, NZ, 23):
                z1 = min(NZ, z0 + 23)
                c.dma(c.POOL, d_zt, lambda: nc.gpsimd.dma_start(out=zv[:, z0:z1, :], in_=zt[:].to_broadcast([128, z1 - z0, D])), reads=[b_zt])
        dil = DILS[g]
        SD = seq // dil
        NPT = 512 // dil
        c.dma(c.SP, d_w, lambda: nc.sync.dma_start(out=wg_[:], in_=wv[:, :, g * ODD_G:(g + 1) * ODD_G]), writes=[b_wg])

        def perm_out(buf_ap, t):
            return buf_ap.rearrange("p (r n) -> p r n", r=dil)[:, :, t * NPT:(t + 1) * NPT]

        def perm_in(ap):
            return ap.rearrange("p (n r) -> p r n", r=dil)

        for t in range(NTL):
            tsl = slice(t * 512, (t + 1) * 512)
            if not (g == 2 and t == NTL - 1):
                prefetch_x((t + 1) % NTL)
            for j in range(6):
                k = rcount % 2
                rcount += 1
                if j < 5:
                    ba, bb = gb(), gb()
                    c0 = j * 128 if j < 4 else 1024
                    fm_group(wg_, b_wg, c0, ba)
                    fm_group(wg_, b_wg, c0 + (512 if j < 4 else 128), bb)
                    dst = perm_out(QT[:, j, :], t) if j < 4 else perm_out(KT[:], t)
                    bo = b_Q[j][t] if j < 4 else b_K[t]
                    c.op(c.DVE, lambda: nc.vector.tensor_tensor(t1[k][:], ps[ba][:], cosT[:, tsl], ALU.mult),
                         reads=[bps[ba], b_tab], writes=[b_t1[k]])
                    c.op(c.DVE, lambda: nc.vector.tensor_tensor(t2[k][:], ps[bb][:], sinT[:, tsl], ALU.mult),
                         reads=[bps[bb], b_tab], writes=[b_t2[k]])
                    c.op(c.DVE, lambda: nc.vector.tensor_tensor(dst, perm_in(t1[k][:]), perm_in(t2[k][:]), ALU.add),
                         reads=[b_t1[k], b_t2[k]], writes=[bo])
                else:
                    ba = gb()
                    fm_group(wg_, b_wg, 1280, ba)
                    c.op(c.ACT, lambda: nc.scalar.copy(out=perm_out(VT[:], t), in_=perm_in(ps[ba][:])),
                         reads=[bps[ba]], writes=[b_VT[t]])
            if not (g == 2 and t == NTL - 1):
                transpose_x((t + 1) % NTL)
        for blk in range(NBK):
            bk = gb()
            pT = ps[bk][:].bitcast(BF16)
            c.op(c.PE, lambda: nc.tensor.transpose(pT[:, 0:128], VT[:, blk * 128:(blk + 1) * 128], ident[:]),
                 reads=b_VT, writes=[bps[bk]])
            c.op(c.ACT, lambda: nc.scalar.copy(out=Vs[:, blk, :, 0:64], in_=pT[:, 0:128].rearrange("p (h d) -> p h d", h=2)),
                 reads=[bps[bk]], writes=[b_V[blk]])
        NJ = SD // 128
        for r in range(dil):
            for jb in range(NJ):
                blk = r * NJ + jb
                mi = blk % 2
                kbs = [jb - 1, jb] if jb > 0 else [jb]
                for kh in range(2):
                    psl = slice(kh * 64, (kh + 1) * 64)
                    for kbi, kb in enumerate(kbs):
                        own = (kb == jb)
                        sb_ = 4 + (kh * 2 + kbi) % 2
                        pidx = kh * 2 + kbi
                        kc0 = (r * NJ + kb) * 128

                        def fs():
                            _mm(c, ps[sb_][:], KT[psl, kc0:kc0 + 128], QT[psl, :, blk * 128:(blk + 1) * 128], True, False)
                            return _mm(c, ps[sb_][:], ident[:], (m_own if own else m_prev)[:], False, True)
                        c.op(c.PE, fs, reads=b_K + [b_m] + [b for bl in b_Q for b in bl], writes=[bps[sb_]])
                        c.op(c.ACT, lambda: nc.scalar.activation(out=PT[mi][pidx][:], in_=ps[sb_][:], func=AF.Exp, scale=0.125),
                             reads=[bps[sb_]], writes=[b_PT[mi][pidx]])
                for kh in range(2):
                    ob = 6 + kh

                    def fo():
                        ins = None
                        for i4 in range(4):
                            for kbi, kb in enumerate(kbs):
                                ins = _mm(c, ps[ob][:, i4 * 65:(i4 + 1) * 65], PT[mi][kh * 2 + kbi][:, i4 * 128:(i4 + 1) * 128],
                                          Vs[:, r * NJ + kb, kh, :], kbi == 0, kbi == len(kbs) - 1)
                        return ins
                    c.op(c.PE, fo, reads=b_PT[mi] + [b_V[r * NJ + kb] for kb in kbs], writes=[bps[ob]])
                    c.op(c.ACT, lambda: nc.scalar.copy(out=Osb[mi][:, kh * 260:(kh + 1) * 260], in_=ps[ob][:, 0:260]),
                         reads=[bps[ob]], writes=[b_O[mi]])
                tok0 = jb * 128 * dil + r
                c.dma(c.SP, d_O[mi], lambda: nc.sync.dma_start(out=obuf.rearrange("(n r) g c -> r n g c", r=dil)[r, jb * 128:(jb + 1) * 128, g, :], in_=Osb[mi][:]),
                      reads=[b_O[mi]])
    c.barrier()
    c.release(m2)

    w_out = c.sb("c_wout", [128, KC, D], BF16)
    Ob = [c.sb(f"c_Ob{i}", [128, 3, 520], F32) for i in range(2)]
    dl = [c.sb(f"c_dl{i}", [128, 4, 512], BF16) for i in range(2)]
    cmix = [c.sb(f"c_cmix{i}", [128, 512], BF16) for i in range(2)]
    cT = [c.sb(f"c_cT{i}", [128, 4, 128], BF16) for i in range(2)]
    xres = [c.sb(f"c_xres{i}", [128, D], F32) for i in range(3)]
    ot = [c.sb(f"c_ot{i}", [128, D], F32) for i in range(3)]
    lnp = LNPipe(c, lnw[:], lnb[:], "c_ln", pool_add=False, store="sp_delayed")
    rd = c.sb("c_rd", [128, 8], F32)
    z = c.sb("c_z", [128, D], F32)
    tmp = c.sb("c_tmp", [128, D], F32)
    st = c.sb("c_st", [128, 12], F32)
    mv = c.sb("c_mv", [128, 8], F32)
    b_wo, b_Ob, b_dl, b_cmix, b_cT = Buf(), [Buf(), Buf()], [Buf(), Buf()], [Buf(), Buf()], [Buf(), Buf()]
    b_xres, b_ot, b_rd, b_z, b_tmp, b_small = [Buf(), Buf(), Buf()], [Buf(), Buf(), Buf()], Buf(), Buf(), Buf(), Buf()
    d_Ob = [c.dsem(f"c_dOb{i}") for i in range(2)]
    d_dl = [c.dsem(f"c_ddl{i}") for i in range(2)]
    d_xres = [c.dsem(f"c_dxres{i}") for i in range(3)]
    d_ot = [c.dsem(f"c_dot{i}") for i in range(3)]
    c.dma(c.SP, d_w, lambda: nc.sync.dma_start(out=w_out[:], in_=w_out_bf.rearrange("(k p) n -> p k n", p=128)), writes=[b_wo])
    nrot[0] = 8
    rd2 = [rd, c.sb("c_rd2", [128, 8], F32)]
    b_rd2 = [b_rd, Buf()]
    proj_banks = {}

    def stage1(bi_):
        mi = bi_ % 2
        r0 = bi_ * 128
        t, sub = bi_ // 4, bi_ % 4
        li = t % 2
        rdm, b_rdm = rd2[mi], b_rd2[mi]
        if sub == 0:
            c.dma(c.SP, d_dl[li], lambda: nc.sync.dma_start(out=dl[li][:], in_=dT_d[:, :, t * 512:(t + 1) * 512].rearrange("c p t -> p c t")),
                  writes=[b_dl[li]])
        c.dma(c.SP, d_Ob[mi], lambda: nc.sync.dma_start(out=Ob[mi][:], in_=obuf[r0:r0 + 128, :, :]), writes=[b_Ob[mi]])
        x3i = bi_ % 3
        c.dma(c.SP, d_xres[x3i], lambda: nc.sync.dma_start(out=xres[x3i][:], in_=x_in[r0:r0 + 128, :]), writes=[b_xres[x3i]])
        c.op(c.DVE, lambda: nc.vector.tensor_tensor(Ob[mi][:, 0, :], Ob[mi][:, 0, :], Ob[mi][:, 1, :], ALU.add),
             reads=[b_Ob[mi]], writes=[b_Ob[mi]])
        c.op(c.DVE, lambda: nc.vector.tensor_tensor(Ob[mi][:, 0, :], Ob[mi][:, 0, :], Ob[mi][:, 2, :], ALU.add),
             reads=[b_Ob[mi]], writes=[b_Ob[mi]])
        A3 = Ob[mi][:, 0, :].rearrange("p (h c) -> p h c", c=65)
        c.op(c.DVE, lambda: nc.vector.reciprocal(rdm[:].rearrange("p (h o) -> p h o", o=1), A3[:, :, 64:65]),
             reads=[b_Ob[mi]], writes=[b_rdm])
        c.op(c.DVE, lambda: nc.vector.tensor_tensor(cmix[mi][:].rearrange("p (h d) -> p h d", h=8), A3[:, :, 0:64],
                                                    rdm[:, 0:8].unsqueeze(2).to_broadcast([128, 8, 64]), ALU.mult),
             reads=[b_Ob[mi], b_rdm], writes=[b_cmix[mi]])
        bk = gb()
        pT = ps[bk][:].bitcast(BF16)

        def trm():
            ins = None
            for kc in range(4):
                ins = nc.tensor.transpose(pT[:, kc * 128:(kc + 1) * 128], cmix[mi][:, kc * 128:(kc + 1) * 128], ident[:])
            return ins
        c.op(c.PE, trm, reads=[b_cmix[mi]], writes=[bps[bk]])
        c.op(c.ACT, lambda: nc.scalar.copy(out=cT[mi][:], in_=pT[:, 0:512].rearrange("p (k t) -> p k t", k=4)),
             reads=[bps[bk]], writes=[b_cT[mi]])
        bos = []
        for dh in range(2):
            bo = gb()
            bos.append(bo)

            def fproj():
                ins = None
                for kc in range(KC):
                    lhs = cT[mi][:, kc, :] if kc < 4 else dl[li][:, kc - 4, sub * 128:(sub + 1) * 128]
                    ins = _mm(c, ps[bo][:], lhs, w_out[:, kc, dh * 512:(dh + 1) * 512], kc == 0, kc == KC - 1)
                return ins
            c.op(c.PE, fproj, reads=[b_cT[mi], b_dl[li], b_wo], writes=[bps[bo]])
        proj_banks[bi_] = bos

    def stage2(bi_):
        mi = bi_ % 2
        x3i = bi_ % 3
        r0 = bi_ * 128
        for dh in range(2):
            bo = proj_banks[bi_][dh]
            c.op(c.DVE, lambda: nc.vector.scalar_tensor_tensor(xres[x3i][:, dh * 512:(dh + 1) * 512], xres[x3i][:, dh * 512:(dh + 1) * 512],
                                                               ALPHA, ps[bo][:], ALU.mult, ALU.add),
                 reads=[bps[bo], b_xres[x3i]], writes=[b_xres[x3i]])

        def fin(Q, q):
            c.dma(Q, d_ot[x3i], lambda: q.dma_start(out=x_out[r0:r0 + 128, :], in_=ot[x3i][:]), reads=[b_ot[x3i]])
        lnp.push(xres[x3i][:], ot[x3i][:], b_xres[x3i], b_ot[x3i], fin)

    stage1(0)
    for bi_ in range(NBK):
        if bi_ + 1 < NBK:
            stage1(bi_ + 1)
        stage2(bi_)
        lnp.step()
    lnp.flush()
    c.barrier()
    c.release(m0)


def flat_view(ap, b):
    nd = len(ap.shape)
    names = " ".join(f"d{i}" for i in range(nd))
    f = ap.rearrange(f"{names} -> ({names})")
    return f.rearrange("(p a b) -> p a b", p=128, b=b)


def build_program(seq=S):
    nc = bass.Bass("TRN2", target_bir_lowering=False)
    c = make_ctx(nc)

    def inp(name, shape, dt=F32):
        return nc.dram_tensor(name, list(shape), dt, kind="ExternalInput").ap()

    def scr(name, shape, dt):
        return nc.dram_tensor(name, list(shape), dt).ap()

    x = inp("x", [seq, D])
    pos = inp("pos", [seq], I32)
    ln_w = inp("ln_w", [4, D])
    ln_b = inp("ln_b", [4, D])
    even_w_in = inp("even_w_in", [D, 2560])
    pool_w = inp("pool_w", [4, 64, 64])
    pool_scale = inp("pool_scale", [256])
    sinks = inp("sinks", [12])
    even_w_out = inp("even_w_out", [D, D])
    ffn_g = inp("ffn_w_gate", [1, D, D_FF])
    ffn_u = inp("ffn_w_up", [1, D, D_FF])
    ffn_d = inp("ffn_w_down", [1, D_FF, D])
    odd_w_in = inp("odd_w_in", [D, ODD_W])
    conv_w = inp("conv_w", [128, 4, 3])
    odd_w_out = inp("odd_w_out", [D, D])
    router = inp("router_wT", [N_EXP, D])
    router_kd = inp("router_w", [D, N_EXP])
    moe_g = inp("moe_w_gate", [N_EXP, D, D_FFE])
    moe_u = inp("moe_w_up", [N_EXP, D, D_FFE])
    moe_d = inp("moe_w_down", [N_EXP, D_FFE, D])
    invf = inp("invf", [128, 1])
    sgn = inp("sgn", [128, 1])
    wcol = inp("wcol", [128, 2])
    out = nc.dram_tensor("out", [seq, D], F32, kind="ExternalOutput").ap()

    x1 = scr("x1", [seq, D], F32)
    x2 = scr("x2", [seq, D], F32)
    x3 = scr("x3", [seq, D], F32)
    ffn_g_bf = scr("ffn_g_bf", [1, D, D_FF], BF16)
    ffn_u_bf = scr("ffn_u_bf", [1, D, D_FF], BF16)
    ffn_d_bf = scr("ffn_d_bf", [1, D_FF, D], BF16)
    odd_in_bf = scr("odd_in_bf", [D, ODD_W], BF16)
    odd_out_bf = scr("odd_out_bf", [D, D], BF16)
    NROW = N_EXP * MOE_NG * 128
    wg_s = scr("moe_wg_s", [NROW, 4096], BF16)
    wu_s = scr("moe_wu_s", [NROW, 4096], BF16)
    wd_s = scr("moe_wd_s", [NROW, 4096], BF16)
    NBLK = (2 * seq) // MOE_BLK + N_EXP - 1
    xs_d = scr("moe_xs", [NBLK * MOE_BLK, D], BF16)
    yrow_d = scr("moe_yrow", [NBLK * MOE_BLK, D], F32)
    obuf = scr("obuf", [seq, 3, 520], F32)
    dT_d = scr("dT_d", [4, 128, seq], BF16)

    ident, idx, iot = build_consts(c)
    rope = (c.sb("k_cos", [128, seq], F32), c.sb("k_sin", [128, seq], F32), Buf())
    pc_ffn = Eng(nc, None, "pc_ffn")
    pc_odd = Eng(nc, None, "pc_odd")
    pc_moe = [Eng(nc, None, f"pc_moe{e}") for e in range(N_EXP)]

    def cast(Dm, dst, src, b):
        c.dma(c.POOL, Dm, lambda: nc.gpsimd.dma_start(out=flat_view(dst, b), in_=flat_view(src, b)))

    def precasts():
        cast(pc_ffn, ffn_g_bf, ffn_g, 2048)
        cast(pc_ffn, ffn_u_bf, ffn_u, 2048)
        cast(pc_ffn, ffn_d_bf, ffn_d, 2048)
        cast(pc_odd, odd_in_bf, odd_w_in, 1536)
        cast(pc_odd, odd_out_bf, odd_w_out, 2048)

    def precast_expert(e):
        wg5 = wg_s.rearrange("(e g p) (k n) -> e g p k n", e=N_EXP, g=MOE_NG, k=KC)
        wu5 = wu_s.rearrange("(e g p) (k n) -> e g p k n", e=N_EXP, g=MOE_NG, k=KC)
        wd5 = wd_s.rearrange("(e g p) (c d) -> e g p c d", e=N_EXP, g=MOE_NG, c=4)
        if c.PE.n > 0:
            c._wait(c.POOL, [(c.PE, c.PE.n)])
        if True:
            sg_ = moe_g[e].rearrange("(k p) (g n) -> g p k n", p=128, n=512)
            su_ = moe_u[e].rearrange("(k p) (g n) -> g p k n", p=128, n=512)
            sd_ = moe_d[e].rearrange("(g c p) d -> g p c d", c=4, p=128)
            for g in range(MOE_NG):
                c.dma(c.POOL, pc_moe[e], lambda: nc.gpsimd.dma_start(out=wg5[e, g], in_=sg_[g]))
                c.dma(c.POOL, pc_moe[e], lambda: nc.gpsimd.dma_start(out=wu5[e, g], in_=su_[g]))
                c.dma(c.POOL, pc_moe[e], lambda: nc.gpsimd.dma_start(out=wd5[e, g], in_=sd_[g]))

    mixer0_phase(c, x, x1, pos, even_w_in, even_w_out, pool_w, pool_scale, sinks, ln_w[0], ln_b[0],
                 invf, sgn, wcol, ident, idx, iot, seq=seq, after_weights=precasts, rope=rope)
    ffn_phase(c, x1, x2, ffn_g_bf, ffn_u_bf, ffn_d_bf, 1, D_FF, 2, ln_w[1], ln_b[1], ident, TP=1024, seq=seq,
              wready=[[(pc_ffn, 48)]], pass_hooks=[(lambda e=e: precast_expert(e)) for e in range(4)])
    mixer1_phase(c, x2, x3, pos, odd_in_bf, odd_out_bf, conv_w, ln_w[2], ln_b[2], invf, sgn, obuf, dT_d, ident, idx,
                 wready=[(pc_odd, 32)], seq=seq, rope=rope, hooks=[(lambda e=e: precast_expert(e)) for e in range(4, 8)], zero_fill=xs_d)
    moe_sparse_phase(c, x3, out, wg_s, wu_s, wd_s, router, ln_w[3], ln_b[3], ident, idx, iot, c.gp, xs_d, yrow_d,
                     wready=[(pc_moe[e], 48 * MOE_NG) for e in range(N_EXP)], seq=seq, router_kd=router_kd)
    return nc


_CACHE = {}


def prep_shared(inputs):
    f = lambda a: np.ascontiguousarray(np.asarray(a))
    invf, sgn, wcol = rope_consts()
    conv = np.asarray(inputs["conv_w"])[0]
    conv_l = np.ascontiguousarray(conv.reshape(3, 4, 128).transpose(2, 1, 0))
    return {
        "ln_w": f(np.asarray(inputs["ln_w"]).reshape(4, D)),
        "ln_b": f(np.asarray(inputs["ln_b"]).reshape(4, D)),
        "even_w_in": f(np.asarray(inputs["even_w_in"])[0][:, even_perm()]),
        "pool_w": f(np.asarray(inputs["pool_w"])[0]),
        "pool_scale": f(np.asarray(inputs["pool_scale"])[0]),
        "sinks": f(np.asarray(inputs["swa_sinks"])[0]),
        "even_w_out": f(np.asarray(inputs["even_w_out"])[0]),
        "ffn_w_gate": f(inputs["ffn_w_gate"]),
        "ffn_w_up": f(inputs["ffn_w_up"]),
        "ffn_w_down": f(inputs["ffn_w_down"]),
        "odd_w_in": f(np.asarray(inputs["odd_w_in"])[0][:, odd_perm()]),
        "conv_w": conv_l,
        "odd_w_out": f(np.asarray(inputs["odd_w_out"])[0]),
        "router_wT": f(np.asarray(inputs["router_w"])[0].T),
        "router_w": f(np.asarray(inputs["router_w"])[0]),
        "moe_w_gate": f(np.asarray(inputs["moe_w_gate"])[0]),
        "moe_w_up": f(np.asarray(inputs["moe_w_up"])[0]),
        "moe_w_down": f(np.asarray(inputs["moe_w_down"])[0]),
        "invf": invf, "sgn": sgn, "wcol": wcol,
    }


def kernel(**inputs):
    x = np.asarray(inputs["x"], dtype=np.float32)
    pos = np.asarray(inputs["positions"]).astype(np.int32)
    shared = prep_shared(inputs)
    if "nc" not in _CACHE:
        _CACHE["nc"] = build_program()
    nc = _CACHE["nc"]
    in_maps = []
    for b in range(NB):
        m = dict(shared)
        m["x"] = np.ascontiguousarray(x[b])
        m["pos"] = np.ascontiguousarray(pos[b])
        in_maps.append(m)
    res = run_bass_kernel_spmd(nc, in_maps, core_ids=list(range(NB)))
    return np.stack([np.asarray(r["out"]) for r in res.results], axis=0).astype(np.float32)


U32 = mybir.dt.uint32
MOE_BLK = 512
MOE_NBLK = (2 * S) // MOE_BLK + N_EXP - 1
MOE_NG = D_FFE // 512
IND = bass.IndirectOffsetOnAxis


def moe_sparse_phase(c, x_in, x_out, wg_s, wu_s, wd_s, router_d, lnw_d, lnb_d, ident, idx, iot, gp,
                     xs_d, yrow_d, wready, seq=S, dbg=None, router_kd=None):
    nc = c.nc
    m0 = c.mark()
    NTK = seq // 128
    NBLK = (2 * seq) // MOE_BLK + N_EXP - 1
    ps, bps = c.psum, c.b_psum
    gcount = [0]

    def gb():
        k = gcount[0] % 4
        gcount[0] += 1
        return k

    GG = c.sb("m_GG", [128, NTK, 2], F32)
    DSTu = c.sb("m_DSTu", [128, NTK, 2], U32)
    IWu = c.sb("m_IWu", [128, NBLK * MOE_NG], U32)
    lnw = c.sb("m_lnw", [128, D], F32)
    lnb = c.sb("m_lnb", [128, D], F32)
    b_GG, b_DST, b_IW, b_const = Buf(), Buf(), Buf(), Buf()
    d_c = c.dsem("m_dc")
    c.dma(c.SP, d_c, lambda: nc.sync.dma_start(out=lnw[:], in_=lnw_d.partition_broadcast(128)), writes=[b_const])
    c.dma(c.SP, d_c, lambda: nc.sync.dma_start(out=lnb[:], in_=lnb_d.partition_broadcast(128)), writes=[b_const])

    m1 = c.mark()
    wr32 = c.sb("m_wr32", [128, KC, N_EXP], F32)
    id32 = c.sb("m_id32", [128, 128], F32)
    xT32 = [c.sb(f"m_xT32{i}", [128, KC, 128], F32) for i in range(2)]
    b_xT32 = [Buf(), Buf()]
    XB = c.sb("m_XB", [128, NTK, D], BF16)
    xin = [c.sb(f"m_xin{i}", [128, D], F32) for i in range(2)]
    tmp = c.sb("m_tmp", [128, D], F32)
    LG = c.sb("m_LG", [128, NTK, N_EXP], F32)
    L2 = c.sb("m_L2", [128, NTK, N_EXP], F32)
    CN = c.sb("m_CN", [128, NTK, N_EXP], F32)
    BASE = c.sb("m_BASE", [128, NTK, N_EXP], F32)
    sv = c.sb("m_sv", [128, 5, NTK], F32)
    RK = c.sb("m_RK", [128, NTK, N_EXP], F32)
    E1 = c.sb("m_E1", [128, NTK, N_EXP], F32)
    E2 = c.sb("m_E2", [128, NTK, N_EXP], F32)
    selb = c.sb("m_selb", [128, NTK * N_EXP], BF16)
    tri = c.sb("m_tri", [128, 128], BF16)
    ones = c.sb("m_ones", [128, 128], BF16)
    base = c.sb("m_base", [128, N_EXP], F32)
    sm = c.sb("m_sm", [128, 64], F32)
    smi = c.sb("m_smi", [128, 8], I32)
    DST = c.sb("m_DST", [128, NTK, 2], F32)
    IW = c.sb("m_IW", [128, NBLK, MOE_NG], F32)
    EBf = c.sb("m_EBf", [128, NBLK], F32)
    b_xin, b_XB = [Buf(), Buf()], [Buf() for _ in range(NTK)]
    b_tmp, b_lg, b_sel, b_base, b_sm = Buf(), Buf(), [Buf(), Buf()], Buf(), Buf()
    b_RK, b_E = Buf(), Buf()
    d_xin = [c.dsem(f"m_dxin{i}") for i in range(2)]
    d_sc = c.dsem("m_dsc")
    c.dma(c.SP, d_c, lambda: nc.sync.dma_start(out=wr32[:], in_=router_kd.rearrange("(k p) e -> p k e", p=128)), writes=[b_const])
    c.op(c.DVE, lambda: nc.vector.tensor_scalar(id32[:], idx[:], 0.0, None, ALU.is_equal), writes=[b_const])
    c.op(c.DVE, lambda: nc.vector.tensor_scalar(tri[:], idx[:], 0.0, None, ALU.is_gt), writes=[b_const])
    c.op(c.DVE, lambda: nc.vector.memset(ones[:], 1.0), writes=[b_const])
    c.op(c.DVE, lambda: nc.vector.memset(base[:], 0.0), writes=[b_base])
    c.barrier()
    for s in range(NTK):
        i = s % 2
        t0 = s * 128
        c.dma(c.SP, d_xin[i], lambda: nc.sync.dma_start(out=xin[i][:], in_=x_in[t0:t0 + 128, :]), writes=[b_xin[i]])
        c.op(c.ACT, lambda: nc.scalar.copy(out=XB[:, s, :], in_=xin[i][:]), reads=[b_xin[i]], writes=[b_XB[s]])
        bt0, bt1, bl = 4 + 2 * i, 5 + 2 * i, gb()
        for hfx, bt in enumerate((bt0, bt1)):
            def trx():
                ins = None
                for q in range(4):
                    kc = hfx * 4 + q
                    ins = nc.tensor.transpose(ps[bt][:, q * 128:(q + 1) * 128], xin[i][:, kc * 128:(kc + 1) * 128], id32[:])
                return ins
            c.op(c.PE, trx, reads=[b_xin[i]], writes=[bps[bt]])
            c.op(c.ACT, lambda: nc.scalar.copy(out=xT32[i][:, hfx * 4:(hfx + 1) * 4, :],
                                               in_=ps[bt][:].rearrange("p (k t) -> p k t", k=4)),
                 reads=[bps[bt]], writes=[b_xT32[i]])

        def mlog():
            ins = None
            for kc in range(KC):
                ins = _mm(c, ps[bl][:, 0:8], xT32[i][:, kc, :], wr32[:, kc, :], kc == 0, kc == KC - 1)
            return ins
        c.op(c.PE, mlog, reads=[b_xT32[i]], writes=[bps[bl]])
        c.op(c.DVE, lambda: nc.vector.tensor_copy(LG[:, s, :], ps[bl][:, 0:8]), reads=[bps[bl]], writes=[b_lg])
    T_ = NTK
    R = dict(reads=[b_lg, b_E], writes=[b_lg, b_E])

    def bc(ap2):
        return ap2.unsqueeze(2).to_broadcast([128, T_, N_EXP])
    M1, M2, DD, ED, DEN = sv[:, 0, :], sv[:, 1, :], sv[:, 2, :], sv[:, 3, :], sv[:, 4, :]
    c.op(c.DVE, lambda: nc.vector.reduce_max(M1, LG[:], axis=AX.X), **R)
    c.op(c.DVE, lambda: nc.vector.tensor_tensor(E1[:], LG[:], bc(M1), ALU.is_equal), **R)
    c.op(c.DVE, lambda: nc.vector.scalar_tensor_tensor(L2[:], E1[:], -1e30, LG[:], ALU.mult, ALU.add), **R)
    c.op(c.DVE, lambda: nc.vector.reduce_max(M2, L2[:], axis=AX.X), **R)
    c.op(c.DVE, lambda: nc.vector.tensor_tensor(E2[:], L2[:], bc(M2), ALU.is_equal), **R)
    c.op(c.DVE, lambda: nc.vector.tensor_tensor(L2[:], E1[:], E2[:], ALU.add), **R)
    c.op(c.DVE, lambda: nc.vector.tensor_copy(selb[:], L2[:].rearrange("p t e -> p (t e)")), reads=[b_lg], writes=[b_sel[0]])
    c.op(c.DVE, lambda: nc.vector.tensor_tensor(DD, M2, M1, ALU.subtract), **R)
    c.op(c.ACT, lambda: nc.scalar.activation(out=ED, in_=DD, func=AF.Exp), **R)
    c.op(c.DVE, lambda: nc.vector.tensor_scalar(DEN, ED, 1.0, None, ALU.add), **R)
    c.op(c.DVE, lambda: nc.vector.reciprocal(GG[:, :, 0], DEN), reads=[b_lg], writes=[b_GG])
    c.op(c.DVE, lambda: nc.vector.tensor_tensor(GG[:, :, 1], ED, GG[:, :, 0], ALU.mult), reads=[b_lg, b_GG], writes=[b_GG])
    b1, b2 = gb(), gb()
    NC_ = T_ * N_EXP
    c.op(c.PE, lambda: _mm(c, ps[b1][:, 0:NC_], tri[:], selb[:], True, True), reads=[b_sel[0]], writes=[bps[b1]])
    c.op(c.PE, lambda: _mm(c, ps[b2][:, 0:NC_], ones[:], selb[:], True, True), reads=[b_sel[0]], writes=[bps[b2]])
    c.op(c.DVE, lambda: nc.vector.tensor_copy(CN[:].rearrange("p t e -> p (t e)"), ps[b2][:, 0:NC_]), reads=[bps[b2]], writes=[b_base])
    c.op(c.DVE, lambda: nc.vector.memset(BASE[:, 0, :], 0.0), reads=[b_base], writes=[b_base])
    for s in range(1, T_):
        c.op(c.DVE, lambda: nc.vector.tensor_tensor(BASE[:, s, :], BASE[:, s - 1, :], CN[:, s - 1, :], ALU.add),
             reads=[b_base], writes=[b_base])
    c.op(c.DVE, lambda: nc.vector.tensor_tensor(base[:], BASE[:, T_ - 1, :], CN[:, T_ - 1, :], ALU.add), reads=[b_base], writes=[b_base])
    c.op(c.DVE, lambda: nc.vector.tensor_tensor(RK[:].rearrange("p t e -> p (t e)"), ps[b1][:, 0:NC_],
                                                BASE[:].rearrange("p t e -> p (t e)"), ALU.add),
         reads=[bps[b1], b_base], writes=[b_RK])
    cnt, padf, pst, pend = base[:], sm[:, 0:8], sm[:, 8:16], sm[:, 16:24]
    S_ = dict(reads=[b_base, b_sm], writes=[b_sm])
    qv, qf, qc = sm[:, 32:40], sm[:, 40:48], sm[:, 48:56]
    c.op(c.DVE, lambda: nc.vector.tensor_scalar(qv, cnt, float(MOE_BLK - 1), 1.0 / MOE_BLK, ALU.add, ALU.mult), **S_)
    c.op(c.DVE, lambda: nc.vector.tensor_copy(smi[:], qv), **S_)
    c.op(c.DVE, lambda: nc.vector.tensor_copy(qf, smi[:]), **S_)
    c.op(c.DVE, lambda: nc.vector.tensor_tensor(qc, qf, qv, ALU.is_gt), **S_)
    c.op(c.DVE, lambda: nc.vector.tensor_tensor(qf, qf, qc, ALU.subtract), **S_)
    c.op(c.DVE, lambda: nc.vector.tensor_scalar(padf, qf, float(MOE_BLK), None, ALU.mult), **S_)
    c.op(c.DVE, lambda: nc.vector.memset(pst[:, 0:1], 0.0), **S_)
    for e in range(1, N_EXP):
        c.op(c.DVE, lambda: nc.vector.tensor_tensor(pst[:, e:e + 1], pst[:, e - 1:e], padf[:, e - 1:e], ALU.add), **S_)
    c.op(c.DVE, lambda: nc.vector.tensor_tensor(pend, pst, padf, ALU.add), **S_)
    BVt = c.sb("m_bv", [128, NBLK], F32)
    BV = BVt[:]
    tbuf = c.sb("m_tb", [128, NBLK], F32)
    c.op(c.DVE, lambda: nc.vector.tensor_scalar(BV, iot[:, 0:NBLK], float(MOE_BLK), None, ALU.mult), **S_)
    c.op(c.DVE, lambda: nc.vector.memset(EBf[:], 0.0), **S_)
    for e in range(N_EXP):
        c.op(c.DVE, lambda: nc.vector.tensor_scalar(tbuf[:], BV, pend[:, e:e + 1], None, ALU.is_ge), **S_)
        c.op(c.DVE, lambda: nc.vector.tensor_tensor(EBf[:], EBf[:], tbuf[:], ALU.add), **S_)
    c.op(c.DVE, lambda: nc.vector.tensor_scalar(EBf[:], EBf[:], float(N_EXP - 1), float(MOE_NG * 128), ALU.min, ALU.mult), **S_)
    for b in range(NBLK):
        c.op(c.DVE, lambda: nc.vector.tensor_scalar(IW[:, b, :], gp[:], EBf[:, b:b + 1], None, ALU.add), **S_)
    c.op(c.DVE, lambda: nc.vector.tensor_copy(IWu[:], IW[:].rearrange("p b g -> p (b g)")), reads=[b_sm], writes=[b_IW])
    c.op(c.DVE, lambda: nc.vector.tensor_tensor(RK[:], RK[:], pst.unsqueeze(1).to_broadcast([128, T_, N_EXP]), ALU.add),
         reads=[b_RK, b_sm], writes=[b_RK])
    for k, EE in enumerate((E1, E2)):
        c.op(c.DVE, lambda: nc.vector.tensor_tensor(L2[:], RK[:], EE[:], ALU.mult), reads=[b_RK, b_E, b_lg], writes=[b_lg])
        c.op(c.DVE, lambda: nc.vector.reduce_sum(DST[:, :, k], L2[:], axis=AX.X), reads=[b_lg], writes=[b_DST])
    c.op(c.DVE, lambda: nc.vector.tensor_copy(DSTu[:], DST[:]), reads=[b_DST], writes=[b_DST])
    if dbg is not None:
        dd = c.dsem("m_dbg")
        c.dma(c.SP, dd, lambda: nc.sync.dma_start(out=dbg["dst"], in_=DSTu[:]), reads=[b_DST])
        c.dma(c.SP, dd, lambda: nc.sync.dma_start(out=dbg["iw"], in_=IWu[:]), reads=[b_IW])
        c.dma(c.SP, dd, lambda: nc.sync.dma_start(out=dbg["gg"], in_=GG[:]), reads=[b_GG])
        c.dma(c.SP, dd, lambda: nc.sync.dma_start(out=dbg["sm"], in_=sm[:]), reads=[b_sm])
        c.barrier()
        if dbg.get("stage", "route") == "route":
            return
    for s in range(NTK):
        for k in range(2):
            c.dma(c.POOL, d_sc, lambda: nc.gpsimd.indirect_dma_start(out=xs_d[:, :], out_offset=IND(ap=DSTu[:, s, k:k + 1], axis=0),
                                                                     in_=XB[:, s, :], in_offset=None),
                  reads=[b_DST, b_XB[s]])
    c.barrier()
    c.release(m1)
    if dbg is not None and dbg.get("stage") == "scatter":
        return

    m2 = c.mark()
    xsb = [c.sb(f"m_xsb{i}", [128, 4, D], BF16) for i in range(2)]
    xT2 = [c.sb(f"m_xT{i}", [128, KC, 512], BF16) for i in range(2)]
    hT = c.sb("m_hT", [128, 4 * MOE_NG, 512], BF16)
    wgb = [c.sb(f"m_wg{i}", [128, KC, 512], BF16) for i in range(2)]
    wub = [c.sb(f"m_wu{i}", [128, KC, 512], BF16) for i in range(2)]
    wdf = c.sb("m_wd", [128, MOE_NG, 4, D], BF16)
    sg = [c.sb(f"m_sg{i}", [128, 512], BF16) for i in range(2)]
    yt = [c.sb(f"m_yt{i}", [128, D], F32) for i in range(2)]
    b_xsb, b_xT2 = [Buf(), Buf()], [[Buf() for _ in range(4)] for _ in range(2)]
    b_hT = [Buf() for _ in range(4 * MOE_NG)]
    b_wg, b_wu, b_wd = [Buf(), Buf()], [Buf(), Buf()], [Buf() for _ in range(MOE_NG)]
    b_sg, b_yt = [Buf(), Buf()], [Buf(), Buf()]
    d_xsb = [c.dsem(f"m_dxsb{i}") for i in range(2)]
    d_wg = [c.dsem(f"m_dwg{i}") for i in range(2)]
    d_wu = [c.dsem(f"m_dwu{i}") for i in range(2)]
    d_wd = [c.dsem(f"m_dwd{i}") for i in range(MOE_NG)]
    d_yt = [c.dsem(f"m_dyt{i}") for i in range(2)]
    psG, psU, psY = ps[0:2], ps[2:4], ps[4:8]
    b_psG, b_psU, b_psY = bps[0:2], bps[2:4], bps[4:8]
    if wready:
        c._wait(c.POOL, wready)
    wg3 = wg_s.rearrange("r (k n) -> r k n", k=KC)
    wu3 = wu_s.rearrange("r (k n) -> r k n", k=KC)
    wd3 = wd_s.rearrange("r (c d) -> r c d", c=4)
    NGT = NBLK * MOE_NG
    loaded = [-1]

    def load_gu(j):
        if j >= NGT or j <= loaded[0]:
            return
        loaded[0] = j
        sl = j % 2
        c.dma(c.POOL, d_wg[sl], lambda: nc.gpsimd.indirect_dma_start(out=wgb[sl][:].rearrange("p k n -> p (k n)"), out_offset=None, in_=wg_s[:, :],
                                                                     in_offset=IND(ap=IWu[:, j:j + 1], axis=0)),
              reads=[b_IW], writes=[b_wg[sl]])
        c.dma(c.POOL, d_wu[sl], lambda: nc.gpsimd.indirect_dma_start(out=wub[sl][:].rearrange("p k n -> p (k n)"), out_offset=None, in_=wu_s[:, :],
                                                                     in_offset=IND(ap=IWu[:, j:j + 1], axis=0)),
              reads=[b_IW], writes=[b_wu[sl]])

    def load_wd(b):
        if b >= NBLK:
            return
        for g in range(MOE_NG):
            j = b * MOE_NG + g
            c.dma(c.POOL, d_wd[g], lambda: nc.gpsimd.indirect_dma_start(out=wdf[:, g, :, :].rearrange("p c d -> p (c d)"), out_offset=None, in_=wd_s[:, :],
                                                                        in_offset=IND(ap=IWu[:, j:j + 1], axis=0)),
                  reads=[b_IW], writes=[b_wd[g]])

    def load_x(b):
        if b >= NBLK:
            return
        i = b % 2
        c.dma(c.SP, d_xsb[i], lambda: nc.sync.dma_start(
            out=xsb[i][:], in_=xs_d[b * MOE_BLK:(b + 1) * MOE_BLK, :].rearrange("(s p) d -> p s d", p=128)), writes=[b_xsb[i]])

    load_x(0)
    load_gu(0)
    load_gu(1)
    load_wd(0)
    kk = 0
    ycount = 0
    def transposes(b):
        if b >= NBLK:
            return
        i = b % 2
        xT, b_xT = xT2[i], b_xT2[i]
        for sub in range(4):
            bk = 4 + sub
            pT = ps[bk][:].bitcast(BF16)

            def tr():
                ins = None
                for kc in range(KC):
                    ins = nc.tensor.transpose(pT[:, kc * 128:(kc + 1) * 128], xsb[i][:, sub, kc * 128:(kc + 1) * 128], ident[:])
                return ins
            c.op(c.PE, tr, reads=[b_xsb[i]], writes=[bps[bk]])
            c.op(c.ACT, lambda: nc.scalar.copy(out=xT[:, :, sub * 128:(sub + 1) * 128],
                                               in_=pT.rearrange("p (k t) -> p k t", k=KC)),
                 reads=[bps[bk]], writes=[b_xT[sub]])

    load_x(1)
    transposes(0)
    for b in range(NBLK):
        load_x(b + 2)
        i = b % 2
        xT, b_xT = xT2[i], b_xT2[i]
        for g in range(MOE_NG):
            j = b * MOE_NG + g
            sl = j % 2
            for ci in range(4):
                k = kk % 2
                kk += 1
                ch = g * 4 + ci

                def mmg(w, out):
                    ins = None
                    for kc in range(KC):
                        ins = _mm(c, out[:], w[sl][:, kc, ci * 128:(ci + 1) * 128], xT[:, kc, :], kc == 0, kc == KC - 1)
                    return ins
                c.op(c.PE, lambda: mmg(wgb, psG[k]), reads=[b_wg[sl]] + b_xT, writes=[b_psG[k]])
                c.op(c.PE, lambda: mmg(wub, psU[k]), reads=[b_wu[sl]] + b_xT, writes=[b_psU[k]])
                c.op(c.ACT, lambda: nc.scalar.activation(out=sg[k][:], in_=psG[k][:], func=AF.Silu),
                     reads=[b_psG[k]], writes=[b_sg[k]])
                c.op(c.DVE, lambda: nc.vector.tensor_tensor(hT[:, ch, :], sg[k][:], psU[k][:], ALU.mult),
                     reads=[b_sg[k], b_psU[k]], writes=[b_hT[ch]])
            load_gu(j + 2)
        transposes(b + 1)
        for hf in range(2):
            for sb2 in range(2):
                sub = hf * 2 + sb2
                for dh in range(2):
                    bk = sb2 * 2 + dh

                    def mmd():
                        ins = None
                        for ch in range(4 * MOE_NG):
                            ins = _mm(c, psY[bk][:], hT[:, ch, sub * 128:(sub + 1) * 128],
                                      wdf[:, ch // 4, ch % 4, dh * 512:(dh + 1) * 512], ch == 0, ch == 4 * MOE_NG - 1)
                        return ins
                    c.op(c.PE, mmd, reads=b_wd + b_hT, writes=[b_psY[bk]])
                yi = ycount % 2
                ycount += 1
                for dh in range(2):
                    bk = sb2 * 2 + dh
                    eng = c.ACT if dh == 0 else c.DVE
                    if dh == 0:
                        c.op(c.ACT, lambda: nc.scalar.copy(out=yt[yi][:, 0:512], in_=psY[bk][:]), reads=[b_psY[bk]], writes=[b_yt[yi]])
                    else:
                        c.op(c.DVE, lambda: nc.vector.tensor_copy(yt[yi][:, 512:1024], psY[bk][:]), reads=[b_psY[bk]], writes=[b_yt[yi]])
                r0 = b * MOE_BLK + sub * 128
                c.dma(c.SP, d_yt[yi], lambda: nc.sync.dma_start(out=yrow_d[r0:r0 + 128, :], in_=yt[yi][:]), reads=[b_yt[yi]])
        load_wd(b + 1)
    c.barrier()
    c.release(m2)
    if dbg is not None and dbg.get("stage") == "blocks":
        return

    y1 = [c.sb(f"m_y1{i}", [128, D], F32) for i in range(4)]
    y2 = [c.sb(f"m_y2{i}", [128, D], F32) for i in range(4)]
    xr = [c.sb(f"m_xr{i}", [128, D], F32) for i in range(5)]
    lnp = LNPipe(c, lnw[:], lnb[:], "m_ln", pool_add=True, store="sp_delayed")
    tmp2 = c.sb("m_tmp2", [128, D], F32)
    st = c.sb("m_st", [128, 12], F32)
    mv = c.sb("m_mv", [128, 8], F32)
    b_y1, b_y2, b_xr = [Buf() for _ in range(4)], [Buf() for _ in range(4)], [Buf() for _ in range(5)]
    b_tmp2, b_small = Buf(), Buf()
    d_y1 = [c.dsem(f"m_dy1{i}") for i in range(4)]
    d_y2 = [c.dsem(f"m_dy2{i}") for i in range(4)]
    d_xr = [c.dsem(f"m_dxr{i}") for i in range(5)]
    d_o = [c.dsem(f"m_do{i}") for i in range(5)]
    def gath(s):
        if s >= NTK:
            return
        i = s % 4
        c.dma(c.POOL, d_y1[i], lambda: nc.gpsimd.indirect_dma_start(out=y1[i][:], out_offset=None, in_=yrow_d[:, :],
                                                                    in_offset=IND(ap=DSTu[:, s, 0:1], axis=0)),
              reads=[b_DST], writes=[b_y1[i]])
        c.dma(c.POOL, d_y2[i], lambda: nc.gpsimd.indirect_dma_start(out=y2[i][:], out_offset=None, in_=yrow_d[:, :],
                                                                    in_offset=IND(ap=DSTu[:, s, 1:2], axis=0)),
              reads=[b_DST], writes=[b_y2[i]])

    def ldx(s):
        if s >= NTK:
            return
        j = s % 5
        t0 = s * 128
        c.dma(c.SP, d_xr[j], lambda: nc.sync.dma_start(out=xr[j][:], in_=x_in[t0:t0 + 128, :]), writes=[b_xr[j]])
        c.op(c.ACT, lambda: nc.scalar.activation(out=xr[j][:], in_=xr[j][:], func=AF.Copy, scale=ALPHA),
             reads=[b_xr[j]], writes=[b_xr[j]])

    gath(0)
    gath(1)
    gath(2)
    ldx(0)
    for s in range(NTK):
        i = s % 4
        j = s % 5
        t0 = s * 128
        lnp.step()
        ldx(s + 1)
        c.op(c.DVE, lambda: nc.vector.scalar_tensor_tensor(xr[j][:], y1[i][:], GG[:, s, 0:1], xr[j][:], ALU.mult, ALU.add),
             reads=[b_y1[i], b_xr[j], b_GG], writes=[b_xr[j]])
        c.op(c.DVE, lambda: nc.vector.scalar_tensor_tensor(xr[j][:], y2[i][:], GG[:, s, 1:2], xr[j][:], ALU.mult, ALU.add),
             reads=[b_y2[i], b_xr[j], b_GG], writes=[b_xr[j]])
        gath(s + 3)

        def fin(Q, q, j=j, t0=t0):
            c.dma(Q, d_o[j], lambda: q.dma_start(out=x_out[t0:t0 + 128, :], in_=xr[j][:]), reads=[b_xr[j]])
        lnp.push(xr[j][:], xr[j][:], b_xr[j], b_xr[j], fin)
    lnp.flush()
    c.barrier()
    c.release(m0)
```
